# Optimizing a Trainium2 kernel written in Bass

```python
import math
import jax
import jax.numpy as jnp
from jax import lax
import numpy as np

D_MODEL = 1024
BATCH = 4
SEQ = 8192
DEPTH = 2

D_MIX = D_MODEL
DIFF_HEADS = 4
DIFF_HD = 32
DIL_HEADS = 4
DIL_HD = 64
DIL_PATTERNS = ((128, 1), (512, 4), (2048, 16))
MLA_HEADS = 4
MLA_Q_LORA = 384
MLA_KV_LORA = 128
MLA_NOPE = 64
MLA_ROPE = 32
MLA_V = 64
ROPE_THETA = 10000.0
SWA_HEADS = 4
SWA_KV_HEADS = 2
SWA_HD = 64
SWA_WINDOW = 128
Q_BLOCK = 128
BAND_BLOCK = 128
D_FF = 2816
N_EXPERTS = 8
TOP_K = 2
D_FF_EXPERT = 3584
MOE_BLOCK = 256
RMS_EPS = 1e-6
NEG_INF = -1e30
N_ALIBI = DIFF_HEADS + DIL_HEADS + SWA_HEADS

IN_WIDTHS = (
    DIFF_HEADS * 2 * DIFF_HD, DIFF_HEADS * 2 * DIFF_HD, DIFF_HEADS * 2 * DIFF_HD,
    DIL_HEADS * DIL_HD, DIL_HEADS * DIL_HD, DIL_HEADS * DIL_HD,
    MLA_Q_LORA, MLA_KV_LORA, MLA_ROPE,
    SWA_HEADS * SWA_HD, SWA_KV_HEADS * SWA_HD, SWA_KV_HEADS * SWA_HD,
)
IN_COLS = 2592

kernel_name = 'hybrid_parallel_heads_diff_dilated_mla_swa_moe'


def _rmsnorm(x, g):
    xf = x.astype(jnp.float32)
    y = xf * lax.rsqrt(jnp.mean(xf * xf, axis=-1, keepdims=True) + RMS_EPS)
    return (y * g.astype(jnp.float32)).astype(x.dtype)


def _alibi_slopes():
    s = 2.0 ** (-8.0 * (np.arange(N_ALIBI) + 1) / N_ALIBI)
    f = lambda a: jnp.asarray(a, dtype=jnp.float32)
    return f(s[0::3]), f(s[1::3]), f(s[2::3])


def _split_heads(t, n, dh):
    b, s = t.shape[:2]
    return t.reshape(b, s, n, dh).transpose(0, 2, 1, 3)


def _merge_heads(t):
    b, h, s, dh = t.shape
    return t.transpose(0, 2, 1, 3).reshape(b, s, h * dh)


def _to_query_blocks(t):
    b, h, s, dh = t.shape
    return t.reshape(b, h, s // Q_BLOCK, Q_BLOCK, dh).transpose(2, 0, 1, 3, 4)


def _from_query_blocks(t):
    nb, b, h, qb, dh = t.shape
    return t.transpose(1, 2, 0, 3, 4).reshape(b, h, nb * qb, dh)


def _causal_block_probs(q_blk, k, q_start, scale, slopes):
    s = jnp.einsum('bhqd,bhkd->bhqk', q_blk, k).astype(jnp.float32) * scale
    dist = (q_start + jnp.arange(q_blk.shape[2]))[:, None] - jnp.arange(k.shape[2])[None, :]
    if slopes is not None:
        s = s - slopes[:, None, None] * dist.astype(jnp.float32)
    s = jnp.where(dist >= 0, s, NEG_INF)
    return jax.nn.softmax(s, axis=-1)


def _diff_attention(q1, q2, k1, k2, v, lam, slopes):
    scale = DIFF_HD ** -0.5
    nb = q1.shape[2] // Q_BLOCK

    def body(args):
        qa, qb, i = args
        start = i * Q_BLOCK
        p = (_causal_block_probs(qa, k1, start, scale, slopes)
             - lam * _causal_block_probs(qb, k2, start, scale, slopes))
        return jnp.einsum('bhqk,bhkd->bhqd', p.astype(v.dtype), v)

    o = lax.map(body, (_to_query_blocks(q1), _to_query_blocks(q2), jnp.arange(nb)))
    return _from_query_blocks(o)


def _dense_causal_attention(q, k, v, scale):
    nb = q.shape[2] // Q_BLOCK

    def body(args):
        qb, i = args
        p = _causal_block_probs(qb, k, i * Q_BLOCK, scale, None)
        return jnp.einsum('bhqk,bhkd->bhqd', p.astype(v.dtype), v)

    o = lax.map(body, (_to_query_blocks(q), jnp.arange(nb)))
    return _from_query_blocks(o)


def _banded_attention(q, k, v, max_dist, dist_unit, slopes, sinks):
    bn, g, r, n, dh = q.shape
    L = BAND_BLOCK
    pad = (-n) % L
    if pad:
        q = jnp.pad(q, ((0, 0), (0, 0), (0, 0), (0, pad), (0, 0)))
        k = jnp.pad(k, ((0, 0), (0, 0), (0, pad), (0, 0)))
        v = jnp.pad(v, ((0, 0), (0, 0), (0, pad), (0, 0)))
    npad = n + pad
    nb = npad // L
    qb = q.reshape(bn, g, r, nb, L, dh)

    def band(t):
        tb = t.reshape(bn, g, nb, L, dh)
        prev = jnp.pad(tb, ((0, 0), (0, 0), (1, 0), (0, 0), (0, 0)))[:, :, :nb]
        return jnp.concatenate([prev, tb], axis=3)

    kk, vv = band(k), band(v)
    s = jnp.einsum('bgrnqd,bgnkd->bgrnqk', qb, kk).astype(jnp.float32) * (dh ** -0.5)
    dist = jnp.arange(L)[:, None] + L - jnp.arange(2 * L)[None, :]
    k_pos = jnp.arange(nb)[:, None, None] * L + jnp.arange(2 * L)[None, None, :] - L
    valid = (dist >= 0) & (dist <= max_dist) & (k_pos >= 0)
    if slopes is not None:
        s = s - slopes[None, :, :, None, None, None] * (dist * dist_unit).astype(jnp.float32)
    s = jnp.where(valid, s, NEG_INF)
    m = jnp.max(s, axis=-1)
    if sinks is not None:
        sk = sinks.astype(jnp.float32)[None, :, :, None, None]
        m = jnp.maximum(m, sk)
    p = jnp.exp(s - m[..., None])
    denom = jnp.sum(p, axis=-1)
    if sinks is not None:
        denom = denom + jnp.exp(sk - m)
    o = jnp.einsum('bgrnqk,bgnkd->bgrnqd', p.astype(vv.dtype), vv)
    o = (o.astype(jnp.float32) / denom[..., None]).astype(v.dtype)
    lse = m + jnp.log(denom)
    o = o.reshape(bn, g, r, npad, dh)[:, :, :, :n]
    lse = lse.reshape(bn, g, r, npad)[..., :n]
    return o, lse


def _to_strided(t, d):
    b, h, s, dh = t.shape
    return t.reshape(b, h, s // d, d, dh).transpose(0, 3, 1, 2, 4).reshape(b * d, h, s // d, dh)


def _from_strided(t, d, b):
    _, h, n, dh = t.shape
    return t.reshape(b, d, h, n, dh).transpose(0, 2, 3, 1, 4).reshape(b, h, n * d, dh)


def _dilated_attention(q, k, v, slopes):
    bn = q.shape[0]
    outs, lses = [], []
    for window, d in DIL_PATTERNS:
        qs, ks, vs = _to_strided(q, d), _to_strided(k, d), _to_strided(v, d)
        o, lse = _banded_attention(qs[:, :, None], ks, vs, window // d, d, slopes[:, None], None)
        outs.append(_from_strided(o[:, :, 0], d, bn))
        lses.append(_from_strided(lse[:, :, 0][..., None], d, bn)[..., 0])
    w = jax.nn.softmax(jnp.stack(lses), axis=0)
    return jnp.einsum('gbhs,gbhsd->bhsd', w.astype(q.dtype), jnp.stack(outs))


def _rope_tables(s, dtype):
    inv = ROPE_THETA ** (-jnp.arange(0, MLA_ROPE, 2, dtype=jnp.float32) / MLA_ROPE)
    ang = jnp.arange(s, dtype=jnp.float32)[:, None] * inv[None, :]
    return jnp.cos(ang).astype(dtype), jnp.sin(ang).astype(dtype)


def _rope(t, cos, sin):
    half = MLA_ROPE // 2
    t1, t2 = t[..., :half], t[..., half:]
    return jnp.concatenate([t1 * cos - t2 * sin, t2 * cos + t1 * sin], axis=-1)


def _diff_mixer(qa, ka, va, lam_params, subln, layer_idx, slopes):
    bn, s, _ = qa.shape
    q12 = qa.reshape(bn, s, DIFF_HEADS, 2, DIFF_HD).transpose(3, 0, 2, 1, 4)
    k12 = ka.reshape(bn, s, DIFF_HEADS, 2, DIFF_HD).transpose(3, 0, 2, 1, 4)
    v = _split_heads(va, DIFF_HEADS, 2 * DIFF_HD)
    lam_init = 0.8 - 0.6 * math.exp(-0.3 * layer_idx)
    lp = lam_params.astype(jnp.float32)
    lam = jnp.exp(jnp.sum(lp[0] * lp[1])) - jnp.exp(jnp.sum(lp[2] * lp[3])) + lam_init
    o = _diff_attention(q12[0], q12[1], k12[0], k12[1], v, lam, slopes)
    o = _rmsnorm(o, subln) * (1.0 - lam_init)
    return _merge_heads(o)


def _dilated_mixer(qb, kb, vb, slopes):
    q = _split_heads(qb, DIL_HEADS, DIL_HD)
    k = _split_heads(kb, DIL_HEADS, DIL_HD)
    v = _split_heads(vb, DIL_HEADS, DIL_HD)
    return _merge_heads(_dilated_attention(q, k, v, slopes))


def _mla_mixer(cq, ckv, kpe, q_norm, w_uq, kv_norm, w_ukv):
    bn, s, _ = cq.shape
    q = _split_heads(_rmsnorm(cq, q_norm) @ w_uq, MLA_HEADS, MLA_NOPE + MLA_ROPE)
    kv = _split_heads(_rmsnorm(ckv, kv_norm) @ w_ukv, MLA_HEADS, MLA_NOPE + MLA_V)
    cos, sin = _rope_tables(s, cq.dtype)
    q = jnp.concatenate([q[..., :MLA_NOPE], _rope(q[..., MLA_NOPE:], cos, sin)], axis=-1)
    k_pe = jnp.broadcast_to(_rope(kpe[:, None], cos, sin), (bn, MLA_HEADS, s, MLA_ROPE))
    k = jnp.concatenate([kv[..., :MLA_NOPE], k_pe], axis=-1)
    v = kv[..., MLA_NOPE:]
    return _merge_heads(_dense_causal_attention(q, k, v, (MLA_NOPE + MLA_ROPE) ** -0.5))


def _swa_mixer(qd, kd, vd, sinks, slopes):
    bn, s, _ = qd.shape
    rep = SWA_HEADS // SWA_KV_HEADS
    q = qd.reshape(bn, s, SWA_KV_HEADS, rep, SWA_HD).transpose(0, 2, 3, 1, 4)
    k = _split_heads(kd, SWA_KV_HEADS, SWA_HD)
    v = _split_heads(vd, SWA_KV_HEADS, SWA_HD)
    o, _ = _banded_attention(q, k, v, SWA_WINDOW - 1, 1,
                             slopes.reshape(SWA_KV_HEADS, rep), sinks.reshape(SWA_KV_HEADS, rep))
    return o.transpose(0, 3, 1, 2, 4).reshape(bn, s, SWA_HEADS * SWA_HD)


def _swiglu(h, w1, w3, w2):
    return (jax.nn.silu(h @ w1) * (h @ w3)) @ w2


def _moe_swiglu(h, router, w1, w3, w2):
    bn, s, d = h.shape
    t = bn * s
    xt = h.reshape(t, d)
    logits = (xt @ router).astype(jnp.float32)
    top_v, top_i = lax.top_k(logits, TOP_K)
    gates = jax.nn.softmax(top_v, axis=-1)
    a = t * TOP_K
    e_flat = top_i.reshape(a)
    tok_flat = jnp.repeat(jnp.arange(t, dtype=jnp.int32), TOP_K)
    g_flat = gates.reshape(a)
    order = jnp.argsort(e_flat)
    e_s, tok_s, g_s = e_flat[order], tok_flat[order], g_flat[order]
    counts = jnp.bincount(e_flat, length=N_EXPERTS)
    starts = jnp.cumsum(counts) - counts
    padded = (counts + MOE_BLOCK - 1) // MOE_BLOCK * MOE_BLOCK
    pends = jnp.cumsum(padded)
    pstarts = pends - padded
    dest = pstarts[e_s] + jnp.arange(a) - starts[e_s]
    nb = -(-a // MOE_BLOCK) + N_EXPERTS
    rows = nb * MOE_BLOCK
    row_tok = jnp.zeros((rows,), jnp.int32).at[dest].set(tok_s)
    row_gate = jnp.zeros((rows,), jnp.float32).at[dest].set(g_s)
    blk_expert = jnp.clip(jnp.searchsorted(pends, jnp.arange(nb) * MOE_BLOCK, side='right'), 0, N_EXPERTS - 1)

    def body(args):
        tok, g, e = args
        xb = xt[tok]
        y = (jax.nn.silu(xb @ w1[e]) * (xb @ w3[e])) @ w2[e]
        return y * g[:, None].astype(y.dtype)

    ys = lax.map(body, (row_tok.reshape(nb, MOE_BLOCK), row_gate.reshape(nb, MOE_BLOCK), blk_expert))
    out = jnp.zeros((t, d), h.dtype).at[row_tok].add(ys.reshape(rows, d).astype(h.dtype))
    return out.reshape(bn, s, d)


def setup_inputs(seed: int = 0) -> dict:
    key = jax.random.key(seed)
    ks = jax.random.split(key, 24)
    n_dense = (DEPTH + 1) // 2
    n_moe = DEPTH // 2
    nrm = lambda k, shape, sc: jax.random.normal(k, shape, jnp.float32) * sc
    gain = lambda k, shape: 1.0 + 0.02 * jax.random.normal(k, shape, jnp.float32)
    return {
        'x': nrm(ks[0], (BATCH, SEQ, D_MODEL), 1.0),
        'attn_norm': gain(ks[1], (DEPTH, D_MODEL)),
        'w_in': nrm(ks[2], (DEPTH, D_MODEL, IN_COLS), D_MODEL ** -0.5),
        'w_out': nrm(ks[3], (DEPTH, D_MIX, D_MODEL), D_MIX ** -0.5),
        'diff_lambda': nrm(ks[4], (DEPTH, 4, DIFF_HD), 0.1),
        'diff_subln': gain(ks[5], (DEPTH, 2 * DIFF_HD)),
        'mla_q_norm': gain(ks[6], (DEPTH, MLA_Q_LORA)),
        'mla_w_uq': nrm(ks[7], (DEPTH, MLA_Q_LORA, MLA_HEADS * (MLA_NOPE + MLA_ROPE)), MLA_Q_LORA ** -0.5),
        'mla_kv_norm': gain(ks[8], (DEPTH, MLA_KV_LORA)),
        'mla_w_ukv': nrm(ks[9], (DEPTH, MLA_KV_LORA, MLA_HEADS * (MLA_NOPE + MLA_V)), MLA_KV_LORA ** -0.5),
        'swa_sinks': nrm(ks[10], (DEPTH, SWA_HEADS), 0.5),
        'ffn_norm': gain(ks[11], (DEPTH, D_MODEL)),
        'ffn_w1': nrm(ks[12], (n_dense, D_MODEL, D_FF), D_MODEL ** -0.5),
        'ffn_w3': nrm(ks[13], (n_dense, D_MODEL, D_FF), D_MODEL ** -0.5),
        'ffn_w2': nrm(ks[14], (n_dense, D_FF, D_MODEL), D_FF ** -0.5),
        'moe_router': nrm(ks[15], (n_moe, D_MODEL, N_EXPERTS), D_MODEL ** -0.5),
        'moe_w1': nrm(ks[16], (n_moe, N_EXPERTS, D_MODEL, D_FF_EXPERT), D_MODEL ** -0.5),
        'moe_w3': nrm(ks[17], (n_moe, N_EXPERTS, D_MODEL, D_FF_EXPERT), D_MODEL ** -0.5),
        'moe_w2': nrm(ks[18], (n_moe, N_EXPERTS, D_FF_EXPERT, D_MODEL), D_FF_EXPERT ** -0.5),
        'final_norm': gain(ks[19], (D_MODEL,)),
    }


def reference(x, attn_norm, w_in, w_out, diff_lambda, diff_subln, mla_q_norm, mla_w_uq, mla_kv_norm,
              mla_w_ukv, swa_sinks, ffn_norm, ffn_w1, ffn_w3, ffn_w2, moe_router, moe_w1, moe_w3, moe_w2,
              final_norm):
    slopes_a, slopes_b, slopes_d = _alibi_slopes()
    split_points = [int(c) for c in np.cumsum(IN_WIDTHS)[:-1]]
    for l in range(DEPTH):
        h = _rmsnorm(x, attn_norm[l])
        (qa, ka, va, qb, kb, vb, cq, ckv, kpe, qd, kd, vd) = jnp.split(h @ w_in[l], split_points, axis=-1)
        o_a = _diff_mixer(qa, ka, va, diff_lambda[l], diff_subln[l], l, slopes_a)
        o_b = _dilated_mixer(qb, kb, vb, slopes_b)
        o_c = _mla_mixer(cq, ckv, kpe, mla_q_norm[l], mla_w_uq[l], mla_kv_norm[l], mla_w_ukv[l])
        o_d = _swa_mixer(qd, kd, vd, swa_sinks[l], slopes_d)
        x = x + jnp.concatenate([o_a, o_b, o_c, o_d], axis=-1) @ w_out[l]
        h2 = _rmsnorm(x, ffn_norm[l])
        if l % 2 == 0:
            j = l // 2
            x = x + _swiglu(h2, ffn_w1[j], ffn_w3[j], ffn_w2[j])
        else:
            j = l // 2
            x = x + _moe_swiglu(h2, moe_router[j], moe_w1[j], moe_w3[j], moe_w2[j])
    return _rmsnorm(x, final_norm)
```

```python
import contextlib
import numpy as np
import ml_dtypes
import concourse.bass as bass
import concourse.mybir as mybir
from concourse.bass_utils import run_bass_kernel_spmd

F32 = mybir.dt.float32
BF16 = mybir.dt.bfloat16
AF = mybir.ActivationFunctionType
ALU = mybir.AluOpType

ENGS = ("pe", "act", "dve", "pool", "sp")
DMAQ = ("sp", "pool", "act")

D = 1024
KC = 8
EPS = 1e-6
NEG = -1e30
D_FF = 2816
NFC0 = 22
D_FFE = 3584
NFCE = 28
NEXP = 8
NCOLS = 2624
GROWS = 1504
FMROWS = 1056
R_A = 0
R_B = 256
R_CQ = 512
R_CK = 704
R_KPE = 832
R_DQ = 864
R_DK = 992
VSLOT = {"A0": 0, "A1": 1, "B0": 2, "B1": 3, "C0": 4, "C1": 5, "D": 6}
HMAP_A = [[0, 3], [1, 2]]
INV_A = {0: (0, 0), 3: (0, 1), 1: (1, 0), 2: (1, 1)}
ALIBI_TH = 80.0


class Op:
    __slots__ = ("eng", "fn", "waits", "needed", "idx", "sigval", "kind", "slot", "seq", "q")

    def __init__(self, eng, fn, kind):
        self.eng = eng
        self.fn = fn
        self.kind = kind
        self.waits = []
        self.needed = False
        self.idx = -1
        self.sigval = 0
        self.slot = -1
        self.seq = 0
        self.q = None


class Sched:
    def __init__(self, nc, n_slots=8):
        self.nc = nc
        self.n_slots = n_slots
        self.lists = {e: [] for e in ENGS}
        self.last_write = {}
        self.readers = {}
        self.waited = {e: {} for e in ENGS}
        self.waited_d = {e: {} for e in ENGS}
        self.slots = {q: [None] * n_slots for q in DMAQ}
        self.rr = {q: 0 for q in DMAQ}
        self.rr_bg = 0
        self.cc_count = 0
        self.waited_cc = {e: 0 for e in ENGS}
        self.groups = {}

    def _dep(self, x, d):
        if d is None or d is x:
            return
        if d.kind == "c":
            if x.eng == "pe" and d.eng == "pe":
                return
            w = self.waited[x.eng]
            if w.get(d.eng, -1) >= d.idx:
                return
            w[d.eng] = d.idx
            d.needed = True
            x.waits.append(d)
        elif d.kind == "d":
            key = (d.q, d.slot)
            w = self.waited_d[x.eng]
            if w.get(key, 0) >= d.seq:
                return
            w[key] = d.seq
            x.waits.append(d)
        elif d.kind == "cc":
            if self.waited_cc[x.eng] >= d.seq:
                return
            self.waited_cc[x.eng] = d.seq
            x.waits.append(d)

    def _track(self, op, reads, writes):
        for t in reads:
            self._dep(op, self.last_write.get(t))
        for t in writes:
            self._dep(op, self.last_write.get(t))
            for r in self.readers.get(t, ()):
                self._dep(op, r)
        for t in reads:
            self.readers.setdefault(t, []).append(op)
        for t in writes:
            self.last_write[t] = op
            self.readers[t] = []
            if isinstance(t, tuple):
                self.groups.setdefault(t[0], set()).add(t)

    def group(self, prefix):
        return list(self.groups.get(prefix, ()))

    def _append(self, op):
        lst = self.lists[op.eng]
        op.idx = len(lst)
        lst.append(op)

    def op(self, eng, fn, reads=(), writes=()):
        o = Op(eng, fn, "c")
        self._append(o)
        self._track(o, reads, writes)
        return o

    def dma(self, q, fn, reads=(), writes=(), bg=False):
        o = Op(q, fn, "d")
        o.q = q
        if bg:
            j = self.n_slots - 2 + self.rr_bg
            self.rr_bg = (self.rr_bg + 1) % 2
        else:
            j = self.rr[q]
            self.rr[q] = (j + 1) % (self.n_slots - 2 if q == "pool" else self.n_slots)
        prev = self.slots[q][j]
        o.slot = j
        o.seq = (prev.seq + 1) if prev is not None else 1
        self._append(o)
        if prev is not None:
            self._dep(o, prev)
        self.slots[q][j] = o
        self._track(o, reads, writes)
        return o

    def collective(self, fn, reads=(), writes=()):
        o = Op("pool", fn, "cc")
        self.cc_count += 1
        o.seq = self.cc_count
        self._append(o)
        self._track(o, reads, writes)
        return o

    def barrier(self, full=False):
        lasts = []
        for e in ENGS:
            for o in reversed(self.lists[e]):
                if o.kind == "c":
                    lasts.append(o)
                    break
        dl = [o for q in DMAQ if (full or q != "pool") for o in self.slots[q] if o is not None]
        for e in ENGS:
            m = Op(e, None, "m")
            self._append(m)
            for d in lasts:
                if d.eng != e:
                    self._dep(m, d)
            for d in dl:
                self._dep(m, d)
            if self.cc_count:
                cc = Op("pool", None, "cc")
                cc.seq = self.cc_count
                self._dep(m, cc)

    def emit(self):
        nc = self.nc
        for e in ENGS:
            c = 0
            for o in self.lists[e]:
                if o.kind == "c" and o.needed:
                    c += 1
                    o.sigval = c
        with contextlib.ExitStack() as st:
            sem = {e: st.enter_context(nc.semaphore("s_" + e)) for e in ENGS}
            dsem = {q: [st.enter_context(nc.semaphore("d_%s%d" % (q, j))) for j in range(self.n_slots)]
                    for q in DMAQ}
            ccsem = st.enter_context(nc.semaphore("s_cc"))
            block = st.enter_context(nc.Block())
            sched = self

            def run(e, h):
                for o in sched.lists[e]:
                    for d in o.waits:
                        if d.kind == "c":
                            h.wait_ge(sem[d.eng], d.sigval)
                        elif d.kind == "d":
                            h.wait_ge(dsem[d.q][d.slot], 16 * d.seq)
                        else:
                            h.wait_ge(ccsem, d.seq)
                    if o.fn is None:
                        continue
                    ins = o.fn(h)
                    if o.kind == "d":
                        ins.then_inc(dsem[o.q][o.slot], 16)
                    elif o.kind == "cc":
                        ins.then_inc(ccsem)
                    elif o.needed:
                        ins.then_inc(sem[e], 1)

            @block.tensor
            def _(h):
                run("pe", h)

            @block.scalar
            def _(h):
                run("act", h)

            @block.vector
            def _(h):
                run("dve", h)

            @block.gpsimd
            def _(h):
                run("pool", h)

            @block.sync
            def _(h):
                run("sp", h)


def _split3(v):
    v = np.asarray(v, np.float64)
    out = []
    rem = v.copy()
    for _ in range(3):
        a = rem.astype(np.float32).astype(ml_dtypes.bfloat16).astype(np.float64)
        out.append(a.astype(np.float32))
        rem = rem - a
    return out


def _alibi_slopes():
    s = (2.0 ** (-8.0 * (np.arange(12) + 1) / 12)).astype(np.float32).astype(np.float64)
    return s[0::3], s[1::3], s[2::3]


def _win_cols():
    A_Q, A_K, A_V, B_Q, B_K, B_V = 0, 256, 512, 768, 1024, 1280
    C_Q, C_KV, C_PE, D_Q, D_K, D_V = 1536, 1920, 2048, 2080, 2336, 2464
    r = lambda a, n: list(range(a, a + n))
    cols = []
    for h in range(4):
        cols += r(A_Q + 64 * h, 64) + r(A_K + 64 * h, 64)
    for h in range(4):
        cols += r(B_Q + 64 * h, 64) + r(B_K + 64 * h, 64)
    cols += r(D_Q, 256)
    cols += r(D_K, 128)
    cols += r(C_PE, 32)
    cols += r(C_PE + 16, 16) + r(C_PE, 16)
    cols += r(C_Q, 384) + r(C_KV, 128)
    cols += r(A_V, 256) + r(B_V, 256)
    cols += r(D_V, 128)
    assert len(cols) == NCOLS
    return np.array(cols)


CO_A = 0
CO_B = 512
CO_DQ = 1024
CO_DK = 1280
CO_KPE = 1408
CO_KPES = 1440
CO_TA = 1472
CO_TB = 1984
CO_TC = 2496


def _pk(w, ncol):
    k = w.shape[0] // 128
    return np.ascontiguousarray(w.reshape(k, 128, ncol).transpose(1, 0, 2))


def _consts(S):
    masks = np.zeros((7, 128, 512), np.float32)
    j = np.arange(128)[:, None]
    c = np.arange(512)[None, :]
    for v in range(4):
        masks[v] = np.where(c >= 128 * v + j, 0.0, NEG)
    m = c % 128
    masks[4] = np.where(j <= m, 0.0, NEG)
    masks[5] = np.where(j > m, 0.0, NEG)
    masks[6] = np.where(j >= m, 0.0, NEG)
    return masks


def _prep(inputs, S):
    B = inputs["x"].shape[0]
    T = S // 2
    f32 = lambda a: np.ascontiguousarray(np.asarray(a, np.float32))
    sl_a, sl_b, sl_d = _alibi_slopes()
    cols = _win_cols()
    shared = {"ident": np.eye(128, dtype=np.float32), "masks": _consts(S)}
    for l in range(2):
        shared["win%d" % l] = _pk(f32(inputs["w_in"][l])[:, cols], NCOLS)
        wo = f32(inputs["w_out"][l])
        ridx = list(range(1024))
        for rank in range(2):
            for j in range(2):
                hh = HMAP_A[rank][j]
                ridx[rank * 128 + j * 64: rank * 128 + (j + 1) * 64] = list(range(hh * 64, hh * 64 + 64))
        shared["wout%d" % l] = _pk(wo[ridx], 1024)
        wq = f32(inputs["mla_w_uq"][l])
        qc = []
        for h in range(4):
            qc += list(range(h * 96, h * 96 + 96))
        for h in range(4):
            qc += list(range(h * 96, h * 96 + 64)) + list(range(h * 96 + 80, h * 96 + 96)) + list(range(h * 96 + 64, h * 96 + 80))
        shared["wuq%d" % l] = _pk(wq[:, qc], 768)
        wkv = f32(inputs["mla_w_ukv"][l])
        kc_ = []
        for h in range(4):
            kc_ += list(range(h * 128, h * 128 + 64))
        for h in range(4):
            kc_ += list(range(h * 128 + 64, h * 128 + 128))
        shared["wukv%d" % l] = np.ascontiguousarray(wkv[:, kc_])
        g = np.zeros((128, 24), np.float32)
        g[:, 0:8] = f32(inputs["attn_norm"][l]).reshape(8, 128).T
        g[:, 8:16] = f32(inputs["ffn_norm"][l]).reshape(8, 128).T
        g[:, 16:19] = f32(inputs["mla_q_norm"][l]).reshape(3, 128).T
        g[:, 19] = f32(inputs["mla_kv_norm"][l])
        g[:, 20] = np.tile(f32(inputs["diff_subln"][l]), 2)
        shared["gains%d" % l] = g
        shared["dlam%d" % l] = f32(inputs["diff_lambda"][l]).reshape(1, 128)
    shared["fnorm"] = f32(inputs["final_norm"]).reshape(1, 1024)
    w1, w3, w2 = f32(inputs["ffn_w1"][0]), f32(inputs["ffn_w3"][0]), f32(inputs["ffn_w2"][0])

    def pack13(a, b, nfc):
        a = a.reshape(8, 128, nfc, 128).transpose(2, 1, 0, 3)
        b = b.reshape(8, 128, nfc, 128).transpose(2, 1, 0, 3)
        return np.ascontiguousarray(np.concatenate([a, b], axis=3))

    def pack2(a, nfc):
        return np.ascontiguousarray(a.reshape(nfc, 128, 2, 512).transpose(2, 0, 1, 3))

    shared["w13_0"] = pack13(w1, w3, NFC0)
    shared["w2_0"] = pack2(w2, NFC0)
    m1, m3, m2 = inputs["moe_w1"][0], inputs["moe_w3"][0], inputs["moe_w2"][0]
    shared["w13_m"] = np.stack([pack13(f32(m1[e]), f32(m3[e]), NFCE) for e in range(NEXP)])
    shared["w2_m"] = np.stack([pack2(f32(m2[e]), NFCE) for e in range(NEXP)])
    shared["router"] = _pk(f32(inputs["moe_router"][0]), 8)

    pos = np.arange(S, dtype=np.float64)
    inv = (10000.0 ** (-np.arange(0, 32, 2, dtype=np.float32) / 32)).astype(np.float32)
    ang = np.arange(S, dtype=np.float32)[:, None] * inv[None, :]
    cos, sin = np.cos(ang).astype(np.float32), np.sin(ang).astype(np.float32)
    rope_full = np.stack([np.concatenate([cos, cos], 1).T, np.concatenate([-sin, sin], 1).T])

    def aug(slope, scale):
        v = slope * pos / scale
        q = [-a for a in _split3(v)] + [np.ones(S, np.float32)] * 3
        k = [np.ones(S, np.float32)] * 3 + _split3(v)
        return np.stack(q).astype(np.float32), np.stack(k).astype(np.float32)

    in_maps = []
    x = f32(inputs["x"])
    for c in range(2 * B):
        b, r = c // 2, c % 2
        m = dict(shared)
        m["x"] = np.ascontiguousarray(x[b, r * T:(r + 1) * T])
        m["rope"] = np.ascontiguousarray(rope_full[:, :, r * T:(r + 1) * T])
        qa, ka = [], []
        for j in range(2):
            q_, k_ = aug(sl_a[HMAP_A[r][j]], 32 ** -0.5); qa.append(q_); ka.append(k_)
        for j in range(2):
            q_, k_ = aug(sl_b[2 * r + j], 64 ** -0.5); qa.append(q_); ka.append(k_)
        for j in range(2):
            q_, k_ = aug(sl_d[2 * r + j], 64 ** -0.5); qa.append(q_); ka.append(k_)
        m["qaug"] = np.stack(qa)
        m["kaug"] = np.stack(ka)
        for l in range(2):
            m["sinks%d" % l] = f32(inputs["swa_sinks"][l])[2 * r:2 * r + 2].reshape(1, 2)
        in_maps.append(m)
    return in_maps


_USED = []


class Arena:
    def __init__(self, ap, nelem):
        self.ap = ap
        self.n = nelem
        self.off = 0
        self.base = 0

    def alloc(self, shape, dt):
        per = 1
        for d_ in shape[1:]:
            per *= d_
        size = per * (2 if dt == F32 else 1)
        off = self.off + (self.off % 2)
        assert off + size <= self.n, ("arena overflow", off, size, self.n)
        v = self.ap[:, off:off + size]
        if dt == F32:
            v = v.bitcast(F32)
        if len(shape) == 3:
            v = v.rearrange("p (a b) -> p a b", a=shape[1])
        elif len(shape) == 4:
            v = v.rearrange("p (a b c) -> p a b c", a=shape[1], b=shape[2])
        self.off = off + size
        return v

    def persist(self):
        self.base = self.off

    def reset(self):
        self.off = self.base


def build(S, dbg=(), stop_after=None, n_layers=2):
    T = S // 2
    NTA = T // 512
    NQT = S // 512
    NKB = S // 128
    NBT = T // 128
    nc = bass.Bass("TRN2", target_bir_lowering=False)
    RG = [[0, 1], [2, 3], [4, 5], [6, 7]]

    _USED.clear()
    full = (n_layers == 2 and stop_after is None)

    def ein(name, shape, dt=F32):
        if not full and name in ("w13_m", "w2_m", "router"):
            return nc.dram_tensor(name + "_unused", list(shape), dt)
        _USED.append(name)
        return nc.dram_tensor(name, list(shape), dt, kind="ExternalInput")

    def scr(name, shape, dt):
        return nc.dram_tensor(name, list(shape), dt)

    x_in = ein("x", [T, D]).ap()
    ident_d = ein("ident", [128, 128]).ap()
    masks_d = ein("masks", [7, 128, 512]).ap()
    rope_d = ein("rope", [2, 32, T]).ap()
    qaug_d = ein("qaug", [6, 6, S]).ap()
    kaug_d = ein("kaug", [6, 6, S]).ap()
    fnorm_h = ein("fnorm", [1, 1024])
    win_d = [ein("win%d" % l, [128, 8, NCOLS]).ap() for l in range(2)]
    wout_d = [ein("wout%d" % l, [128, 8, 1024]).ap() for l in range(2)]
    wuq_d = [ein("wuq%d" % l, [128, 3, 768]).ap() for l in range(2)]
    wukv_d = [ein("wukv%d" % l, [128, 512]).ap() for l in range(2)]
    gains_d = [ein("gains%d" % l, [128, 24]).ap() for l in range(2)]
    dlam_h = [ein("dlam%d" % l, [1, 128]) for l in range(2)]
    sinks_h = [ein("sinks%d" % l, [1, 2]) for l in range(2)]
    w13_0_d = ein("w13_0", [NFC0, 128, 8, 256]).ap()
    w2_0_d = ein("w2_0", [2, NFC0, 128, 512]).ap()
    w13_m_d = ein("w13_m", [NEXP, NFCE, 128, 8, 256]).ap()
    w2_m_d = ein("w2_m", [NEXP, 2, NFCE, 128, 512]).ap()
    router_d = ein("router", [128, 8, 8]).ap()
    out_d = nc.dram_tensor("out", [T, D], F32, kind="ExternalOutput").ap()

    win_b = [scr("win_b%d" % l, [128, 8, NCOLS], BF16).ap() for l in range(2)]
    wout_b = [scr("wout_b%d" % l, [128, 8, 1024], BF16).ap() for l in range(2)]
    wuq_b = [scr("wuq_b%d" % l, [128, 3, 768], BF16).ap() for l in range(2)]
    wukv_b = [scr("wukv_b%d" % l, [128, 512], BF16).ap() for l in range(2)]
    w13_0_b = scr("w13_0_b", [NFC0, 128, 8, 256], BF16).ap()
    w2_0_b = scr("w2_0_b", [2, NFC0, 128, 512], BF16).ap()
    w13_m_b = scr("w13_m_b", [NEXP, NFCE, 128, 8, 256], BF16).ap()
    w2_m_b = scr("w2_m_b", [NEXP, 2, NFCE, 128, 512], BF16).ap()
    qaug_b = scr("qaug_b", [6, 6, S], BF16).ap()
    kaug_b = scr("kaug_b", [6, 6, S], BF16).ap()
    mine_h = scr("qkv_mine", [2 * GROWS, T], BF16)
    allq_h = scr("qkv_all", [4 * GROWS, T], BF16)
    my_h = scr("qkv_my", [2 * GROWS, T], BF16)
    mine, allq, my = mine_h.ap(), allq_h.ap(), my_h.ap()
    omine = scr("o_mine", [512, S], BF16).ap()
    oall = scr("o_all", [1024, S], BF16).ap()
    omy = scr("o_my", [1024, T], BF16).ap()
    x1 = scr("x1", [T, D], F32).ap()

    dbg_out = {}
    for name, shape, dt in dbg:
        dbg_out[name] = nc.dram_tensor("dbg_" + name, list(shape), dt, kind="ExternalOutput").ap()

    ARENA = 94208
    arena_t = nc.alloc_sbuf_tensor("arena", [128, ARENA], BF16)
    ar = Arena(arena_t.ap(), ARENA)
    ps = [nc.alloc_psum_tensor("ps%d" % i, [128, 512], F32).ap() for i in range(8)]
    psb = [p.bitcast(BF16) for p in ps]

    s = Sched(nc)

    identf = ar.alloc([128, 128], F32)
    identb = ar.alloc([128, 128], BF16)
    masks = ar.alloc([128, 7, 512], F32)
    onesf = ar.alloc([128, 128], F32)
    gains = [ar.alloc([128, 24], F32) for _ in range(2)]
    ar.persist()

    s.dma("sp", lambda h: h.dma_start(out=identf, in_=ident_d), writes=["identf"])
    s.dma("pool", lambda h: h.dma_start(out=identb, in_=ident_d), writes=["identb"])
    s.dma("sp", lambda h: h.dma_start(out=masks, in_=masks_d.rearrange("v p c -> p v c")), writes=["masks"])
    s.op("pool", lambda h: h.memset(onesf, 1.0), writes=["onesf"])
    for l in range(2):
        s.dma("sp", lambda h, l=l: h.dma_start(out=gains[l], in_=gains_d[l]), writes=[("gains", l)])

    def cast(dst, src, tok, bg=False):
        s.dma("pool", lambda h: h.dma_start(out=dst, in_=src), writes=[tok], bg=bg)

    def cast_layer_small(l):
        for kc in range(8):
            cast(win_b[l][:, kc, :], win_d[l][:, kc, :], ("wb_win", l, kc))
        cast(wuq_b[l], wuq_d[l], ("wb_wuq", l))
        cast(wukv_b[l], wukv_d[l], ("wb_wukv", l))
        for kc in range(0, 8, 4):
            cast(wout_b[l][:, kc:kc + 4, :], wout_d[l][:, kc:kc + 4, :], ("wb_wout", l, kc // 4))

    def cast_ffn0():
        for f0 in range(0, NFC0, 4):
            f1 = min(NFC0, f0 + 4)
            cast(w13_0_b[f0:f1], w13_0_d[f0:f1], ("wb_w13_0", f0 // 4), bg=True)
        for hf in range(2):
            for f0 in range(0, NFC0, 8):
                f1 = min(NFC0, f0 + 8)
                cast(w2_0_b[hf, f0:f1], w2_0_d[hf, f0:f1], ("wb_w2_0", hf, f0 // 8), bg=True)

    def cast_moe(e):
        for f0 in range(0, NFCE, 2):
            cast(w13_m_b[e, f0:f0 + 2], w13_m_d[e, f0:f0 + 2], ("wb_w13_m", e, f0 // 2), bg=True)
        for hf in range(2):
            for f0 in range(0, NFCE, 7):
                cast(w2_m_b[e, hf, f0:f0 + 7], w2_m_d[e, hf, f0:f0 + 7], ("wb_w2_m", e, hf, f0 // 7), bg=True)

    cast(qaug_b, qaug_d, "qaug_b")
    cast(kaug_b, kaug_d, "kaug_b")
    cast_layer_small(0)
    cast_ffn0()

    rot = {}

    def nxt(key, n):
        v = rot.get(key, 0)
        rot[key] = (v + 1) % n
        return v

    def mm_group(out, parts, tok):
        n = len(parts)
        for i, (l_, r_, rd) in enumerate(parts):
            s.op("pe", lambda h, l_=l_, r_=r_, i=i: h.matmul(out, l_, r_, start=(i == 0), stop=(i == n - 1)),
                 reads=list(rd), writes=[tok] if i in (0, n - 1) else [])

    def dump(name, src, reads):
        if name in dbg_out:
            s.dma("sp", lambda h: h.dma_start(out=dbg_out[name], in_=src), reads=reads)

    def rms_rstd(ss_ap, out_ap, n, toks_in, tok_out):
        s.op("act", lambda h: h.activation(out=out_ap, in_=ss_ap, func=AF.Sqrt, scale=1.0 / n, bias=EPS),
             reads=toks_in, writes=[tok_out])
        s.op("dve", lambda h: h.reciprocal(out=out_ap, in_=out_ap), reads=[tok_out], writes=[tok_out])

    def phase_A(l, xsrc):
        s.barrier()
        ar.reset()
        win = ar.alloc([128, 8, NCOLS], BF16)
        wuq = ar.alloc([128, 3, 768], BF16)
        wukv = ar.alloc([128, 512], BF16)
        xt = [ar.alloc([128, 4, 1024], F32) for _ in range(2)]
        xn = ar.alloc([128, 4, 1024], BF16)
        junk = ar.alloc([128, 1024], BF16)
        hT = [ar.alloc([128, 8, 512], BF16) for _ in range(2)]
        ss4 = ar.alloc([128, 4], F32)
        rs4 = ar.alloc([128, 4], F32)
        ssm = ar.alloc([128, 4, 2], F32)
        rsm = ar.alloc([128, 4, 2], F32)
        cn = [ar.alloc([128, 512], BF16) for _ in range(2)]
        cT = ar.alloc([128, 4, 512], BF16)
        fmst = [ar.alloc([128, 512], BF16) for _ in range(4)]
        vst = ar.alloc([128, 4, 896], BF16)
        ropeK = ar.alloc([128, 2, 512], F32)
        ropeQ = ar.alloc([128, 2, 512], F32)
        rtmp = [ar.alloc([128, 512], F32) for _ in range(2)]
        G = gains[l]
        for kc in range(8):
            s.dma("sp", lambda h, kc=kc: h.dma_start(out=win[:, kc, :], in_=win_b[l][:, kc, :]),
                  reads=[("wb_win", l, kc)], writes=[("win", kc)])
        s.dma("sp", lambda h: h.dma_start(out=wuq, in_=wuq_b[l]), reads=[("wb_wuq", l)], writes=["wuq"])
        s.dma("sp", lambda h: h.dma_start(out=wukv, in_=wukv_b[l]), reads=[("wb_wukv", l)], writes=["wukv"])
        winr = [("win", kc) for kc in range(8)]

        def load_x(t):
            b = t % 2
            s.dma("sp", lambda h: h.dma_start(out=xt[b], in_=xsrc[t * 512:(t + 1) * 512, :].rearrange("(b p) d -> p b d", p=128)),
                  reads=[("xsrc", t)], writes=[("xt", b)])

        def store_fm(src_ap, nrows, g, row, t, rd):
            dst = mine[g * GROWS + row: g * GROWS + row + nrows, t * 512:(t + 1) * 512]
            s.dma("act", lambda h: h.dma_start(out=dst, in_=src_ap), reads=rd, writes=[("mine", g, row, t)])

        def evac(bank_ap, dst_ap, rd, wr, k=[0]):
            k[0] += 1
            if k[0] % 2:
                s.op("act", lambda h: h.copy(out=dst_ap, in_=bank_ap), reads=rd, writes=wr)
            else:
                s.op("dve", lambda h: h.tensor_copy(out=dst_ap, in_=bank_ap), reads=rd, writes=wr)

        def tileA(t):
            b = t % 2
            X = xt[b]
            s.dma("sp", lambda h, t=t: h.dma_start(out=ropeK[0:32], in_=rope_d[:, :, t * 512:(t + 1) * 512].rearrange("a r c -> r a c")),
                  writes=["ropeK"])
            s.dma("sp", lambda h, t=t: h.dma_start(out=ropeQ[64:96], in_=rope_d[:, :, t * 512:(t + 1) * 512].rearrange("a r c -> r a c")),
                  writes=["ropeQ"])
            s.op("dve", lambda h: h.memset(ss4, 0.0), writes=[("ss4", i) for i in range(4)])
            for blk in range(4):
                s.op("act", lambda h, blk=blk: h.activation(out=junk, in_=X[:, blk, :], func=AF.Square, accum_out=ss4[:, blk:blk + 1]),
                     reads=[("xt", b), ("ss4", blk)], writes=[("ss4", blk)])
            rms_rstd(ss4, rs4, D, [("ss4", i) for i in range(4)], "rs4")
            for blk in range(4):
                s.op("dve", lambda h, blk=blk: h.tensor_scalar(out=xn[:, blk, :], in0=X[:, blk, :], scalar1=rs4[:, blk:blk + 1],
                                                              scalar2=None, op0=ALU.mult),
                     reads=[("xt", b), "rs4"], writes=[("xn", blk)])
            for blk in range(4):
                tb = nxt("TB", 2)
                for kc in range(8):
                    s.op("pe", lambda h, blk=blk, kc=kc, tb=tb: h.transpose(out=psb[tb][:, kc * 128:(kc + 1) * 128],
                                                                            in_=xn[:, blk, kc * 128:(kc + 1) * 128], identity=identb),
                         reads=[("xn", blk), "identb"], writes=[("ps", tb)] if kc in (0, 7) else [])
                s.op("dve", lambda h, blk=blk, tb=tb: h.tensor_tensor(
                    out=hT[b][:, :, blk * 128:(blk + 1) * 128], in0=psb[tb].rearrange("p (k t) -> p k t", k=8),
                    in1=G[:, 0:8].unsqueeze(2).broadcast_to([128, 8, 128]), op=ALU.mult),
                    reads=[("ps", tb), ("gains", l)], writes=[("hT", b, blk)])
            hTr = [("hT", b, i) for i in range(4)]

            def fm_group(co, M):
                bank = 2 + nxt("FM", 3)
                mm_group(ps[bank][0:M, :], [(win[:, kc, co:co + M], hT[b][:, kc, :], winr[kc:kc + 1] + hTr) for kc in range(8)],
                         ("ps", bank))
                return bank

            for hh in range(4):
                bank = fm_group(CO_A + hh * 128, 128)
                st = nxt("fmst", 4)
                evac(ps[bank], fmst[st], [("ps", bank)], [("fmst", st)])
                store_fm(fmst[st], 128, INV_A[hh][0], R_A + INV_A[hh][1] * 128, t, [("fmst", st)])
            for hh in range(4):
                bank = fm_group(CO_B + hh * 128, 128)
                st = nxt("fmst", 4)
                evac(ps[bank], fmst[st], [("ps", bank)], [("fmst", st)])
                store_fm(fmst[st], 128, hh // 2, R_B + (hh % 2) * 128, t, [("fmst", st)])
            for g in range(2):
                bank = fm_group(CO_DQ + g * 128, 128)
                st = nxt("fmst", 4)
                evac(ps[bank], fmst[st], [("ps", bank)], [("fmst", st)])
                store_fm(fmst[st], 128, g, R_DQ, t, [("fmst", st)])
            bank = fm_group(CO_DK, 128)
            st = nxt("fmst", 4)
            evac(ps[bank], fmst[st], [("ps", bank)], [("fmst", st)])
            for g in range(2):
                store_fm(fmst[st][g * 64:(g + 1) * 64], 64, g, R_DK, t, [("fmst", st)])
            b1 = fm_group(CO_KPE, 32)
            b2 = fm_group(CO_KPES, 32)
            s.op("dve", lambda h, b1=b1: h.tensor_tensor(out=rtmp[0][0:32], in0=ps[b1][0:32], in1=ropeK[0:32, 0, :], op=ALU.mult),
                 reads=[("ps", b1), "ropeK"], writes=["rtmp0"])
            s.op("dve", lambda h, b2=b2: h.tensor_tensor(out=rtmp[1][0:32], in0=ps[b2][0:32], in1=ropeK[0:32, 1, :], op=ALU.mult),
                 reads=[("ps", b2), "ropeK"], writes=["rtmp1"])
            st = nxt("fmst", 4)
            s.op("dve", lambda h, st=st: h.tensor_tensor(out=fmst[st][0:32], in0=rtmp[0][0:32], in1=rtmp[1][0:32], op=ALU.add),
                 reads=["rtmp0", "rtmp1"], writes=[("fmst", st)])
            for g in range(2):
                store_fm(fmst[st][0:32], 32, g, R_KPE, t, [("fmst", st)])
            for blk in range(4):
                def tm_group(co, N):
                    bank = 5 + nxt("TM", 3)
                    mm_group(ps[bank][:, 0:N], [(hT[b][:, kc, blk * 128:(blk + 1) * 128], win[:, kc, co:co + N],
                                                 winr[kc:kc + 1] + [("hT", b, blk)]) for kc in range(8)], ("ps", bank))
                    return bank
                bk = tm_group(CO_TB, 512)
                evac(ps[bk], vst[:, blk, 0:512], [("ps", bk)], [("vst", blk, 0)])
                bk = tm_group(CO_TC, 128)
                evac(ps[bk][:, 0:128], vst[:, blk, 768:896], [("ps", bk)], [("vst", blk, 2)])
                bk = tm_group(CO_TA, 512)
                s.op("dve", lambda h, blk=blk: h.memset(ssm[:, blk, :], 0.0), writes=[("ssm", blk)])
                s.op("act", lambda h, blk=blk, bk=bk: h.activation(out=junk[:, 0:384], in_=ps[bk][:, 0:384], func=AF.Square,
                                                                  accum_out=ssm[:, blk, 0:1]),
                     reads=[("ps", bk), ("ssm", blk)], writes=[("ssm", blk)])
                s.op("act", lambda h, blk=blk, bk=bk: h.activation(out=junk[:, 384:512], in_=ps[bk][:, 384:512], func=AF.Square,
                                                                  accum_out=ssm[:, blk, 1:2]),
                     reads=[("ps", bk), ("ssm", blk)], writes=[("ssm", blk)])
                s.op("act", lambda h, blk=blk: h.activation(out=rsm[:, blk, 0:1], in_=ssm[:, blk, 0:1], func=AF.Sqrt, scale=1.0 / 384, bias=EPS),
                     reads=[("ssm", blk)], writes=[("rsm", blk)])
                s.op("act", lambda h, blk=blk: h.activation(out=rsm[:, blk, 1:2], in_=ssm[:, blk, 1:2], func=AF.Sqrt, scale=1.0 / 128, bias=EPS),
                     reads=[("ssm", blk)], writes=[("rsm", blk)])
                s.op("dve", lambda h, blk=blk: h.reciprocal(out=rsm[:, blk, :], in_=rsm[:, blk, :]), reads=[("rsm", blk)], writes=[("rsm", blk)])
                ci = nxt("cn", 2)
                s.op("dve", lambda h, blk=blk, bk=bk, ci=ci: h.tensor_scalar(out=cn[ci][:, 0:384], in0=ps[bk][:, 0:384], scalar1=rsm[:, blk, 0:1],
                                                                            scalar2=None, op0=ALU.mult),
                     reads=[("ps", bk), ("rsm", blk)], writes=[("cn", ci)])
                s.op("dve", lambda h, blk=blk, bk=bk, ci=ci: h.tensor_scalar(out=cn[ci][:, 384:512], in0=ps[bk][:, 384:512], scalar1=rsm[:, blk, 1:2],
                                                                            scalar2=None, op0=ALU.mult),
                     reads=[("ps", bk), ("rsm", blk)], writes=[("cn", ci)])
                tb = nxt("TB", 2)
                for i in range(4):
                    s.op("pe", lambda h, i=i, tb=tb, ci=ci: h.transpose(out=psb[tb][:, i * 128:(i + 1) * 128], in_=cn[ci][:, i * 128:(i + 1) * 128],
                                                                       identity=identb),
                         reads=[("cn", ci), "identb"], writes=[("ps", tb)] if i in (0, 3) else [])
                s.op("dve", lambda h, blk=blk, tb=tb: h.tensor_tensor(
                    out=cT[:, :, blk * 128:(blk + 1) * 128], in0=psb[tb][:, 0:512].rearrange("p (k t) -> p k t", k=4),
                    in1=G[:, 16:20].unsqueeze(2).broadcast_to([128, 4, 128]), op=ALU.mult),
                    reads=[("ps", tb), ("gains", l)], writes=[("cT", blk)])
                bank = 5 + nxt("TM", 3)
                mm_group(ps[bank][:, 0:256], [(cT[:, 3, blk * 128:(blk + 1) * 128], wukv[:, 256:512], ["wukv", ("cT", blk)])], ("ps", bank))
                evac(ps[bank][:, 0:256], vst[:, blk, 512:768], [("ps", bank)], [("vst", blk, 1)])
            cTr = [("cT", i) for i in range(4)]
            vr = [("vst", i, k) for i in range(4) for k in range(3)]
            for g in range(2):
                for name, slot in VSLOT.items():
                    if name[0] == "D":
                        col = 768 + g * 64
                    elif name[0] == "A":
                        col = HMAP_A[g][int(name[1])] * 64
                    else:
                        col = {"A": 0, "B": 256, "C": 512}[name[0]] + (2 * g + int(name[1])) * 64
                    dst = bass.AP(tensor=mine_h, offset=(g * GROWS + FMROWS + slot * 64) * T + t * 512 * 64,
                                  ap=[[64, 128], [128 * 64, 4], [1, 64]])
                    s.dma("act", lambda h, dst=dst, col=col: h.dma_start(out=dst, in_=vst[:, :, col:col + 64]),
                          reads=vr, writes=[("mine", g, "v", slot, t)])
            for hh in range(4):
                bo = 2 + nxt("FM", 3)
                mm_group(ps[bo][0:96, :], [(wuq[:, kc, hh * 96:(hh + 1) * 96], cT[:, kc, :], ["wuq"] + cTr) for kc in range(3)], ("ps", bo))
                bs = 2 + nxt("FM", 3)
                mm_group(ps[bs][0:96, :], [(wuq[:, kc, 384 + hh * 96:384 + (hh + 1) * 96], cT[:, kc, :], ["wuq"] + cTr) for kc in range(3)], ("ps", bs))
                st = nxt("fmst", 4)
                s.op("act", lambda h, bo=bo, st=st: h.copy(out=fmst[st][0:64], in_=ps[bo][0:64]), reads=[("ps", bo)], writes=[("fmst", st)])
                s.op("dve", lambda h, bo=bo: h.tensor_tensor(out=rtmp[0][64:96], in0=ps[bo][64:96], in1=ropeQ[64:96, 0, :], op=ALU.mult),
                     reads=[("ps", bo), "ropeQ"], writes=["rtmp0"])
                s.op("dve", lambda h, bs=bs: h.tensor_tensor(out=rtmp[1][64:96], in0=ps[bs][64:96], in1=ropeQ[64:96, 1, :], op=ALU.mult),
                     reads=[("ps", bs), "ropeQ"], writes=["rtmp1"])
                s.op("dve", lambda h, st=st: h.tensor_tensor(out=fmst[st][64:96], in0=rtmp[0][64:96], in1=rtmp[1][64:96], op=ALU.add),
                     reads=["rtmp0", "rtmp1", ("fmst", st)], writes=[("fmst", st)])
                store_fm(fmst[st][0:96], 96, hh // 2, R_CQ + (hh % 2) * 96, t, [("fmst", st)])
            for g in range(2):
                bank = 2 + nxt("FM", 3)
                mm_group(ps[bank][:, :], [(wukv[:, g * 128:(g + 1) * 128], cT[:, 3, :], ["wukv"] + cTr)], ("ps", bank))
                st = nxt("fmst", 4)
                evac(ps[bank], fmst[st], [("ps", bank)], [("fmst", st)])
                store_fm(fmst[st], 128, g, R_CK, t, [("fmst", st)])

        load_x(0)
        for t in range(NTA):
            if t + 1 < NTA:
                load_x(t + 1)
            tileA(t)

    rank_cache = {}

    def rank_of(h):
        if "r" not in rank_cache:
            rank_cache["r"] = h.partition_id() % 2
        return rank_cache["r"]

    CR = GROWS // 8
    NCG = 8

    def exchange1():
        toks = []
        for k in range(2 * NCG):
            g_, kk = k // NCG, k % NCG
            rows = slice(g_ * GROWS + kk * CR, g_ * GROWS + (kk + 1) * CR)
            mt = [t_ for t_ in s.group("mine")]
            s.collective(lambda h, k=k, rows=rows: h.collective_compute(
                "AllGather", ALU.bypass, replica_groups=RG, ins=[mine[rows, :]], outs=[allq[k * 2 * CR:(k + 1) * 2 * CR, :]]),
                reads=mt, writes=[("allq", k)])
            toks.append(("allq", k))
        for hf in range(2):
            def f(h, hf=hf):
                r = rank_of(h)
                src = allq[bass.ds(r * (NCG * 2 * CR), NCG * 2 * CR), :].rearrange("(k two c) t -> k two c t", two=2, c=CR)[:, hf]
                dst = my[hf * GROWS:(hf + 1) * GROWS, :].rearrange("(k c) t -> k c t", c=CR)
                return h.dma_start(out=dst, in_=src)
            s.dma("pool", f, reads=toks, writes=[("my", hf)])

    def exchange2():
        toks = []
        for k in range(4):
            s.collective(lambda h, k=k: h.collective_compute(
                "AllGather", ALU.bypass, replica_groups=RG, ins=[omine[k * 128:(k + 1) * 128, :]], outs=[oall[k * 256:(k + 1) * 256, :]]),
                reads=s.group("omine"), writes=[("oall", k)])
            toks.append(("oall", k))
        for half in range(2):
            def f(h, half=half):
                r = rank_of(h)
                return h.dma_start(out=omy[half * 512:(half + 1) * 512, :], in_=oall[half * 512:(half + 1) * 512, bass.ds(r * T, T)])
            s.dma("pool", f, reads=toks, writes=[("omy", half)])

    def phase_B(l):
        s.barrier()
        ar.reset()
        lam_init = 0.8 - 0.6 * float(np.exp(-0.3 * l))
        Kt = [ar.alloc([128, S], BF16) for _ in range(2)]
        Vsb = ar.alloc([128, NKB, 65], BF16)
        Qt = [[ar.alloc([128, 512], BF16) for _ in range(2)] for _ in range(2)]
        P = [ar.alloc([128, 512], BF16) for _ in range(4)]
        Ssb = [ar.alloc([128, 512], F32) for _ in range(2)]
        rec = ar.alloc([128, 512], F32)
        bcs = ar.alloc([128, 512], F32)
        onr = [ar.alloc([128, 512], F32) for _ in range(2)]
        df = ar.alloc([128, 512], F32)
        sq = ar.alloc([128, 512], F32)
        rsb = ar.alloc([128, 512], F32)
        ost = [ar.alloc([128, 512], BF16) for _ in range(2)]
        acc = ar.alloc([128, S], F32)
        dl = ar.alloc([128, 128], F32)
        sm = ar.alloc([128, 16], F32)
        G = gains[l]
        myr = s.group("my")
        HQ = NQT // 2

        s.dma("sp", lambda h: h.dma_start(out=dl, in_=bass.AP(tensor=dlam_h[l], offset=0, ap=[[0, 128], [1, 128]])), writes=["dl"])
        s.dma("sp", lambda h: h.dma_start(out=sm[:, 8:10], in_=bass.AP(tensor=sinks_h[l], offset=0, ap=[[0, 128], [1, 2]])), writes=["sink"])
        s.op("dve", lambda h: h.tensor_tensor(out=dl[:, 0:32], in0=dl[:, 0:32], in1=dl[:, 32:64], op=ALU.mult), reads=["dl"], writes=["dl"])
        s.op("dve", lambda h: h.tensor_tensor(out=dl[:, 64:96], in0=dl[:, 64:96], in1=dl[:, 96:128], op=ALU.mult), reads=["dl"], writes=["dl"])
        s.op("dve", lambda h: h.reduce_sum(out=sm[:, 0:1], in_=dl[:, 0:32], axis=mybir.AxisListType.X), reads=["dl"], writes=["sm"])
        s.op("dve", lambda h: h.reduce_sum(out=sm[:, 1:2], in_=dl[:, 64:96], axis=mybir.AxisListType.X), reads=["dl", "sm"], writes=["sm"])
        s.op("act", lambda h: h.activation(out=sm[:, 2:4], in_=sm[:, 0:2], func=AF.Exp), reads=["sm"], writes=["sm"])
        s.op("dve", lambda h: h.tensor_tensor(out=sm[:, 4:5], in0=sm[:, 3:4], in1=sm[:, 2:3], op=ALU.subtract), reads=["sm"], writes=["sm"])
        s.op("dve", lambda h: h.tensor_scalar(out=sm[:, 4:5], in0=sm[:, 4:5], scalar1=-lam_init, scalar2=None, op0=ALU.add), reads=["sm"], writes=["sm"])
        s.op("dve", lambda h: h.tensor_scalar(out=sm[:, 5:6], in0=G[:, 20:21], scalar1=1.0 - lam_init, scalar2=None, op0=ALU.mult),
             reads=["sm", ("gains", l)], writes=["sm"])
        s.op("act", lambda h: h.activation(out=sm[:, 10:12], in_=sm[:, 8:10], func=AF.Exp), reads=["sink", "sm"], writes=["sm"])
        neglam = sm[:, 4:5]
        gainA = sm[:, 5:6]
        s.op("dve", lambda h: h.memset(Vsb[:, :, 64:65], 1.0), writes=["Vones"])

        def load_rows(dst_tile, prow, nrows, row, tok_fn, cols=None):
            for hf in range(2):
                s.dma("sp", lambda h, hf=hf: h.dma_start(out=dst_tile[prow:prow + nrows, hf * T:(hf + 1) * T],
                                                        in_=my[hf * GROWS + row: hf * GROWS + row + nrows, :]),
                      reads=myr, writes=[tok_fn(hf)])

        def load_V(slot, d):
            for hf in range(2):
                c16 = [("Vsb", k) for k in range(hf * NBT // 16, (hf + 1) * NBT // 16)]
                if d == 1:
                    src = bass.AP(tensor=my_h, offset=(hf * GROWS + FMROWS + slot * 64) * T, ap=[[64, 128], [128 * 64, NBT], [1, 64]])
                    s.dma("sp", lambda h, hf=hf, src=src: h.dma_start(out=Vsb[:, hf * NBT:(hf + 1) * NBT, 0:64], in_=src),
                          reads=myr, writes=c16)
                else:
                    ncb = T // (128 * d)
                    for c in range(ncb):
                        src = bass.AP(tensor=my_h, offset=(hf * GROWS + FMROWS + slot * 64) * T + c * 128 * d * 64,
                                      ap=[[d * 64, 128], [64, d], [1, 64]])
                        b0 = hf * NBT + c * d
                        s.dma("sp", lambda h, src=src, b0=b0: h.dma_start(out=Vsb[:, b0:b0 + d, 0:64], in_=src),
                              reads=myr, writes=[("Vsb", b0 // 16)])

        def finalize_norm(src65, qt_cols_tok, extra_den=None):
            if extra_den is not None:
                s.op("dve", lambda h: h.tensor_scalar(out=rec[64:65, :], in0=src65[64:65, :], scalar1=extra_den, scalar2=None, op0=ALU.add),
                     reads=qt_cols_tok + ["sm"], writes=["rec"])
                s.op("dve", lambda h: h.reciprocal(out=rec[64:65, :], in_=rec[64:65, :]), reads=["rec"], writes=["rec"])
            else:
                s.op("dve", lambda h: h.reciprocal(out=rec[64:65, :], in_=src65[64:65, :]), reads=qt_cols_tok, writes=["rec"])
            s.op("pe", lambda h: h.matmul(ps[7][0:64, :], onesf[64:65, 0:64], rec[64:65, :], start=True, stop=True),
                 reads=["rec", "onesf"], writes=[("ps", 7)])
            s.op("act", lambda h: h.copy(out=bcs[0:64], in_=ps[7][0:64]), reads=[("ps", 7)], writes=["bcs"])

        def store_o(st, mixer, j, qt):
            dst = omine[mixer * 128 + j * 64: mixer * 128 + j * 64 + 64, qt * 512:(qt + 1) * 512]
            s.dma("act", lambda h: h.dma_start(out=dst, in_=ost[st][0:64]), reads=[("ost", st)], writes=[("omine", mixer, j, qt)])

        def dense_head(kind, j):
            if kind == "A":
                maps, dk, dd = 2, 96, 32
                scale = 32 ** -0.5
                rq = [R_A + j * 128, R_A + j * 128 + 32]
                rk = [R_A + j * 128 + 64, R_A + j * 128 + 96]
                vslot, mixer = VSLOT["A%d" % j], 0
            else:
                maps, dk, dd = 1, 96, 96
                scale = 96 ** -0.5
                rq = [R_CQ + j * 96]
                rk = [R_CK + j * 64]
                vslot, mixer = VSLOT["C%d" % j], 2
            for m in range(maps):
                if kind == "A":
                    for p0 in (32, 64):
                        s.op("dve", lambda h, m=m, p0=p0: h.memset(Kt[m][p0:p0 + 32, :], 0.0), writes=[("Kt", m, "aug")])
                        for b_ in range(2):
                            s.op("dve", lambda h, m=m, b_=b_, p0=p0: h.memset(Qt[b_][m][p0:p0 + 32, :], 0.0), writes=[("Qa", b_, m)])
                    load_rows(Kt[m], 0, 32, rk[m], lambda hf, m=m: ("Kt", m, hf))
                    s.dma("sp", lambda h, m=m: h.dma_start(out=Kt[m][32:38, :], in_=kaug_b[j]), reads=["kaug_b"], writes=[("Kt", m, "aug")])
                else:
                    load_rows(Kt[m], 0, 64, rk[m], lambda hf, m=m: ("Kt", m, hf))
                    load_rows(Kt[m], 64, 32, R_KPE, lambda hf, m=m: ("Kt", m, "aug"))
            load_V(vslot, 1)
            def qtile(qt, hook):
                b = qt % 2
                hfq, lq = qt // HQ, qt % HQ
                nr = 32 if kind == "A" else 96
                for m in range(maps):
                    s.dma("sp", lambda h, m=m: h.dma_start(out=Qt[b][m][0:nr, :],
                                                           in_=my[hfq * GROWS + rq[m]: hfq * GROWS + rq[m] + nr, lq * 512:(lq + 1) * 512]),
                          reads=myr, writes=[("Qt", b, m)])
                    if kind == "A":
                        s.dma("sp", lambda h, m=m: h.dma_start(out=Qt[b][m][32:38, :], in_=qaug_b[j][:, qt * 512:(qt + 1) * 512]),
                              reads=["qaug_b"], writes=[("Qa", b, m)])
                kb_lo = 0
                if kind == "A":
                    sl_min = min(float(_alibi_slopes()[0][HMAP_A[0][j]]), float(_alibi_slopes()[0][HMAP_A[1][j]]))
                    kb_lo = max(0, int(np.ceil((qt * 512 - 127 - ALIBI_TH / sl_min) / 128.0)))
                kbs = list(range(kb_lo, 4 * qt + 4))
                units = [(kb, m) for kb in kbs for m in range(maps)]
                ob = [3 + 2 * (qt % 2) + m for m in range(maps)]
                pend = []

                def issue_S(kb, m):
                    bank = nxt("SB", 3)
                    hfk = kb // NBT
                    s.op("pe", lambda h: h.matmul(ps[bank][:, :], Kt[m][0:dk, kb * 128:(kb + 1) * 128], Qt[b][m][0:dk, :], start=True, stop=True),
                         reads=[("Kt", m, hfk), ("Kt", m, "aug"), ("Qt", b, m), ("Qa", b, m)], writes=[("ps", bank)])
                    pi = nxt("P", 4)
                    v = kb - 4 * qt
                    if v >= 0:
                        si = nxt("Ssb", 2)
                        s.op("dve", lambda h: h.tensor_tensor(out=Ssb[si], in0=ps[bank], in1=masks[:, v, :], op=ALU.add),
                             reads=[("ps", bank), "masks"], writes=[("Ssb", si)])
                        s.op("act", lambda h: h.activation(out=P[pi], in_=Ssb[si], func=AF.Exp, scale=scale), reads=[("Ssb", si)], writes=[("P", pi)])
                    else:
                        s.op("act", lambda h: h.activation(out=P[pi], in_=ps[bank], func=AF.Exp, scale=scale), reads=[("ps", bank)], writes=[("P", pi)])
                    return pi

                def issue_PV(kb, m, pi):
                    first, last = kb == kbs[0], kb == kbs[-1]
                    s.op("pe", lambda h: h.matmul(ps[ob[m]][0:65, :], Vsb[:, kb, 0:65], P[pi], start=first, stop=last),
                         reads=[("Vsb", kb // 16), "Vones", ("P", pi)], writes=[("ps", ob[m])] if (first or last) else [])

                for ui, (kb, m) in enumerate(units):
                    pi = issue_S(kb, m)
                    pend.append((kb, m, pi))
                    if len(pend) > 2:
                        issue_PV(*pend.pop(0))
                    if ui == 3 and hook is not None:
                        hook()
                        hook = None
                while pend:
                    issue_PV(*pend.pop(0))
                if hook is not None:
                    hook()
                return lambda: fin(qt, ob)

            def fin(qt, ob):
                st = nxt("ost", 2)
                if kind == "C":
                    finalize_norm(ps[ob[0]], [("ps", ob[0])])
                    s.op("dve", lambda h: h.tensor_tensor(out=ost[st][0:64], in0=ps[ob[0]][0:64], in1=bcs[0:64], op=ALU.mult),
                         reads=[("ps", ob[0]), "bcs"], writes=[("ost", st)])
                else:
                    for m in range(2):
                        finalize_norm(ps[ob[m]], [("ps", ob[m])])
                        s.op("dve", lambda h, m=m: h.tensor_tensor(out=onr[m][0:64], in0=ps[ob[m]][0:64], in1=bcs[0:64], op=ALU.mult),
                             reads=[("ps", ob[m]), "bcs"], writes=[("onr", m)])
                    s.op("dve", lambda h: h.scalar_tensor_tensor(out=df[0:64], in0=onr[1][0:64], scalar=neglam[0:64], in1=onr[0][0:64],
                                                                 op0=ALU.mult, op1=ALU.add),
                         reads=[("onr", 0), ("onr", 1), "sm"], writes=["df"])
                    s.op("act", lambda h: h.activation(out=sq[0:64], in_=df[0:64], func=AF.Square), reads=["df"], writes=["sq"])
                    s.op("pe", lambda h: h.matmul(ps[7][0:64, :], onesf[0:64, 0:64], sq[0:64, :], start=True, stop=True),
                         reads=["sq", "onesf"], writes=[("ps", 7)])
                    s.op("act", lambda h: h.activation(out=rsb[0:64], in_=ps[7][0:64], func=AF.Sqrt, scale=1.0 / 64, bias=EPS),
                         reads=[("ps", 7)], writes=["rsb"])
                    s.op("dve", lambda h: h.reciprocal(out=rsb[0:64], in_=rsb[0:64]), reads=["rsb"], writes=["rsb"])
                    s.op("dve", lambda h: h.scalar_tensor_tensor(out=ost[st][0:64], in0=df[0:64], scalar=gainA[0:64], in1=rsb[0:64],
                                                                 op0=ALU.mult, op1=ALU.mult),
                         reads=["df", "rsb", "sm"], writes=[("ost", st)])
                store_o(st, mixer, j, qt)

            prev = None
            for qt in range(NQT):
                prev = qtile(qt, prev)
            prev()

        def banded_head(kind, j):
            scale = 64 ** -0.5
            if kind == "B":
                rowq, rowk = R_B + j * 128, R_B + j * 128 + 64
                augslot, vslot, mixer = 2 + j, VSLOT["B%d" % j], 1
                patterns = [(1, 6), (4, 6), (16, 6)]
            else:
                rowq, rowk = R_DQ + j * 64, R_DK
                augslot, vslot, mixer = 4 + j, VSLOT["D"], 3
                patterns = [(1, 5)]
            Kf, Qf = Kt[0], Kt[1]
            s.op("dve", lambda h: h.memset(Kf[64:96, :], 0.0), writes=[("Kt", 0, "aug")])
            s.op("dve", lambda h: h.memset(Qf[64:96, :], 0.0), writes=[("Kt", 1, "aug")])
            load_rows(Kf, 0, 64, rowk, lambda hf: ("Kt", 0, hf))
            s.dma("sp", lambda h: h.dma_start(out=Kf[64:70, :], in_=kaug_b[augslot]), reads=["kaug_b"], writes=[("Kt", 0, "aug")])
            load_rows(Qf, 0, 64, rowq, lambda hf: ("Kt", 1, hf))
            s.dma("sp", lambda h: h.dma_start(out=Qf[64:70, :], in_=qaug_b[augslot]), reads=["qaug_b"], writes=[("Kt", 1, "aug")])
            kq_r = [("Kt", m, x) for m in range(2) for x in (0, 1, "aug")]
            def group4(pidx, d, mprev, g4):
                if True:
                    idxs = [g4 * 4 + i for i in range(4)]
                    cr = [divmod(ix, d) for ix in idxs]
                    base = [128 * d * c + r for (c, r) in cr]
                    has_prev = [c >= 1 for (c, r) in cr]
                    bo = nxt("SB", 3)
                    for i in range(4):
                        sl = slice(base[i], base[i] + 127 * d + 1, d)
                        s.op("pe", lambda h, i=i, sl=sl: h.matmul(ps[bo][:, i * 128:(i + 1) * 128], Kf[0:96, sl], Qf[0:96, sl], start=True, stop=True),
                             reads=kq_r, writes=[("ps", bo)] if i in (0, 3) else [])
                    so = nxt("Ssb", 2)
                    s.op("dve", lambda h: h.tensor_tensor(out=Ssb[so], in0=ps[bo], in1=masks[:, 4, :], op=ALU.add),
                         reads=[("ps", bo), "masks"], writes=[("Ssb", so)])
                    po = nxt("P", 4)
                    s.op("act", lambda h: h.activation(out=P[po], in_=Ssb[so], func=AF.Exp, scale=scale), reads=[("Ssb", so)], writes=[("P", po)])
                    pp = None
                    if any(has_prev):
                        bp = nxt("SB", 3)
                        ii = [i for i in range(4) if has_prev[i]]
                        for i in ii:
                            slq = slice(base[i], base[i] + 127 * d + 1, d)
                            slk = slice(base[i] - 128 * d, base[i] - d + 1, d)
                            s.op("pe", lambda h, i=i, slq=slq, slk=slk: h.matmul(ps[bp][:, i * 128:(i + 1) * 128], Kf[0:96, slk], Qf[0:96, slq],
                                                                                start=True, stop=True),
                                 reads=kq_r, writes=[("ps", bp)] if i in (ii[0], ii[-1]) else [])
                        sp_ = nxt("Ssb", 2)
                        s.op("dve", lambda h: h.tensor_tensor(out=Ssb[sp_], in0=ps[bp], in1=masks[:, mprev, :], op=ALU.add),
                             reads=[("ps", bp), "masks"], writes=[("Ssb", sp_)])
                        pp = nxt("P", 4)
                        s.op("act", lambda h: h.activation(out=P[pp], in_=Ssb[sp_], func=AF.Exp, scale=scale), reads=[("Ssb", sp_)], writes=[("P", pp)])
                    obk = 3 + nxt("OB", 4)
                    nmm = []
                    for i in range(4):
                        if has_prev[i]:
                            nmm.append((i, idxs[i] - d, pp, True, False))
                        nmm.append((i, idxs[i], po, not has_prev[i], True))
                    for k_, (i, vb, pt, st_, sp2) in enumerate(nmm):
                        s.op("pe", lambda h, i=i, vb=vb, pt=pt, st_=st_, sp2=sp2: h.matmul(
                            ps[obk][0:65, i * 128:(i + 1) * 128], Vsb[:, vb, 0:65], P[pt][:, i * 128:(i + 1) * 128], start=st_, stop=sp2),
                            reads=[("Vsb", vb // 16), "Vones", ("P", pt)], writes=[("ps", obk)] if k_ in (0, len(nmm) - 1) else [])
                    for i in range(4):
                        sl = slice(base[i], base[i] + 127 * d + 1, d)
                        tl = sorted(set([base[i] // 512, (base[i] + 128 * d - 1) // 512]))
                        toks = [("acc", q_) for q_ in range(tl[0], tl[-1] + 1)]
                        if pidx == 0:
                            s.op("act", lambda h, i=i, sl=sl: h.copy(out=acc[0:65, sl], in_=ps[obk][0:65, i * 128:(i + 1) * 128]),
                                 reads=[("ps", obk)], writes=toks)
                        else:
                            s.op("dve", lambda h, i=i, sl=sl: h.tensor_tensor(out=acc[0:65, sl], in0=ps[obk][0:65, i * 128:(i + 1) * 128],
                                                                              in1=acc[0:65, sl], op=ALU.add),
                                 reads=[("ps", obk)] + toks, writes=toks)
            for pidx, (d, mprev) in enumerate(patterns):
                load_V(vslot, d)
                for g4 in range(NKB // 4):
                    group4(pidx, d, mprev, g4)
            for qt in range(NQT):
                cols = slice(qt * 512, (qt + 1) * 512)
                finalize_norm(acc[0:65, cols], [("acc", qt)], extra_den=(sm[64:65, 10 + j:11 + j] if kind == "D" else None))
                st = nxt("ost", 2)
                s.op("dve", lambda h, cols=cols, st=st: h.tensor_tensor(out=ost[st][0:64], in0=acc[0:64, cols], in1=bcs[0:64], op=ALU.mult),
                     reads=[("acc", qt), "bcs"], writes=[("ost", st)])
                store_o(st, mixer, j, qt)

        for j in range(2):
            banded_head("D", j)
        for j in range(2):
            banded_head("B", j)
        for j in range(2):
            dense_head("C", j)
        for j in range(2):
            dense_head("A", j)

    def phase_C(l, xsrc, xdst, final):
        s.barrier()
        ar.reset()
        moe = (l % 2 == 1)
        NFC = NFCE if moe else NFC0
        nexp = NEXP if moe else 1
        wout = ar.alloc([128, 8, 1024], BF16)
        xs = [ar.alloc([128, 4, 1024], F32) for _ in range(2)]
        oT = [ar.alloc([128, 8, 512], BF16) for _ in range(2)]
        xn = ar.alloc([128, 4, 1024], F32 if moe else BF16)
        junk = ar.alloc([128, 1024], BF16)
        h2T = ar.alloc([128, 8, 512], BF16)
        h2Tf = ar.alloc([128, 8, 512], F32) if moe else None
        actT = ar.alloc([128, NFC, 512], BF16)
        silt = [ar.alloc([128, 512], BF16) for _ in range(2)]
        w13 = [ar.alloc([128, 8, 256], BF16) for _ in range(3)]
        w2 = [ar.alloc([128, 4, 512], BF16) for _ in range(3)]
        ss4 = ar.alloc([128, 4], F32)
        rs4 = ar.alloc([128, 4], F32)
        fn = ar.alloc([128, 1024], F32) if final else None
        if moe:
            rt = ar.alloc([128, 8, 8], F32)
            lg = ar.alloc([128, 4, 8], F32)
            gt = ar.alloc([128, 4, 8], F32)
            t8 = [ar.alloc([128, 8], F32) for _ in range(3)]
            m4 = ar.alloc([128, 8], F32)
        G = gains[l]
        omr = s.group("omy")
        for kc in range(0, 8, 4):
            s.dma("sp", lambda h, kc=kc: h.dma_start(out=wout[:, kc:kc + 4, :], in_=wout_b[l][:, kc:kc + 4, :]),
                  reads=[("wb_wout", l, kc // 4)], writes=[("wout", kc // 4)])
        if moe:
            s.dma("sp", lambda h: h.dma_start(out=rt, in_=router_d), writes=["rt"])
        if final:
            s.dma("sp", lambda h: h.dma_start(out=fn, in_=bass.AP(tensor=fnorm_h, offset=0, ap=[[0, 128], [1, 1024]])), writes=["fn"])

        def load_tile(t):
            b = t % 2
            s.dma("sp", lambda h: h.dma_start(out=xs[b], in_=xsrc[t * 512:(t + 1) * 512, :].rearrange("(b p) d -> p b d", p=128)),
                  writes=[("xs", b, i) for i in range(4)])
            for kc in range(0, 8, 4):
                s.dma("sp", lambda h, kc=kc: h.dma_start(out=oT[b][:, kc:kc + 4, :],
                                                        in_=omy[kc * 128:(kc + 4) * 128, t * 512:(t + 1) * 512].rearrange("(k p) c -> p k c", p=128)),
                      reads=omr, writes=[("oT", b, kc // 4)])

        def w13_src(e, fc):
            return (w13_m_b[e, fc], ("wb_w13_m", e, fc // 2)) if moe else (w13_0_b[fc], ("wb_w13_0", fc // 4))

        def w2_src(e, hf, f0):
            if moe:
                return w2_m_b[e, hf, f0:f0 + 4].rearrange("f p c -> p f c"), ("wb_w2_m", e, hf, f0 // 7), ("wb_w2_m", e, hf, (f0 + 3) // 7)
            f1 = min(NFC, f0 + 4)
            return w2_0_b[hf, f0:f1].rearrange("f p c -> p f c"), ("wb_w2_0", hf, f0 // 8), ("wb_w2_0", hf, (f1 - 1) // 8)

        def tileC(t):
            b = t % 2
            X = xs[b]
            for blk in range(4):
                for hf in range(2):
                    bank = nxt("CB", 4)
                    mm_group(ps[bank][:, :], [(oT[b][:, kc, blk * 128:(blk + 1) * 128], wout[:, kc, hf * 512:(hf + 1) * 512],
                                               [("oT", b, kc // 4), ("wout", kc // 4)]) for kc in range(8)], ("ps", bank))
                    s.op("dve", lambda h, blk=blk, hf=hf, bank=bank: h.tensor_tensor(
                        out=X[:, blk, hf * 512:(hf + 1) * 512], in0=ps[bank], in1=X[:, blk, hf * 512:(hf + 1) * 512], op=ALU.add),
                        reads=[("ps", bank), ("xs", b, blk)], writes=[("xs", b, blk)])
            s.op("dve", lambda h: h.memset(ss4, 0.0), writes=[("ss4", i) for i in range(4)])
            for blk in range(4):
                s.op("act", lambda h, blk=blk: h.activation(out=junk, in_=X[:, blk, :], func=AF.Square, accum_out=ss4[:, blk:blk + 1]),
                     reads=[("xs", b, blk), ("ss4", blk)], writes=[("ss4", blk)])
            rms_rstd(ss4, rs4, D, [("ss4", i) for i in range(4)], "rs4")
            for blk in range(4):
                s.op("dve", lambda h, blk=blk: h.tensor_scalar(out=xn[:, blk, :], in0=X[:, blk, :], scalar1=rs4[:, blk:blk + 1],
                                                              scalar2=None, op0=ALU.mult),
                     reads=[("xs", b, blk), "rs4"], writes=[("xn", blk)])
            for blk in range(4):
                if not moe:
                    tb = nxt("CB", 4)
                    for kc in range(8):
                        s.op("pe", lambda h, blk=blk, kc=kc, tb=tb: h.transpose(out=psb[tb][:, kc * 128:(kc + 1) * 128],
                                                                                in_=xn[:, blk, kc * 128:(kc + 1) * 128], identity=identb),
                             reads=[("xn", blk), "identb"], writes=[("ps", tb)] if kc in (0, 7) else [])
                    s.op("dve", lambda h, blk=blk, tb=tb: h.tensor_tensor(
                        out=h2T[:, :, blk * 128:(blk + 1) * 128], in0=psb[tb].rearrange("p (k t) -> p k t", k=8),
                        in1=G[:, 8:16].unsqueeze(2).broadcast_to([128, 8, 128]), op=ALU.mult),
                        reads=[("ps", tb), ("gains", l)], writes=[("h2T", blk)])
                else:
                    for q in range(2):
                        tb = nxt("CB", 4)
                        for k4 in range(4):
                            kc = q * 4 + k4
                            s.op("pe", lambda h, blk=blk, kc=kc, k4=k4, tb=tb: h.transpose(out=ps[tb][:, k4 * 128:(k4 + 1) * 128],
                                                                                          in_=xn[:, blk, kc * 128:(kc + 1) * 128], identity=identf),
                                 reads=[("xn", blk), "identf"], writes=[("ps", tb)] if k4 in (0, 3) else [])
                        s.op("dve", lambda h, blk=blk, q=q, tb=tb: h.tensor_tensor(
                            out=h2Tf[:, q * 4:q * 4 + 4, blk * 128:(blk + 1) * 128], in0=ps[tb].rearrange("p (k t) -> p k t", k=4),
                            in1=G[:, 8 + q * 4:12 + q * 4].unsqueeze(2).broadcast_to([128, 4, 128]), op=ALU.mult),
                            reads=[("ps", tb), ("gains", l)], writes=[("h2Tf", blk, q)])
                        s.op("pool", lambda h, blk=blk, q=q: h.tensor_copy(out=h2T[:, q * 4:q * 4 + 4, blk * 128:(blk + 1) * 128],
                                                                          in_=h2Tf[:, q * 4:q * 4 + 4, blk * 128:(blk + 1) * 128]),
                             reads=[("h2Tf", blk, q)], writes=[("h2T", blk)])
                    bank = nxt("CB", 4)
                    mm_group(ps[bank][:, 0:8], [(h2Tf[:, kc, blk * 128:(blk + 1) * 128], rt[:, kc, :], [("h2Tf", blk, kc // 4), "rt"])
                                                for kc in range(8)], ("ps", bank))
                    L = lg[:, blk, :]
                    s.op("act", lambda h, L=L, bank=bank: h.copy(out=L, in_=ps[bank][:, 0:8]), reads=[("ps", bank)], writes=["lg"])
                    s.op("dve", lambda h, L=L: h.reduce_max(out=m4[:, 0:1], in_=L, axis=mybir.AxisListType.X), reads=["lg"], writes=["m4"])
                    s.op("dve", lambda h, L=L: h.tensor_scalar(out=t8[0], in0=L, scalar1=m4[:, 0:1], scalar2=None, op0=ALU.is_equal),
                         reads=["lg", "m4"], writes=["t80"])
                    s.op("dve", lambda h, L=L: h.scalar_tensor_tensor(out=t8[1], in0=t8[0], scalar=NEG, in1=L, op0=ALU.mult, op1=ALU.add),
                         reads=["t80", "lg"], writes=["t81"])
                    s.op("dve", lambda h: h.reduce_max(out=m4[:, 1:2], in_=t8[1], axis=mybir.AxisListType.X), reads=["t81", "m4"], writes=["m4"])
                    s.op("dve", lambda h: h.tensor_scalar(out=t8[2], in0=t8[1], scalar1=m4[:, 1:2], scalar2=None, op0=ALU.is_equal),
                         reads=["t81", "m4"], writes=["t82"])
                    s.op("dve", lambda h: h.tensor_tensor(out=m4[:, 2:3], in0=m4[:, 0:1], in1=m4[:, 1:2], op=ALU.subtract), reads=["m4"], writes=["m4"])
                    s.op("act", lambda h: h.activation(out=m4[:, 3:4], in_=m4[:, 2:3], func=AF.Sigmoid), reads=["m4"], writes=["m4"])
                    s.op("act", lambda h: h.activation(out=m4[:, 4:5], in_=m4[:, 2:3], func=AF.Sigmoid, scale=-1.0), reads=["m4"], writes=["m4"])
                    s.op("dve", lambda h: h.tensor_scalar(out=t8[0], in0=t8[0], scalar1=m4[:, 3:4], scalar2=None, op0=ALU.mult),
                         reads=["t80", "m4"], writes=["t80"])
                    s.op("dve", lambda h, blk=blk: h.scalar_tensor_tensor(out=gt[:, blk, :], in0=t8[2], scalar=m4[:, 4:5], in1=t8[0],
                                                                         op0=ALU.mult, op1=ALU.add),
                         reads=["t80", "t82", "m4"], writes=[("gt", blk)])
            h2r = [("h2T", i) for i in range(4)]
            for e in range(nexp):
                for fc in range(NFC):
                    wi = nxt("w13", 3)
                    src, tokw = w13_src(e, fc)
                    s.dma("sp", lambda h, wi=wi, src=src: h.dma_start(out=w13[wi], in_=src), reads=[tokw], writes=[("w13", wi)])
                    b1 = nxt("CB", 4)
                    mm_group(ps[b1][:, :], [(w13[wi][:, kc, 0:128], h2T[:, kc, :], [("w13", wi)] + h2r) for kc in range(8)], ("ps", b1))
                    b3 = nxt("CB", 4)
                    mm_group(ps[b3][:, :], [(w13[wi][:, kc, 128:256], h2T[:, kc, :], [("w13", wi)] + h2r) for kc in range(8)], ("ps", b3))
                    si = nxt("silt", 2)
                    s.op("act", lambda h, b1=b1, si=si: h.activation(out=silt[si], in_=ps[b1], func=AF.Silu), reads=[("ps", b1)], writes=[("silt", si)])
                    s.op("dve", lambda h, b3=b3, si=si, fc=fc: h.tensor_tensor(out=actT[:, fc, :], in0=ps[b3], in1=silt[si], op=ALU.mult),
                         reads=[("ps", b3), ("silt", si)], writes=[("actT", fc)])
                for hf in range(2):
                    for f0 in range(0, NFC, 4):
                        f1 = min(NFC, f0 + 4)
                        wi = nxt("w2", 3)
                        src, tk0, tk1 = w2_src(e, hf, f0)
                        s.dma("sp", lambda h, wi=wi, src=src, n=f1 - f0: h.dma_start(out=w2[wi][:, 0:n, :], in_=src),
                              reads=[tk0, tk1], writes=[("w2", wi)])
                        for fc in range(f0, f1):
                            for blk in range(4):
                                s.op("pe", lambda h, fc=fc, blk=blk, wi=wi, f0=f0: h.matmul(
                                    ps[4 + blk][:, :], actT[:, fc, blk * 128:(blk + 1) * 128], w2[wi][:, fc - f0, :],
                                    start=(fc == 0), stop=(fc == NFC - 1)),
                                    reads=[("actT", fc), ("w2", wi)], writes=[("ps", 4 + blk)] if fc in (0, NFC - 1) else [])
                    for blk in range(4):
                        if moe:
                            s.op("dve", lambda h, blk=blk, hf=hf, e=e: h.scalar_tensor_tensor(
                                out=X[:, blk, hf * 512:(hf + 1) * 512], in0=ps[4 + blk], scalar=gt[:, blk, e:e + 1],
                                in1=X[:, blk, hf * 512:(hf + 1) * 512], op0=ALU.mult, op1=ALU.add),
                                reads=[("ps", 4 + blk), ("gt", blk), ("xs", b, blk)], writes=[("xs", b, blk)])
                        else:
                            s.op("dve", lambda h, blk=blk, hf=hf: h.tensor_tensor(
                                out=X[:, blk, hf * 512:(hf + 1) * 512], in0=ps[4 + blk], in1=X[:, blk, hf * 512:(hf + 1) * 512], op=ALU.add),
                                reads=[("ps", 4 + blk), ("xs", b, blk)], writes=[("xs", b, blk)])
            xr = [("xs", b, i) for i in range(4)]
            if final:
                s.op("dve", lambda h: h.memset(ss4, 0.0), writes=[("ss4", i) for i in range(4)])
                for blk in range(4):
                    s.op("act", lambda h, blk=blk: h.activation(out=junk, in_=X[:, blk, :], func=AF.Square, accum_out=ss4[:, blk:blk + 1]),
                         reads=[("xs", b, blk), ("ss4", blk)], writes=[("ss4", blk)])
                rms_rstd(ss4, rs4, D, [("ss4", i) for i in range(4)], "rs4")
                for blk in range(4):
                    s.op("dve", lambda h, blk=blk: h.scalar_tensor_tensor(out=X[:, blk, :], in0=X[:, blk, :], scalar=rs4[:, blk:blk + 1], in1=fn,
                                                                         op0=ALU.mult, op1=ALU.mult),
                         reads=[("xs", b, blk), "rs4", "fn"], writes=[("xs", b, blk)])
            s.dma("act", lambda h: h.dma_start(out=xdst[t * 512:(t + 1) * 512, :].rearrange("(b p) d -> p b d", p=128), in_=X),
                  reads=xr, writes=[("xdst", t)])

        load_tile(0)
        for t in range(NTA):
            if t + 1 < NTA:
                load_tile(t + 1)
            tileC(t)

    for l in range(n_layers):
        xsrc = x_in if l == 0 else x1
        phase_A(l, xsrc)
        dump("mine%d" % l, mine, s.group("mine"))
        if stop_after == ("A", l):
            break
        exchange1()
        if l == 0 and n_layers == 2:
            cast_layer_small(1)
        if l == 1:
            for e in range(NEXP // 2, NEXP):
                cast_moe(e)
        dump("my%d" % l, my, s.group("my"))
        phase_B(l)
        dump("omine%d" % l, omine, s.group("omine"))
        if stop_after == ("B", l):
            break
        exchange2()
        if l == 0 and n_layers == 2:
            for e in range(NEXP // 2):
                cast_moe(e)
        final = (l == n_layers - 1)
        phase_C(l, xsrc, out_d if final else x1, final)
    s.barrier(full=True)
    s.emit()
    return nc


def run(inputs, S, dbg=(), stop_after=None, n_layers=2):
    import time
    t0 = time.time()
    in_maps = _prep(inputs, S)
    t1 = time.time()
    nc = build(S, dbg=dbg, stop_after=stop_after, n_layers=n_layers)
    t2 = time.time()
    if n_layers < 2 or stop_after is not None:
        names = set(_USED)
        in_maps = [{k: v for k, v in m.items() if k in names} for m in in_maps]
    res = run_bass_kernel_spmd(nc, in_maps, core_ids=list(range(len(in_maps))))
    print("prep %.1fs build %.1fs run %.1fs" % (t1 - t0, t2 - t1, time.time() - t2), flush=True)
    return res


def kernel(**inputs):
    x = np.asarray(inputs["x"])
    B, S, _ = x.shape
    res = run(inputs, S)
    T = S // 2
    out = np.empty((B, S, D), np.float32)
    for c in range(2 * B):
        out[c // 2, (c % 2) * T:(c % 2 + 1) * T] = res.results[c]["out"]
    return out
```

```python
import contextlib
import numpy as np
import ml_dtypes
import concourse.bass as bass
import concourse.mybir as mybir
from concourse.bass_utils import run_bass_kernel_spmd

F32 = mybir.dt.float32
BF16 = mybir.dt.bfloat16
AF = mybir.ActivationFunctionType
ALU = mybir.AluOpType

ENGS = ("pe", "act", "dve", "pool", "sp")
DMAQ = ("sp", "pool", "act")

D = 1024
KC = 8
EPS = 1e-6
NEG = -1e30
D_FF = 2816
NFC0 = 22
D_FFE = 3584
NFCE = 28
NEXP = 8
NCOLS = 2624
GROWS = 1504
FMROWS = 1056
R_A = 0
R_B = 256
R_CQ = 512
R_CK = 704
R_KPE = 832
R_DQ = 864
R_DK = 992
VSLOT = {"A0": 0, "A1": 1, "B0": 2, "B1": 3, "C0": 4, "C1": 5, "D": 6}
HMAP_A = [[0, 3], [1, 2]]
INV_A = {0: (0, 0), 3: (0, 1), 1: (1, 0), 2: (1, 1)}
ALIBI_TH = 80.0


class Op:
    __slots__ = ("eng", "fn", "waits", "needed", "idx", "sigval", "kind", "slot", "seq", "q")

    def __init__(self, eng, fn, kind):
        self.eng = eng
        self.fn = fn
        self.kind = kind
        self.waits = []
        self.needed = False
        self.idx = -1
        self.sigval = 0
        self.slot = -1
        self.seq = 0
        self.q = None


class Sched:
    def __init__(self, nc, n_slots=8):
        self.nc = nc
        self.n_slots = n_slots
        self.lists = {e: [] for e in ENGS}
        self.last_write = {}
        self.readers = {}
        self.waited = {e: {} for e in ENGS}
        self.waited_d = {e: {} for e in ENGS}
        self.slots = {q: [None] * n_slots for q in DMAQ}
        self.rr = {q: 0 for q in DMAQ}
        self.rr_bg = 0
        self.cc_count = 0
        self.waited_cc = {e: 0 for e in ENGS}
        self.groups = {}

    def _dep(self, x, d):
        if d is None or d is x:
            return
        if d.kind == "c":
            if x.eng == "pe" and d.eng == "pe":
                return
            w = self.waited[x.eng]
            if w.get(d.eng, -1) >= d.idx:
                return
            w[d.eng] = d.idx
            d.needed = True
            x.waits.append(d)
        elif d.kind == "d":
            key = (d.q, d.slot)
            w = self.waited_d[x.eng]
            if w.get(key, 0) >= d.seq:
                return
            w[key] = d.seq
            x.waits.append(d)
        elif d.kind == "cc":
            if self.waited_cc[x.eng] >= d.seq:
                return
            self.waited_cc[x.eng] = d.seq
            x.waits.append(d)

    def _track(self, op, reads, writes):
        for t in reads:
            self._dep(op, self.last_write.get(t))
        for t in writes:
            self._dep(op, self.last_write.get(t))
            for r in self.readers.get(t, ()):
                self._dep(op, r)
        for t in reads:
            self.readers.setdefault(t, []).append(op)
        for t in writes:
            self.last_write[t] = op
            self.readers[t] = []
            if isinstance(t, tuple):
                self.groups.setdefault(t[0], set()).add(t)

    def group(self, prefix):
        return list(self.groups.get(prefix, ()))

    def _append(self, op):
        lst = self.lists[op.eng]
        op.idx = len(lst)
        lst.append(op)

    def op(self, eng, fn, reads=(), writes=()):
        o = Op(eng, fn, "c")
        self._append(o)
        self._track(o, reads, writes)
        return o

    def dma(self, q, fn, reads=(), writes=(), bg=False):
        o = Op(q, fn, "d")
        o.q = q
        if bg:
            j = self.n_slots - 2 + self.rr_bg
            self.rr_bg = (self.rr_bg + 1) % 2
        else:
            j = self.rr[q]
            self.rr[q] = (j + 1) % (self.n_slots - 2 if q == "pool" else self.n_slots)
        prev = self.slots[q][j]
        o.slot = j
        o.seq = (prev.seq + 1) if prev is not None else 1
        self._append(o)
        if prev is not None:
            self._dep(o, prev)
        self.slots[q][j] = o
        self._track(o, reads, writes)
        return o

    def collective(self, fn, reads=(), writes=()):
        o = Op("pool", fn, "cc")
        self.cc_count += 1
        o.seq = self.cc_count
        self._append(o)
        self._track(o, reads, writes)
        return o

    def barrier(self, full=False):
        lasts = []
        for e in ENGS:
            for o in reversed(self.lists[e]):
                if o.kind == "c":
                    lasts.append(o)
                    break
        dl = [o for q in DMAQ if (full or q != "pool") for o in self.slots[q] if o is not None]
        for e in ENGS:
            m = Op(e, None, "m")
            self._append(m)
            for d in lasts:
                if d.eng != e:
                    self._dep(m, d)
            for d in dl:
                self._dep(m, d)
            if self.cc_count:
                cc = Op("pool", None, "cc")
                cc.seq = self.cc_count
                self._dep(m, cc)

    def emit(self):
        nc = self.nc
        for e in ENGS:
            c = 0
            for o in self.lists[e]:
                if o.kind == "c" and o.needed:
                    c += 1
                    o.sigval = c
        with contextlib.ExitStack() as st:
            sem = {e: st.enter_context(nc.semaphore("s_" + e)) for e in ENGS}
            dsem = {q: [st.enter_context(nc.semaphore("d_%s%d" % (q, j))) for j in range(self.n_slots)]
                    for q in DMAQ}
            ccsem = st.enter_context(nc.semaphore("s_cc"))
            block = st.enter_context(nc.Block())
            sched = self

            def run(e, h):
                for o in sched.lists[e]:
                    for d in o.waits:
                        if d.kind == "c":
                            h.wait_ge(sem[d.eng], d.sigval)
                        elif d.kind == "d":
                            h.wait_ge(dsem[d.q][d.slot], 16 * d.seq)
                        else:
                            h.wait_ge(ccsem, d.seq)
                    if o.fn is None:
                        continue
                    ins = o.fn(h)
                    if o.kind == "d":
                        ins.then_inc(dsem[o.q][o.slot], 16)
                    elif o.kind == "cc":
                        ins.then_inc(ccsem)
                    elif o.needed:
                        ins.then_inc(sem[e], 1)

            @block.tensor
            def _(h):
                run("pe", h)

            @block.scalar
            def _(h):
                run("act", h)

            @block.vector
            def _(h):
                run("dve", h)

            @block.gpsimd
            def _(h):
                run("pool", h)

            @block.sync
            def _(h):
                run("sp", h)


def _split3(v):
    v = np.asarray(v, np.float64)
    out = []
    rem = v.copy()
    for _ in range(3):
        a = rem.astype(np.float32).astype(ml_dtypes.bfloat16).astype(np.float64)
        out.append(a.astype(np.float32))
        rem = rem - a
    return out


def _alibi_slopes():
    s = (2.0 ** (-8.0 * (np.arange(12) + 1) / 12)).astype(np.float32).astype(np.float64)
    return s[0::3], s[1::3], s[2::3]


def _win_cols():
    A_Q, A_K, A_V, B_Q, B_K, B_V = 0, 256, 512, 768, 1024, 1280
    C_Q, C_KV, C_PE, D_Q, D_K, D_V = 1536, 1920, 2048, 2080, 2336, 2464
    r = lambda a, n: list(range(a, a + n))
    cols = []
    for h in range(4):
        cols += r(A_Q + 64 * h, 64) + r(A_K + 64 * h, 64)
    for h in range(4):
        cols += r(B_Q + 64 * h, 64) + r(B_K + 64 * h, 64)
    cols += r(D_Q, 256)
    cols += r(D_K, 128)
    cols += r(C_PE, 32)
    cols += r(C_PE + 16, 16) + r(C_PE, 16)
    cols += r(C_Q, 384) + r(C_KV, 128)
    cols += r(A_V, 256) + r(B_V, 256)
    cols += r(D_V, 128)
    assert len(cols) == NCOLS
    return np.array(cols)


CO_A = 0
CO_B = 512
CO_DQ = 1024
CO_DK = 1280
CO_KPE = 1408
CO_KPES = 1440
CO_TA = 1472
CO_TB = 1984
CO_TC = 2496


def _pk(w, ncol):
    k = w.shape[0] // 128
    return np.ascontiguousarray(w.reshape(k, 128, ncol).transpose(1, 0, 2))


def _consts(S):
    masks = np.zeros((7, 128, 512), np.float32)
    j = np.arange(128)[:, None]
    c = np.arange(512)[None, :]
    for v in range(4):
        masks[v] = np.where(c >= 128 * v + j, 0.0, NEG)
    m = c % 128
    masks[4] = np.where(j <= m, 0.0, NEG)
    masks[5] = np.where(j > m, 0.0, NEG)
    masks[6] = np.where(j >= m, 0.0, NEG)
    return masks


def _prep(inputs, S):
    B = inputs["x"].shape[0]
    T = S // 2
    f32 = lambda a: np.ascontiguousarray(np.asarray(a, np.float32))
    sl_a, sl_b, sl_d = _alibi_slopes()
    cols = _win_cols()
    shared = {"ident": np.eye(128, dtype=np.float32), "masks": _consts(S)}
    for l in range(2):
        shared["win%d" % l] = _pk(f32(inputs["w_in"][l])[:, cols], NCOLS)
        wo = f32(inputs["w_out"][l])
        ridx = list(range(1024))
        for rank in range(2):
            for j in range(2):
                hh = HMAP_A[rank][j]
                ridx[rank * 128 + j * 64: rank * 128 + (j + 1) * 64] = list(range(hh * 64, hh * 64 + 64))
        shared["wout%d" % l] = _pk(wo[ridx], 1024)
        wq = f32(inputs["mla_w_uq"][l])
        qc = []
        for h in range(4):
            qc += list(range(h * 96, h * 96 + 96))
        for h in range(4):
            qc += list(range(h * 96, h * 96 + 64)) + list(range(h * 96 + 80, h * 96 + 96)) + list(range(h * 96 + 64, h * 96 + 80))
        shared["wuq%d" % l] = _pk(wq[:, qc], 768)
        wkv = f32(inputs["mla_w_ukv"][l])
        kc_ = []
        for h in range(4):
            kc_ += list(range(h * 128, h * 128 + 64))
        for h in range(4):
            kc_ += list(range(h * 128 + 64, h * 128 + 128))
        shared["wukv%d" % l] = np.ascontiguousarray(wkv[:, kc_])
        g = np.zeros((128, 24), np.float32)
        g[:, 0:8] = f32(inputs["attn_norm"][l]).reshape(8, 128).T
        g[:, 8:16] = f32(inputs["ffn_norm"][l]).reshape(8, 128).T
        g[:, 16:19] = f32(inputs["mla_q_norm"][l]).reshape(3, 128).T
        g[:, 19] = f32(inputs["mla_kv_norm"][l])
        g[:, 20] = np.tile(f32(inputs["diff_subln"][l]), 2)
        shared["gains%d" % l] = g
        shared["dlam%d" % l] = f32(inputs["diff_lambda"][l]).reshape(1, 128)
    shared["fnorm"] = f32(inputs["final_norm"]).reshape(1, 1024)
    w1, w3, w2 = f32(inputs["ffn_w1"][0]), f32(inputs["ffn_w3"][0]), f32(inputs["ffn_w2"][0])

    def pack13(a, b, nfc):
        a = a.reshape(8, 128, nfc, 128).transpose(2, 1, 0, 3)
        b = b.reshape(8, 128, nfc, 128).transpose(2, 1, 0, 3)
        return np.ascontiguousarray(np.concatenate([a, b], axis=3))

    def pack2(a, nfc):
        return np.ascontiguousarray(a.reshape(nfc, 128, 2, 512).transpose(2, 0, 1, 3))

    shared["w13_0"] = pack13(w1, w3, NFC0)
    shared["w2_0"] = pack2(w2, NFC0)
    m1, m3, m2 = inputs["moe_w1"][0], inputs["moe_w3"][0], inputs["moe_w2"][0]
    shared["w13_m"] = np.stack([pack13(f32(m1[e]), f32(m3[e]), NFCE) for e in range(NEXP)])
    shared["w2_m"] = np.stack([pack2(f32(m2[e]), NFCE) for e in range(NEXP)])
    shared["router"] = _pk(f32(inputs["moe_router"][0]), 8)

    pos = np.arange(S, dtype=np.float64)
    inv = (10000.0 ** (-np.arange(0, 32, 2, dtype=np.float32) / 32)).astype(np.float32)
    ang = np.arange(S, dtype=np.float32)[:, None] * inv[None, :]
    cos, sin = np.cos(ang).astype(np.float32), np.sin(ang).astype(np.float32)
    rope_full = np.stack([np.concatenate([cos, cos], 1).T, np.concatenate([-sin, sin], 1).T])

    def aug(slope, scale):
        v = slope * pos / scale
        q = [-a for a in _split3(v)] + [np.ones(S, np.float32)] * 3
        k = [np.ones(S, np.float32)] * 3 + _split3(v)
        return np.stack(q).astype(np.float32), np.stack(k).astype(np.float32)

    in_maps = []
    x = f32(inputs["x"])
    for c in range(2 * B):
        b, r = c // 2, c % 2
        m = dict(shared)
        m["x"] = np.ascontiguousarray(x[b, r * T:(r + 1) * T])
        m["rope"] = np.ascontiguousarray(rope_full[:, :, r * T:(r + 1) * T])
        qa, ka = [], []
        for j in range(2):
            q_, k_ = aug(sl_a[HMAP_A[r][j]], 32 ** -0.5); qa.append(q_); ka.append(k_)
        for j in range(2):
            q_, k_ = aug(sl_b[2 * r + j], 64 ** -0.5); qa.append(q_); ka.append(k_)
        for j in range(2):
            q_, k_ = aug(sl_d[2 * r + j], 64 ** -0.5); qa.append(q_); ka.append(k_)
        m["qaug"] = np.stack(qa)
        m["kaug"] = np.stack(ka)
        for l in range(2):
            m["sinks%d" % l] = f32(inputs["swa_sinks"][l])[2 * r:2 * r + 2].reshape(1, 2)
        in_maps.append(m)
    return in_maps


_USED = []


class Arena:
    def __init__(self, ap, nelem):
        self.ap = ap
        self.n = nelem
        self.off = 0
        self.base = 0

    def alloc(self, shape, dt):
        per = 1
        for d_ in shape[1:]:
            per *= d_
        size = per * (2 if dt == F32 else 1)
        off = self.off + (self.off % 2)
        assert off + size <= self.n, ("arena overflow", off, size, self.n)
        v = self.ap[:, off:off + size]
        if dt == F32:
            v = v.bitcast(F32)
        if len(shape) == 3:
            v = v.rearrange("p (a b) -> p a b", a=shape[1])
        elif len(shape) == 4:
            v = v.rearrange("p (a b c) -> p a b c", a=shape[1], b=shape[2])
        self.off = off + size
        return v

    def persist(self):
        self.base = self.off

    def reset(self):
        self.off = self.base


def build(S, dbg=(), stop_after=None, n_layers=2):
    T = S // 2
    NTA = T // 512
    NQT = S // 512
    NKB = S // 128
    NBT = T // 128
    nc = bass.Bass("TRN2", target_bir_lowering=False)
    RG = [[0, 1], [2, 3], [4, 5], [6, 7]]

    _USED.clear()
    full = (n_layers == 2 and stop_after is None)

    def ein(name, shape, dt=F32):
        if not full and name in ("w13_m", "w2_m", "router"):
            return nc.dram_tensor(name + "_unused", list(shape), dt)
        _USED.append(name)
        return nc.dram_tensor(name, list(shape), dt, kind="ExternalInput")

    def scr(name, shape, dt):
        return nc.dram_tensor(name, list(shape), dt)

    x_in = ein("x", [T, D]).ap()
    ident_d = ein("ident", [128, 128]).ap()
    masks_d = ein("masks", [7, 128, 512]).ap()
    rope_d = ein("rope", [2, 32, T]).ap()
    qaug_d = ein("qaug", [6, 6, S]).ap()
    kaug_d = ein("kaug", [6, 6, S]).ap()
    fnorm_h = ein("fnorm", [1, 1024])
    win_d = [ein("win%d" % l, [128, 8, NCOLS]).ap() for l in range(2)]
    wout_d = [ein("wout%d" % l, [128, 8, 1024]).ap() for l in range(2)]
    wuq_d = [ein("wuq%d" % l, [128, 3, 768]).ap() for l in range(2)]
    wukv_d = [ein("wukv%d" % l, [128, 512]).ap() for l in range(2)]
    gains_d = [ein("gains%d" % l, [128, 24]).ap() for l in range(2)]
    dlam_h = [ein("dlam%d" % l, [1, 128]) for l in range(2)]
    sinks_h = [ein("sinks%d" % l, [1, 2]) for l in range(2)]
    w13_0_d = ein("w13_0", [NFC0, 128, 8, 256]).ap()
    w2_0_d = ein("w2_0", [2, NFC0, 128, 512]).ap()
    w13_m_d = ein("w13_m", [NEXP, NFCE, 128, 8, 256]).ap()
    w2_m_d = ein("w2_m", [NEXP, 2, NFCE, 128, 512]).ap()
    router_d = ein("router", [128, 8, 8]).ap()
    out_d = nc.dram_tensor("out", [T, D], F32, kind="ExternalOutput").ap()

    win_b = [scr("win_b%d" % l, [128, 8, NCOLS], BF16).ap() for l in range(2)]
    wout_b = [scr("wout_b%d" % l, [128, 8, 1024], BF16).ap() for l in range(2)]
    wuq_b = [scr("wuq_b%d" % l, [128, 3, 768], BF16).ap() for l in range(2)]
    wukv_b = [scr("wukv_b%d" % l, [128, 512], BF16).ap() for l in range(2)]
    w13_0_b = scr("w13_0_b", [NFC0, 128, 8, 256], BF16).ap()
    w2_0_b = scr("w2_0_b", [2, NFC0, 128, 512], BF16).ap()
    w13_m_b = scr("w13_m_b", [NEXP, NFCE, 128, 8, 256], BF16).ap()
    w2_m_b = scr("w2_m_b", [NEXP, 2, NFCE, 128, 512], BF16).ap()
    qaug_b = scr("qaug_b", [6, 6, S], BF16).ap()
    kaug_b = scr("kaug_b", [6, 6, S], BF16).ap()
    mine_h = scr("qkv_mine", [2 * GROWS, T], BF16)
    allq_h = scr("qkv_all", [4 * GROWS, T], BF16)
    my_h = scr("qkv_my", [2 * GROWS, T], BF16)
    mine, allq, my = mine_h.ap(), allq_h.ap(), my_h.ap()
    omine = scr("o_mine", [512, S], BF16).ap()
    oall = scr("o_all", [1024, S], BF16).ap()
    omy = scr("o_my", [1024, T], BF16).ap()
    x1 = scr("x1", [T, D], F32).ap()

    dbg_out = {}
    for name, shape, dt in dbg:
        dbg_out[name] = nc.dram_tensor("dbg_" + name, list(shape), dt, kind="ExternalOutput").ap()

    ARENA = 94208
    arena_t = nc.alloc_sbuf_tensor("arena", [128, ARENA], BF16)
    ar = Arena(arena_t.ap(), ARENA)
    ps = [nc.alloc_psum_tensor("ps%d" % i, [128, 512], F32).ap() for i in range(8)]
    psb = [p.bitcast(BF16) for p in ps]

    s = Sched(nc)

    identf = ar.alloc([128, 128], F32)
    identb = ar.alloc([128, 128], BF16)
    masks = ar.alloc([128, 7, 512], F32)
    onesf = ar.alloc([128, 128], F32)
    gains = [ar.alloc([128, 24], F32) for _ in range(2)]
    ar.persist()

    s.dma("sp", lambda h: h.dma_start(out=identf, in_=ident_d), writes=["identf"])
    s.dma("pool", lambda h: h.dma_start(out=identb, in_=ident_d), writes=["identb"])
    s.dma("sp", lambda h: h.dma_start(out=masks, in_=masks_d.rearrange("v p c -> p v c")), writes=["masks"])
    s.op("pool", lambda h: h.memset(onesf, 1.0), writes=["onesf"])
    for l in range(2):
        s.dma("sp", lambda h, l=l: h.dma_start(out=gains[l], in_=gains_d[l]), writes=[("gains", l)])

    def cast(dst, src, tok, bg=False):
        s.dma("pool", lambda h: h.dma_start(out=dst, in_=src), writes=[tok], bg=bg)

    def cast_layer_small(l):
        for kc in range(8):
            cast(win_b[l][:, kc, :], win_d[l][:, kc, :], ("wb_win", l, kc))
        cast(wuq_b[l], wuq_d[l], ("wb_wuq", l))
        cast(wukv_b[l], wukv_d[l], ("wb_wukv", l))
        for kc in range(0, 8, 4):
            cast(wout_b[l][:, kc:kc + 4, :], wout_d[l][:, kc:kc + 4, :], ("wb_wout", l, kc // 4))

    def cast_ffn0():
        for f0 in range(0, NFC0, 4):
            f1 = min(NFC0, f0 + 4)
            cast(w13_0_b[f0:f1], w13_0_d[f0:f1], ("wb_w13_0", f0 // 4), bg=True)
        for hf in range(2):
            for f0 in range(0, NFC0, 8):
                f1 = min(NFC0, f0 + 8)
                cast(w2_0_b[hf, f0:f1], w2_0_d[hf, f0:f1], ("wb_w2_0", hf, f0 // 8), bg=True)

    def cast_moe(e):
        for f0 in range(0, NFCE):
            cast(w13_m_b[e, f0:f0 + 1], w13_m_d[e, f0:f0 + 1], ("wb_w13_m", e, f0), bg=True)
        for hf in range(2):
            for f0 in range(0, NFCE, 4):
                cast(w2_m_b[e, hf, f0:f0 + 4], w2_m_d[e, hf, f0:f0 + 4], ("wb_w2_m", e, hf, f0 // 4), bg=True)

    cast(qaug_b, qaug_d, "qaug_b")
    cast(kaug_b, kaug_d, "kaug_b")
    cast_layer_small(0)

    rot = {}

    def nxt(key, n):
        v = rot.get(key, 0)
        rot[key] = (v + 1) % n
        return v

    def mm_group(out, parts, tok):
        n = len(parts)
        for i, (l_, r_, rd) in enumerate(parts):
            s.op("pe", lambda h, l_=l_, r_=r_, i=i: h.matmul(out, l_, r_, start=(i == 0), stop=(i == n - 1)),
                 reads=list(rd), writes=[tok] if i in (0, n - 1) else [])

    def dump(name, src, reads):
        if name in dbg_out:
            s.dma("sp", lambda h: h.dma_start(out=dbg_out[name], in_=src), reads=reads)

    def rms_rstd(ss_ap, out_ap, n, toks_in, tok_out):
        s.op("act", lambda h: h.activation(out=out_ap, in_=ss_ap, func=AF.Sqrt, scale=1.0 / n, bias=EPS),
             reads=toks_in, writes=[tok_out])
        s.op("dve", lambda h: h.reciprocal(out=out_ap, in_=out_ap), reads=[tok_out], writes=[tok_out])

    def phase_A(l, xsrc):
        s.barrier()
        ar.reset()
        win = ar.alloc([128, 8, NCOLS], BF16)
        wuq = ar.alloc([128, 3, 768], BF16)
        wukv = ar.alloc([128, 512], BF16)
        xt = [ar.alloc([128, 4, 1024], F32) for _ in range(2)]
        xn = ar.alloc([128, 4, 1024], BF16)
        junk = ar.alloc([128, 1024], BF16)
        hT = [ar.alloc([128, 8, 512], BF16) for _ in range(2)]
        ss4 = ar.alloc([128, 4], F32)
        rs4 = ar.alloc([128, 4], F32)
        ssm = ar.alloc([128, 4, 2], F32)
        rsm = ar.alloc([128, 4, 2], F32)
        cn = [ar.alloc([128, 512], BF16) for _ in range(2)]
        cT = ar.alloc([128, 4, 512], BF16)
        fmst = [ar.alloc([128, 512], BF16) for _ in range(4)]
        vst = ar.alloc([128, 4, 896], BF16)
        ropeK = ar.alloc([128, 2, 512], F32)
        ropeQ = ar.alloc([128, 2, 512], F32)
        rtmp = [ar.alloc([128, 512], F32) for _ in range(2)]
        G = gains[l]
        for kc in range(8):
            s.dma("sp", lambda h, kc=kc: h.dma_start(out=win[:, kc, :], in_=win_b[l][:, kc, :]),
                  reads=[("wb_win", l, kc)], writes=[("win", kc)])
        s.dma("sp", lambda h: h.dma_start(out=wuq, in_=wuq_b[l]), reads=[("wb_wuq", l)], writes=["wuq"])
        s.dma("sp", lambda h: h.dma_start(out=wukv, in_=wukv_b[l]), reads=[("wb_wukv", l)], writes=["wukv"])
        winr = [("win", kc) for kc in range(8)]

        def load_x(t):
            b = t % 2
            s.dma("sp", lambda h: h.dma_start(out=xt[b], in_=xsrc[t * 512:(t + 1) * 512, :].rearrange("(b p) d -> p b d", p=128)),
                  reads=[("xsrc", t)], writes=[("xt", b)])

        def store_fm(src_ap, nrows, g, row, t, rd):
            dst = mine[g * GROWS + row: g * GROWS + row + nrows, t * 512:(t + 1) * 512]
            s.dma("act", lambda h: h.dma_start(out=dst, in_=src_ap), reads=rd, writes=[("mine", g, row, t)])

        def evac(bank_ap, dst_ap, rd, wr, k=[0]):
            k[0] += 1
            if k[0] % 2:
                s.op("act", lambda h: h.copy(out=dst_ap, in_=bank_ap), reads=rd, writes=wr)
            else:
                s.op("dve", lambda h: h.tensor_copy(out=dst_ap, in_=bank_ap), reads=rd, writes=wr)

        def tileA(t):
            b = t % 2
            X = xt[b]
            s.dma("sp", lambda h, t=t: h.dma_start(out=ropeK[0:32], in_=rope_d[:, :, t * 512:(t + 1) * 512].rearrange("a r c -> r a c")),
                  writes=["ropeK"])
            s.dma("sp", lambda h, t=t: h.dma_start(out=ropeQ[64:96], in_=rope_d[:, :, t * 512:(t + 1) * 512].rearrange("a r c -> r a c")),
                  writes=["ropeQ"])
            s.op("dve", lambda h: h.memset(ss4, 0.0), writes=[("ss4", i) for i in range(4)])
            for blk in range(4):
                s.op("act", lambda h, blk=blk: h.activation(out=junk, in_=X[:, blk, :], func=AF.Square, accum_out=ss4[:, blk:blk + 1]),
                     reads=[("xt", b), ("ss4", blk)], writes=[("ss4", blk)])
            rms_rstd(ss4, rs4, D, [("ss4", i) for i in range(4)], "rs4")
            for blk in range(4):
                s.op("dve", lambda h, blk=blk: h.tensor_scalar(out=xn[:, blk, :], in0=X[:, blk, :], scalar1=rs4[:, blk:blk + 1],
                                                              scalar2=None, op0=ALU.mult),
                     reads=[("xt", b), "rs4"], writes=[("xn", blk)])
            for blk in range(4):
                tb = nxt("TB", 2)
                for kc in range(8):
                    s.op("pe", lambda h, blk=blk, kc=kc, tb=tb: h.transpose(out=psb[tb][:, kc * 128:(kc + 1) * 128],
                                                                            in_=xn[:, blk, kc * 128:(kc + 1) * 128], identity=identb),
                         reads=[("xn", blk), "identb"], writes=[("ps", tb)] if kc in (0, 7) else [])
                s.op("dve", lambda h, blk=blk, tb=tb: h.tensor_tensor(
                    out=hT[b][:, :, blk * 128:(blk + 1) * 128], in0=psb[tb].rearrange("p (k t) -> p k t", k=8),
                    in1=G[:, 0:8].unsqueeze(2).broadcast_to([128, 8, 128]), op=ALU.mult),
                    reads=[("ps", tb), ("gains", l)], writes=[("hT", b, blk)])
            hTr = [("hT", b, i) for i in range(4)]

            def fm_group(co, M):
                bank = 2 + nxt("FM", 3)
                mm_group(ps[bank][0:M, :], [(win[:, kc, co:co + M], hT[b][:, kc, :], winr[kc:kc + 1] + hTr) for kc in range(8)],
                         ("ps", bank))
                return bank

            for hh in range(4):
                bank = fm_group(CO_A + hh * 128, 128)
                st = nxt("fmst", 4)
                evac(ps[bank], fmst[st], [("ps", bank)], [("fmst", st)])
                store_fm(fmst[st], 128, INV_A[hh][0], R_A + INV_A[hh][1] * 128, t, [("fmst", st)])
            for hh in range(4):
                bank = fm_group(CO_B + hh * 128, 128)
                st = nxt("fmst", 4)
                evac(ps[bank], fmst[st], [("ps", bank)], [("fmst", st)])
                store_fm(fmst[st], 128, hh // 2, R_B + (hh % 2) * 128, t, [("fmst", st)])
            for g in range(2):
                bank = fm_group(CO_DQ + g * 128, 128)
                st = nxt("fmst", 4)
                evac(ps[bank], fmst[st], [("ps", bank)], [("fmst", st)])
                store_fm(fmst[st], 128, g, R_DQ, t, [("fmst", st)])
            bank = fm_group(CO_DK, 128)
            st = nxt("fmst", 4)
            evac(ps[bank], fmst[st], [("ps", bank)], [("fmst", st)])
            for g in range(2):
                store_fm(fmst[st][g * 64:(g + 1) * 64], 64, g, R_DK, t, [("fmst", st)])
            b1 = fm_group(CO_KPE, 32)
            b2 = fm_group(CO_KPES, 32)
            s.op("dve", lambda h, b1=b1: h.tensor_tensor(out=rtmp[0][0:32], in0=ps[b1][0:32], in1=ropeK[0:32, 0, :], op=ALU.mult),
                 reads=[("ps", b1), "ropeK"], writes=["rtmp0"])
            s.op("dve", lambda h, b2=b2: h.tensor_tensor(out=rtmp[1][0:32], in0=ps[b2][0:32], in1=ropeK[0:32, 1, :], op=ALU.mult),
                 reads=[("ps", b2), "ropeK"], writes=["rtmp1"])
            st = nxt("fmst", 4)
            s.op("dve", lambda h, st=st: h.tensor_tensor(out=fmst[st][0:32], in0=rtmp[0][0:32], in1=rtmp[1][0:32], op=ALU.add),
                 reads=["rtmp0", "rtmp1"], writes=[("fmst", st)])
            for g in range(2):
                store_fm(fmst[st][0:32], 32, g, R_KPE, t, [("fmst", st)])
            for blk in range(4):
                def tm_group(co, N):
                    bank = 5 + nxt("TM", 3)
                    mm_group(ps[bank][:, 0:N], [(hT[b][:, kc, blk * 128:(blk + 1) * 128], win[:, kc, co:co + N],
                                                 winr[kc:kc + 1] + [("hT", b, blk)]) for kc in range(8)], ("ps", bank))
                    return bank
                bk = tm_group(CO_TB, 512)
                evac(ps[bk], vst[:, blk, 0:512], [("ps", bk)], [("vst", blk, 0)])
                bk = tm_group(CO_TC, 128)
                evac(ps[bk][:, 0:128], vst[:, blk, 768:896], [("ps", bk)], [("vst", blk, 2)])
                bk = tm_group(CO_TA, 512)
                s.op("dve", lambda h, blk=blk: h.memset(ssm[:, blk, :], 0.0), writes=[("ssm", blk)])
                s.op("act", lambda h, blk=blk, bk=bk: h.activation(out=junk[:, 0:384], in_=ps[bk][:, 0:384], func=AF.Square,
                                                                  accum_out=ssm[:, blk, 0:1]),
                     reads=[("ps", bk), ("ssm", blk)], writes=[("ssm", blk)])
                s.op("act", lambda h, blk=blk, bk=bk: h.activation(out=junk[:, 384:512], in_=ps[bk][:, 384:512], func=AF.Square,
                                                                  accum_out=ssm[:, blk, 1:2]),
                     reads=[("ps", bk), ("ssm", blk)], writes=[("ssm", blk)])
                s.op("act", lambda h, blk=blk: h.activation(out=rsm[:, blk, 0:1], in_=ssm[:, blk, 0:1], func=AF.Sqrt, scale=1.0 / 384, bias=EPS),
                     reads=[("ssm", blk)], writes=[("rsm", blk)])
                s.op("act", lambda h, blk=blk: h.activation(out=rsm[:, blk, 1:2], in_=ssm[:, blk, 1:2], func=AF.Sqrt, scale=1.0 / 128, bias=EPS),
                     reads=[("ssm", blk)], writes=[("rsm", blk)])
                s.op("dve", lambda h, blk=blk: h.reciprocal(out=rsm[:, blk, :], in_=rsm[:, blk, :]), reads=[("rsm", blk)], writes=[("rsm", blk)])
                ci = nxt("cn", 2)
                s.op("dve", lambda h, blk=blk, bk=bk, ci=ci: h.tensor_scalar(out=cn[ci][:, 0:384], in0=ps[bk][:, 0:384], scalar1=rsm[:, blk, 0:1],
                                                                            scalar2=None, op0=ALU.mult),
                     reads=[("ps", bk), ("rsm", blk)], writes=[("cn", ci)])
                s.op("dve", lambda h, blk=blk, bk=bk, ci=ci: h.tensor_scalar(out=cn[ci][:, 384:512], in0=ps[bk][:, 384:512], scalar1=rsm[:, blk, 1:2],
                                                                            scalar2=None, op0=ALU.mult),
                     reads=[("ps", bk), ("rsm", blk)], writes=[("cn", ci)])
                tb = nxt("TB", 2)
                for i in range(4):
                    s.op("pe", lambda h, i=i, tb=tb, ci=ci: h.transpose(out=psb[tb][:, i * 128:(i + 1) * 128], in_=cn[ci][:, i * 128:(i + 1) * 128],
                                                                       identity=identb),
                         reads=[("cn", ci), "identb"], writes=[("ps", tb)] if i in (0, 3) else [])
                s.op("dve", lambda h, blk=blk, tb=tb: h.tensor_tensor(
                    out=cT[:, :, blk * 128:(blk + 1) * 128], in0=psb[tb][:, 0:512].rearrange("p (k t) -> p k t", k=4),
                    in1=G[:, 16:20].unsqueeze(2).broadcast_to([128, 4, 128]), op=ALU.mult),
                    reads=[("ps", tb), ("gains", l)], writes=[("cT", blk)])
                bank = 5 + nxt("TM", 3)
                mm_group(ps[bank][:, 0:256], [(cT[:, 3, blk * 128:(blk + 1) * 128], wukv[:, 256:512], ["wukv", ("cT", blk)])], ("ps", bank))
                evac(ps[bank][:, 0:256], vst[:, blk, 512:768], [("ps", bank)], [("vst", blk, 1)])
            cTr = [("cT", i) for i in range(4)]
            vr = [("vst", i, k) for i in range(4) for k in range(3)]
            for g in range(2):
                for name, slot in VSLOT.items():
                    if name[0] == "D":
                        col = 768 + g * 64
                    elif name[0] == "A":
                        col = HMAP_A[g][int(name[1])] * 64
                    else:
                        col = {"A": 0, "B": 256, "C": 512}[name[0]] + (2 * g + int(name[1])) * 64
                    dst = bass.AP(tensor=mine_h, offset=(g * GROWS + FMROWS + slot * 64) * T + t * 512 * 64,
                                  ap=[[64, 128], [128 * 64, 4], [1, 64]])
                    s.dma("act", lambda h, dst=dst, col=col: h.dma_start(out=dst, in_=vst[:, :, col:col + 64]),
                          reads=vr, writes=[("mine", g, "v", slot, t)])
            for hh in range(4):
                bo = 2 + nxt("FM", 3)
                mm_group(ps[bo][0:96, :], [(wuq[:, kc, hh * 96:(hh + 1) * 96], cT[:, kc, :], ["wuq"] + cTr) for kc in range(3)], ("ps", bo))
                bs = 2 + nxt("FM", 3)
                mm_group(ps[bs][0:96, :], [(wuq[:, kc, 384 + hh * 96:384 + (hh + 1) * 96], cT[:, kc, :], ["wuq"] + cTr) for kc in range(3)], ("ps", bs))
                st = nxt("fmst", 4)
                s.op("act", lambda h, bo=bo, st=st: h.copy(out=fmst[st][0:64], in_=ps[bo][0:64]), reads=[("ps", bo)], writes=[("fmst", st)])
                s.op("dve", lambda h, bo=bo: h.tensor_tensor(out=rtmp[0][64:96], in0=ps[bo][64:96], in1=ropeQ[64:96, 0, :], op=ALU.mult),
                     reads=[("ps", bo), "ropeQ"], writes=["rtmp0"])
                s.op("dve", lambda h, bs=bs: h.tensor_tensor(out=rtmp[1][64:96], in0=ps[bs][64:96], in1=ropeQ[64:96, 1, :], op=ALU.mult),
                     reads=[("ps", bs), "ropeQ"], writes=["rtmp1"])
                s.op("dve", lambda h, st=st: h.tensor_tensor(out=fmst[st][64:96], in0=rtmp[0][64:96], in1=rtmp[1][64:96], op=ALU.add),
                     reads=["rtmp0", "rtmp1", ("fmst", st)], writes=[("fmst", st)])
                store_fm(fmst[st][0:96], 96, hh // 2, R_CQ + (hh % 2) * 96, t, [("fmst", st)])
            for g in range(2):
                bank = 2 + nxt("FM", 3)
                mm_group(ps[bank][:, :], [(wukv[:, g * 128:(g + 1) * 128], cT[:, 3, :], ["wukv"] + cTr)], ("ps", bank))
                st = nxt("fmst", 4)
                evac(ps[bank], fmst[st], [("ps", bank)], [("fmst", st)])
                store_fm(fmst[st], 128, g, R_CK, t, [("fmst", st)])

        load_x(0)
        for t in range(NTA):
            if t + 1 < NTA:
                load_x(t + 1)
            tileA(t)

    rank_cache = {}

    def rank_of(h):
        if "r" not in rank_cache:
            rank_cache["r"] = h.partition_id() % 2
        return rank_cache["r"]

    CR = GROWS // 8
    NCG = 8

    def exchange1():
        toks = []
        for k in range(2 * NCG):
            g_, kk = k // NCG, k % NCG
            rows = slice(g_ * GROWS + kk * CR, g_ * GROWS + (kk + 1) * CR)
            mt = [t_ for t_ in s.group("mine")]
            s.collective(lambda h, k=k, rows=rows: h.collective_compute(
                "AllGather", ALU.bypass, replica_groups=RG, ins=[mine[rows, :]], outs=[allq[k * 2 * CR:(k + 1) * 2 * CR, :]]),
                reads=mt, writes=[("allq", k)])
            toks.append(("allq", k))
        for hf in range(2):
            def f(h, hf=hf):
                r = rank_of(h)
                src = allq[bass.ds(r * (NCG * 2 * CR), NCG * 2 * CR), :].rearrange("(k two c) t -> k two c t", two=2, c=CR)[:, hf]
                dst = my[hf * GROWS:(hf + 1) * GROWS, :].rearrange("(k c) t -> k c t", c=CR)
                return h.dma_start(out=dst, in_=src)
            s.dma("pool", f, reads=toks, writes=[("my", hf)])

    def exchange2():
        toks = []
        for k in range(4):
            s.collective(lambda h, k=k: h.collective_compute(
                "AllGather", ALU.bypass, replica_groups=RG, ins=[omine[k * 128:(k + 1) * 128, :]], outs=[oall[k * 256:(k + 1) * 256, :]]),
                reads=s.group("omine"), writes=[("oall", k)])
            toks.append(("oall", k))
        for half in range(2):
            def f(h, half=half):
                r = rank_of(h)
                return h.dma_start(out=omy[half * 512:(half + 1) * 512, :], in_=oall[half * 512:(half + 1) * 512, bass.ds(r * T, T)])
            s.dma("pool", f, reads=toks, writes=[("omy", half)])

    def phase_B(l):
        s.barrier()
        ar.reset()
        lam_init = 0.8 - 0.6 * float(np.exp(-0.3 * l))
        Kt = [ar.alloc([128, S], BF16) for _ in range(2)]
        Vsb = ar.alloc([128, NKB, 65], BF16)
        Qt = [[ar.alloc([128, 512], BF16) for _ in range(2)] for _ in range(2)]
        P = [ar.alloc([128, 512], BF16) for _ in range(4)]
        Ssb = [ar.alloc([128, 512], F32) for _ in range(2)]
        rec = ar.alloc([128, 512], F32)
        bcs = ar.alloc([128, 512], F32)
        onr = [ar.alloc([128, 512], F32) for _ in range(2)]
        df = ar.alloc([128, 512], F32)
        sq = ar.alloc([128, 512], F32)
        rsb = ar.alloc([128, 512], F32)
        ost = [ar.alloc([128, 512], BF16) for _ in range(2)]
        acc = ar.alloc([128, S], F32)
        dl = ar.alloc([128, 128], F32)
        sm = ar.alloc([128, 16], F32)
        G = gains[l]
        myr = s.group("my")
        HQ = NQT // 2

        s.dma("sp", lambda h: h.dma_start(out=dl, in_=bass.AP(tensor=dlam_h[l], offset=0, ap=[[0, 128], [1, 128]])), writes=["dl"])
        s.dma("sp", lambda h: h.dma_start(out=sm[:, 8:10], in_=bass.AP(tensor=sinks_h[l], offset=0, ap=[[0, 128], [1, 2]])), writes=["sink"])
        s.op("dve", lambda h: h.tensor_tensor(out=dl[:, 0:32], in0=dl[:, 0:32], in1=dl[:, 32:64], op=ALU.mult), reads=["dl"], writes=["dl"])
        s.op("dve", lambda h: h.tensor_tensor(out=dl[:, 64:96], in0=dl[:, 64:96], in1=dl[:, 96:128], op=ALU.mult), reads=["dl"], writes=["dl"])
        s.op("dve", lambda h: h.reduce_sum(out=sm[:, 0:1], in_=dl[:, 0:32], axis=mybir.AxisListType.X), reads=["dl"], writes=["sm"])
        s.op("dve", lambda h: h.reduce_sum(out=sm[:, 1:2], in_=dl[:, 64:96], axis=mybir.AxisListType.X), reads=["dl", "sm"], writes=["sm"])
        s.op("act", lambda h: h.activation(out=sm[:, 2:4], in_=sm[:, 0:2], func=AF.Exp), reads=["sm"], writes=["sm"])
        s.op("dve", lambda h: h.tensor_tensor(out=sm[:, 4:5], in0=sm[:, 3:4], in1=sm[:, 2:3], op=ALU.subtract), reads=["sm"], writes=["sm"])
        s.op("dve", lambda h: h.tensor_scalar(out=sm[:, 4:5], in0=sm[:, 4:5], scalar1=-lam_init, scalar2=None, op0=ALU.add), reads=["sm"], writes=["sm"])
        s.op("dve", lambda h: h.tensor_scalar(out=sm[:, 5:6], in0=G[:, 20:21], scalar1=1.0 - lam_init, scalar2=None, op0=ALU.mult),
             reads=["sm", ("gains", l)], writes=["sm"])
        s.op("act", lambda h: h.activation(out=sm[:, 10:12], in_=sm[:, 8:10], func=AF.Exp), reads=["sink", "sm"], writes=["sm"])
        neglam = sm[:, 4:5]
        gainA = sm[:, 5:6]
        s.op("dve", lambda h: h.memset(Vsb[:, :, 64:65], 1.0), writes=["Vones"])

        def load_rows(dst_tile, prow, nrows, row, tok_fn, cols=None):
            for hf in range(2):
                s.dma("sp", lambda h, hf=hf: h.dma_start(out=dst_tile[prow:prow + nrows, hf * T:(hf + 1) * T],
                                                        in_=my[hf * GROWS + row: hf * GROWS + row + nrows, :]),
                      reads=myr, writes=[tok_fn(hf)])

        def load_V(slot, d):
            for hf in range(2):
                c16 = [("Vsb", k) for k in range(hf * NBT // 16, (hf + 1) * NBT // 16)]
                if d == 1:
                    src = bass.AP(tensor=my_h, offset=(hf * GROWS + FMROWS + slot * 64) * T, ap=[[64, 128], [128 * 64, NBT], [1, 64]])
                    s.dma("sp", lambda h, hf=hf, src=src: h.dma_start(out=Vsb[:, hf * NBT:(hf + 1) * NBT, 0:64], in_=src),
                          reads=myr, writes=c16)
                else:
                    ncb = T // (128 * d)
                    for c in range(ncb):
                        src = bass.AP(tensor=my_h, offset=(hf * GROWS + FMROWS + slot * 64) * T + c * 128 * d * 64,
                                      ap=[[d * 64, 128], [64, d], [1, 64]])
                        b0 = hf * NBT + c * d
                        s.dma("sp", lambda h, src=src, b0=b0: h.dma_start(out=Vsb[:, b0:b0 + d, 0:64], in_=src),
                              reads=myr, writes=[("Vsb", b0 // 16)])

        def finalize_norm(src65, qt_cols_tok, extra_den=None):
            if extra_den is not None:
                s.op("dve", lambda h: h.tensor_scalar(out=rec[64:65, :], in0=src65[64:65, :], scalar1=extra_den, scalar2=None, op0=ALU.add),
                     reads=qt_cols_tok + ["sm"], writes=["rec"])
                s.op("dve", lambda h: h.reciprocal(out=rec[64:65, :], in_=rec[64:65, :]), reads=["rec"], writes=["rec"])
            else:
                s.op("dve", lambda h: h.reciprocal(out=rec[64:65, :], in_=src65[64:65, :]), reads=qt_cols_tok, writes=["rec"])
            s.op("pe", lambda h: h.matmul(ps[7][0:64, :], onesf[64:65, 0:64], rec[64:65, :], start=True, stop=True),
                 reads=["rec", "onesf"], writes=[("ps", 7)])
            s.op("act", lambda h: h.copy(out=bcs[0:64], in_=ps[7][0:64]), reads=[("ps", 7)], writes=["bcs"])

        def store_o(st, mixer, j, qt):
            dst = omine[mixer * 128 + j * 64: mixer * 128 + j * 64 + 64, qt * 512:(qt + 1) * 512]
            s.dma("act", lambda h: h.dma_start(out=dst, in_=ost[st][0:64]), reads=[("ost", st)], writes=[("omine", mixer, j, qt)])

        def dense_head(kind, j):
            if kind == "A":
                maps, dk, dd = 2, 96, 32
                scale = 32 ** -0.5
                rq = [R_A + j * 128, R_A + j * 128 + 32]
                rk = [R_A + j * 128 + 64, R_A + j * 128 + 96]
                vslot, mixer = VSLOT["A%d" % j], 0
            else:
                maps, dk, dd = 1, 96, 96
                scale = 96 ** -0.5
                rq = [R_CQ + j * 96]
                rk = [R_CK + j * 64]
                vslot, mixer = VSLOT["C%d" % j], 2
            for m in range(maps):
                if kind == "A":
                    for p0 in (32, 64):
                        s.op("dve", lambda h, m=m, p0=p0: h.memset(Kt[m][p0:p0 + 32, :], 0.0), writes=[("Kt", m, "aug")])
                        for b_ in range(2):
                            s.op("dve", lambda h, m=m, b_=b_, p0=p0: h.memset(Qt[b_][m][p0:p0 + 32, :], 0.0), writes=[("Qa", b_, m)])
                    load_rows(Kt[m], 0, 32, rk[m], lambda hf, m=m: ("Kt", m, hf))
                    s.dma("sp", lambda h, m=m: h.dma_start(out=Kt[m][32:38, :], in_=kaug_b[j]), reads=["kaug_b"], writes=[("Kt", m, "aug")])
                else:
                    load_rows(Kt[m], 0, 64, rk[m], lambda hf, m=m: ("Kt", m, hf))
                    load_rows(Kt[m], 64, 32, R_KPE, lambda hf, m=m: ("Kt", m, "aug"))
            load_V(vslot, 1)
            def qtile(qt, hook):
                b = qt % 2
                hfq, lq = qt // HQ, qt % HQ
                nr = 32 if kind == "A" else 96
                for m in range(maps):
                    s.dma("sp", lambda h, m=m: h.dma_start(out=Qt[b][m][0:nr, :],
                                                           in_=my[hfq * GROWS + rq[m]: hfq * GROWS + rq[m] + nr, lq * 512:(lq + 1) * 512]),
                          reads=myr, writes=[("Qt", b, m)])
                    if kind == "A":
                        s.dma("sp", lambda h, m=m: h.dma_start(out=Qt[b][m][32:38, :], in_=qaug_b[j][:, qt * 512:(qt + 1) * 512]),
                              reads=["qaug_b"], writes=[("Qa", b, m)])
                kb_lo = 0
                if kind == "A":
                    sl_min = min(float(_alibi_slopes()[0][HMAP_A[0][j]]), float(_alibi_slopes()[0][HMAP_A[1][j]]))
                    kb_lo = max(0, int(np.ceil((qt * 512 - 127 - ALIBI_TH / sl_min) / 128.0)))
                kbs = list(range(kb_lo, 4 * qt + 4))
                units = [(kb, m) for kb in kbs for m in range(maps)]
                ob = [3 + 2 * (qt % 2) + m for m in range(maps)]
                pend = []

                def issue_S(kb, m):
                    bank = nxt("SB", 3)
                    hfk = kb // NBT
                    s.op("pe", lambda h: h.matmul(ps[bank][:, :], Kt[m][0:dk, kb * 128:(kb + 1) * 128], Qt[b][m][0:dk, :], start=True, stop=True),
                         reads=[("Kt", m, hfk), ("Kt", m, "aug"), ("Qt", b, m), ("Qa", b, m)], writes=[("ps", bank)])
                    pi = nxt("P", 4)
                    v = kb - 4 * qt
                    if v >= 0:
                        si = nxt("Ssb", 2)
                        s.op("dve", lambda h: h.tensor_tensor(out=Ssb[si], in0=ps[bank], in1=masks[:, v, :], op=ALU.add),
                             reads=[("ps", bank), "masks"], writes=[("Ssb", si)])
                        s.op("act", lambda h: h.activation(out=P[pi], in_=Ssb[si], func=AF.Exp, scale=scale), reads=[("Ssb", si)], writes=[("P", pi)])
                    else:
                        s.op("act", lambda h: h.activation(out=P[pi], in_=ps[bank], func=AF.Exp, scale=scale), reads=[("ps", bank)], writes=[("P", pi)])
                    return pi

                def issue_PV(kb, m, pi):
                    first, last = kb == kbs[0], kb == kbs[-1]
                    s.op("pe", lambda h: h.matmul(ps[ob[m]][0:65, :], Vsb[:, kb, 0:65], P[pi], start=first, stop=last),
                         reads=[("Vsb", kb // 16), "Vones", ("P", pi)], writes=[("ps", ob[m])] if (first or last) else [])

                for ui, (kb, m) in enumerate(units):
                    pi = issue_S(kb, m)
                    pend.append((kb, m, pi))
                    if len(pend) > 2:
                        issue_PV(*pend.pop(0))
                    if ui == 3 and hook is not None:
                        hook()
                        hook = None
                while pend:
                    issue_PV(*pend.pop(0))
                if hook is not None:
                    hook()
                return lambda: fin(qt, ob)

            def fin(qt, ob):
                st = nxt("ost", 2)
                if kind == "C":
                    finalize_norm(ps[ob[0]], [("ps", ob[0])])
                    s.op("dve", lambda h: h.tensor_tensor(out=ost[st][0:64], in0=ps[ob[0]][0:64], in1=bcs[0:64], op=ALU.mult),
                         reads=[("ps", ob[0]), "bcs"], writes=[("ost", st)])
                else:
                    for m in range(2):
                        finalize_norm(ps[ob[m]], [("ps", ob[m])])
                        s.op("dve", lambda h, m=m: h.tensor_tensor(out=onr[m][0:64], in0=ps[ob[m]][0:64], in1=bcs[0:64], op=ALU.mult),
                             reads=[("ps", ob[m]), "bcs"], writes=[("onr", m)])
                    s.op("dve", lambda h: h.scalar_tensor_tensor(out=df[0:64], in0=onr[1][0:64], scalar=neglam[0:64], in1=onr[0][0:64],
                                                                 op0=ALU.mult, op1=ALU.add),
                         reads=[("onr", 0), ("onr", 1), "sm"], writes=["df"])
                    s.op("act", lambda h: h.activation(out=sq[0:64], in_=df[0:64], func=AF.Square), reads=["df"], writes=["sq"])
                    s.op("pe", lambda h: h.matmul(ps[7][0:64, :], onesf[0:64, 0:64], sq[0:64, :], start=True, stop=True),
                         reads=["sq", "onesf"], writes=[("ps", 7)])
                    s.op("act", lambda h: h.activation(out=rsb[0:64], in_=ps[7][0:64], func=AF.Sqrt, scale=1.0 / 64, bias=EPS),
                         reads=[("ps", 7)], writes=["rsb"])
                    s.op("dve", lambda h: h.reciprocal(out=rsb[0:64], in_=rsb[0:64]), reads=["rsb"], writes=["rsb"])
                    s.op("dve", lambda h: h.scalar_tensor_tensor(out=ost[st][0:64], in0=df[0:64], scalar=gainA[0:64], in1=rsb[0:64],
                                                                 op0=ALU.mult, op1=ALU.mult),
                         reads=["df", "rsb", "sm"], writes=[("ost", st)])
                store_o(st, mixer, j, qt)

            prev = None
            for qt in range(NQT):
                prev = qtile(qt, prev)
            prev()

        def banded_head(kind, j):
            scale = 64 ** -0.5
            if kind == "B":
                rowq, rowk = R_B + j * 128, R_B + j * 128 + 64
                augslot, vslot, mixer = 2 + j, VSLOT["B%d" % j], 1
                patterns = [(1, 6), (4, 6), (16, 6)]
            else:
                rowq, rowk = R_DQ + j * 64, R_DK
                augslot, vslot, mixer = 4 + j, VSLOT["D"], 3
                patterns = [(1, 5)]
            Kf, Qf = Kt[0], Kt[1]
            s.op("dve", lambda h: h.memset(Kf[64:96, :], 0.0), writes=[("Kt", 0, "aug")])
            s.op("dve", lambda h: h.memset(Qf[64:96, :], 0.0), writes=[("Kt", 1, "aug")])
            load_rows(Kf, 0, 64, rowk, lambda hf: ("Kt", 0, hf))
            s.dma("sp", lambda h: h.dma_start(out=Kf[64:70, :], in_=kaug_b[augslot]), reads=["kaug_b"], writes=[("Kt", 0, "aug")])
            load_rows(Qf, 0, 64, rowq, lambda hf: ("Kt", 1, hf))
            s.dma("sp", lambda h: h.dma_start(out=Qf[64:70, :], in_=qaug_b[augslot]), reads=["qaug_b"], writes=[("Kt", 1, "aug")])
            kq_r = [("Kt", m, x) for m in range(2) for x in (0, 1, "aug")]
            def group4(pidx, d, mprev, g4):
                if True:
                    idxs = [g4 * 4 + i for i in range(4)]
                    cr = [divmod(ix, d) for ix in idxs]
                    base = [128 * d * c + r for (c, r) in cr]
                    has_prev = [c >= 1 for (c, r) in cr]
                    bo = nxt("SB", 3)
                    for i in range(4):
                        sl = slice(base[i], base[i] + 127 * d + 1, d)
                        s.op("pe", lambda h, i=i, sl=sl: h.matmul(ps[bo][:, i * 128:(i + 1) * 128], Kf[0:96, sl], Qf[0:96, sl], start=True, stop=True),
                             reads=kq_r, writes=[("ps", bo)] if i in (0, 3) else [])
                    so = nxt("Ssb", 2)
                    s.op("dve", lambda h: h.tensor_tensor(out=Ssb[so], in0=ps[bo], in1=masks[:, 4, :], op=ALU.add),
                         reads=[("ps", bo), "masks"], writes=[("Ssb", so)])
                    po = nxt("P", 4)
                    s.op("act", lambda h: h.activation(out=P[po], in_=Ssb[so], func=AF.Exp, scale=scale), reads=[("Ssb", so)], writes=[("P", po)])
                    pp = None
                    if any(has_prev):
                        bp = nxt("SB", 3)
                        ii = [i for i in range(4) if has_prev[i]]
                        for i in ii:
                            slq = slice(base[i], base[i] + 127 * d + 1, d)
                            slk = slice(base[i] - 128 * d, base[i] - d + 1, d)
                            s.op("pe", lambda h, i=i, slq=slq, slk=slk: h.matmul(ps[bp][:, i * 128:(i + 1) * 128], Kf[0:96, slk], Qf[0:96, slq],
                                                                                start=True, stop=True),
                                 reads=kq_r, writes=[("ps", bp)] if i in (ii[0], ii[-1]) else [])
                        sp_ = nxt("Ssb", 2)
                        s.op("dve", lambda h: h.tensor_tensor(out=Ssb[sp_], in0=ps[bp], in1=masks[:, mprev, :], op=ALU.add),
                             reads=[("ps", bp), "masks"], writes=[("Ssb", sp_)])
                        pp = nxt("P", 4)
                        s.op("act", lambda h: h.activation(out=P[pp], in_=Ssb[sp_], func=AF.Exp, scale=scale), reads=[("Ssb", sp_)], writes=[("P", pp)])
                    obk = 3 + nxt("OB", 4)
                    nmm = []
                    for i in range(4):
                        if has_prev[i]:
                            nmm.append((i, idxs[i] - d, pp, True, False))
                        nmm.append((i, idxs[i], po, not has_prev[i], True))
                    for k_, (i, vb, pt, st_, sp2) in enumerate(nmm):
                        s.op("pe", lambda h, i=i, vb=vb, pt=pt, st_=st_, sp2=sp2: h.matmul(
                            ps[obk][0:65, i * 128:(i + 1) * 128], Vsb[:, vb, 0:65], P[pt][:, i * 128:(i + 1) * 128], start=st_, stop=sp2),
                            reads=[("Vsb", vb // 16), "Vones", ("P", pt)], writes=[("ps", obk)] if k_ in (0, len(nmm) - 1) else [])
                    for i in range(4):
                        sl = slice(base[i], base[i] + 127 * d + 1, d)
                        tl = sorted(set([base[i] // 512, (base[i] + 128 * d - 1) // 512]))
                        toks = [("acc", q_) for q_ in range(tl[0], tl[-1] + 1)]
                        if pidx == 0:
                            s.op("act", lambda h, i=i, sl=sl: h.copy(out=acc[0:65, sl], in_=ps[obk][0:65, i * 128:(i + 1) * 128]),
                                 reads=[("ps", obk)], writes=toks)
                        else:
                            s.op("dve", lambda h, i=i, sl=sl: h.tensor_tensor(out=acc[0:65, sl], in0=ps[obk][0:65, i * 128:(i + 1) * 128],
                                                                              in1=acc[0:65, sl], op=ALU.add),
                                 reads=[("ps", obk)] + toks, writes=toks)
            for pidx, (d, mprev) in enumerate(patterns):
                load_V(vslot, d)
                for g4 in range(NKB // 4):
                    group4(pidx, d, mprev, g4)
            for qt in range(NQT):
                cols = slice(qt * 512, (qt + 1) * 512)
                finalize_norm(acc[0:65, cols], [("acc", qt)], extra_den=(sm[64:65, 10 + j:11 + j] if kind == "D" else None))
                st = nxt("ost", 2)
                s.op("dve", lambda h, cols=cols, st=st: h.tensor_tensor(out=ost[st][0:64], in0=acc[0:64, cols], in1=bcs[0:64], op=ALU.mult),
                     reads=[("acc", qt), "bcs"], writes=[("ost", st)])
                store_o(st, mixer, j, qt)

        for j in range(2):
            banded_head("D", j)
        for j in range(2):
            banded_head("B", j)
        for j in range(2):
            dense_head("C", j)
        for j in range(2):
            dense_head("A", j)

    def phase_C(l, xsrc, xdst, final):
        s.barrier()
        ar.reset()
        moe = (l % 2 == 1)
        NFC = NFCE if moe else NFC0
        nexp = NEXP if moe else 1
        wout = ar.alloc([128, 8, 1024], BF16)
        xs = [ar.alloc([128, 4, 1024], F32) for _ in range(2)]
        oT = [ar.alloc([128, 8, 512], BF16) for _ in range(2)]
        xn = ar.alloc([128, 4, 1024], F32 if moe else BF16)
        junk = ar.alloc([128, 1024], BF16)
        h2T = ar.alloc([128, 8, 512], BF16)
        h2Tf = ar.alloc([128, 8, 512], F32) if moe else None
        actT = ar.alloc([128, NFC, 512], BF16)
        silt = [ar.alloc([128, 512], BF16) for _ in range(2)]
        w13 = [ar.alloc([128, 8, 256], BF16) for _ in range(3)]
        w2 = [ar.alloc([128, 4, 512], BF16) for _ in range(3)]
        ss4 = ar.alloc([128, 4], F32)
        rs4 = ar.alloc([128, 4], F32)
        fn = ar.alloc([128, 1024], F32) if final else None
        if moe:
            rt = ar.alloc([128, 8, 8], F32)
            lg = ar.alloc([128, 4, 8], F32)
            gt = ar.alloc([128, 4, 8], F32)
            t8 = [ar.alloc([128, 8], F32) for _ in range(3)]
            m4 = ar.alloc([128, 8], F32)
        G = gains[l]
        omr = s.group("omy")
        for kc in range(0, 8, 4):
            s.dma("sp", lambda h, kc=kc: h.dma_start(out=wout[:, kc:kc + 4, :], in_=wout_b[l][:, kc:kc + 4, :]),
                  reads=[("wb_wout", l, kc // 4)], writes=[("wout", kc // 4)])
        if moe:
            s.dma("sp", lambda h: h.dma_start(out=rt, in_=router_d), writes=["rt"])
        if final:
            s.dma("sp", lambda h: h.dma_start(out=fn, in_=bass.AP(tensor=fnorm_h, offset=0, ap=[[0, 128], [1, 1024]])), writes=["fn"])

        def load_tile(t):
            b = t % 2
            s.dma("sp", lambda h: h.dma_start(out=xs[b], in_=xsrc[t * 512:(t + 1) * 512, :].rearrange("(b p) d -> p b d", p=128)),
                  writes=[("xs", b, i) for i in range(4)])
            for kc in range(0, 8, 4):
                s.dma("sp", lambda h, kc=kc: h.dma_start(out=oT[b][:, kc:kc + 4, :],
                                                        in_=omy[kc * 128:(kc + 4) * 128, t * 512:(t + 1) * 512].rearrange("(k p) c -> p k c", p=128)),
                      reads=omr, writes=[("oT", b, kc // 4)])

        def w13_src(e, fc):
            return (w13_m_b[e, fc], ("wb_w13_m", e, fc)) if moe else (w13_0_b[fc], ("wb_w13_0", fc // 4))

        def w2_src(e, hf, f0):
            if moe:
                return w2_m_b[e, hf, f0:f0 + 4].rearrange("f p c -> p f c"), ("wb_w2_m", e, hf, f0 // 4), ("wb_w2_m", e, hf, f0 // 4)
            f1 = min(NFC, f0 + 4)
            return w2_0_b[hf, f0:f1].rearrange("f p c -> p f c"), ("wb_w2_0", hf, f0 // 8), ("wb_w2_0", hf, (f1 - 1) // 8)

        def tileC(t):
            b = t % 2
            X = xs[b]
            for blk in range(4):
                for hf in range(2):
                    bank = nxt("CB", 4)
                    mm_group(ps[bank][:, :], [(oT[b][:, kc, blk * 128:(blk + 1) * 128], wout[:, kc, hf * 512:(hf + 1) * 512],
                                               [("oT", b, kc // 4), ("wout", kc // 4)]) for kc in range(8)], ("ps", bank))
                    s.op("dve", lambda h, blk=blk, hf=hf, bank=bank: h.tensor_tensor(
                        out=X[:, blk, hf * 512:(hf + 1) * 512], in0=ps[bank], in1=X[:, blk, hf * 512:(hf + 1) * 512], op=ALU.add),
                        reads=[("ps", bank), ("xs", b, blk)], writes=[("xs", b, blk)])
            s.op("dve", lambda h: h.memset(ss4, 0.0), writes=[("ss4", i) for i in range(4)])
            for blk in range(4):
                s.op("act", lambda h, blk=blk: h.activation(out=junk, in_=X[:, blk, :], func=AF.Square, accum_out=ss4[:, blk:blk + 1]),
                     reads=[("xs", b, blk), ("ss4", blk)], writes=[("ss4", blk)])
            rms_rstd(ss4, rs4, D, [("ss4", i) for i in range(4)], "rs4")
            for blk in range(4):
                s.op("dve", lambda h, blk=blk: h.tensor_scalar(out=xn[:, blk, :], in0=X[:, blk, :], scalar1=rs4[:, blk:blk + 1],
                                                              scalar2=None, op0=ALU.mult),
                     reads=[("xs", b, blk), "rs4"], writes=[("xn", blk)])
            for blk in range(4):
                if not moe:
                    tb = nxt("CB", 4)
                    for kc in range(8):
                        s.op("pe", lambda h, blk=blk, kc=kc, tb=tb: h.transpose(out=psb[tb][:, kc * 128:(kc + 1) * 128],
                                                                                in_=xn[:, blk, kc * 128:(kc + 1) * 128], identity=identb),
                             reads=[("xn", blk), "identb"], writes=[("ps", tb)] if kc in (0, 7) else [])
                    s.op("dve", lambda h, blk=blk, tb=tb: h.tensor_tensor(
                        out=h2T[:, :, blk * 128:(blk + 1) * 128], in0=psb[tb].rearrange("p (k t) -> p k t", k=8),
                        in1=G[:, 8:16].unsqueeze(2).broadcast_to([128, 8, 128]), op=ALU.mult),
                        reads=[("ps", tb), ("gains", l)], writes=[("h2T", blk)])
                else:
                    for q in range(2):
                        tb = nxt("CB", 4)
                        for k4 in range(4):
                            kc = q * 4 + k4
                            s.op("pe", lambda h, blk=blk, kc=kc, k4=k4, tb=tb: h.transpose(out=ps[tb][:, k4 * 128:(k4 + 1) * 128],
                                                                                          in_=xn[:, blk, kc * 128:(kc + 1) * 128], identity=identf),
                                 reads=[("xn", blk), "identf"], writes=[("ps", tb)] if k4 in (0, 3) else [])
                        s.op("dve", lambda h, blk=blk, q=q, tb=tb: h.tensor_tensor(
                            out=h2Tf[:, q * 4:q * 4 + 4, blk * 128:(blk + 1) * 128], in0=ps[tb].rearrange("p (k t) -> p k t", k=4),
                            in1=G[:, 8 + q * 4:12 + q * 4].unsqueeze(2).broadcast_to([128, 4, 128]), op=ALU.mult),
                            reads=[("ps", tb), ("gains", l)], writes=[("h2Tf", blk, q)])
                        s.op("pool", lambda h, blk=blk, q=q: h.tensor_copy(out=h2T[:, q * 4:q * 4 + 4, blk * 128:(blk + 1) * 128],
                                                                          in_=h2Tf[:, q * 4:q * 4 + 4, blk * 128:(blk + 1) * 128]),
                             reads=[("h2Tf", blk, q)], writes=[("h2T", blk)])
                    bank = nxt("CB", 4)
                    mm_group(ps[bank][:, 0:8], [(h2Tf[:, kc, blk * 128:(blk + 1) * 128], rt[:, kc, :], [("h2Tf", blk, kc // 4), "rt"])
                                                for kc in range(8)], ("ps", bank))
                    L = lg[:, blk, :]
                    s.op("act", lambda h, L=L, bank=bank: h.copy(out=L, in_=ps[bank][:, 0:8]), reads=[("ps", bank)], writes=["lg"])
                    s.op("dve", lambda h, L=L: h.reduce_max(out=m4[:, 0:1], in_=L, axis=mybir.AxisListType.X), reads=["lg"], writes=["m4"])
                    s.op("dve", lambda h, L=L: h.tensor_scalar(out=t8[0], in0=L, scalar1=m4[:, 0:1], scalar2=None, op0=ALU.is_equal),
                         reads=["lg", "m4"], writes=["t80"])
                    s.op("dve", lambda h, L=L: h.scalar_tensor_tensor(out=t8[1], in0=t8[0], scalar=NEG, in1=L, op0=ALU.mult, op1=ALU.add),
                         reads=["t80", "lg"], writes=["t81"])
                    s.op("dve", lambda h: h.reduce_max(out=m4[:, 1:2], in_=t8[1], axis=mybir.AxisListType.X), reads=["t81", "m4"], writes=["m4"])
                    s.op("dve", lambda h: h.tensor_scalar(out=t8[2], in0=t8[1], scalar1=m4[:, 1:2], scalar2=None, op0=ALU.is_equal),
                         reads=["t81", "m4"], writes=["t82"])
                    s.op("dve", lambda h: h.tensor_tensor(out=m4[:, 2:3], in0=m4[:, 0:1], in1=m4[:, 1:2], op=ALU.subtract), reads=["m4"], writes=["m4"])
                    s.op("act", lambda h: h.activation(out=m4[:, 3:4], in_=m4[:, 2:3], func=AF.Sigmoid), reads=["m4"], writes=["m4"])
                    s.op("act", lambda h: h.activation(out=m4[:, 4:5], in_=m4[:, 2:3], func=AF.Sigmoid, scale=-1.0), reads=["m4"], writes=["m4"])
                    s.op("dve", lambda h: h.tensor_scalar(out=t8[0], in0=t8[0], scalar1=m4[:, 3:4], scalar2=None, op0=ALU.mult),
                         reads=["t80", "m4"], writes=["t80"])
                    s.op("dve", lambda h, blk=blk: h.scalar_tensor_tensor(out=gt[:, blk, :], in0=t8[2], scalar=m4[:, 4:5], in1=t8[0],
                                                                         op0=ALU.mult, op1=ALU.add),
                         reads=["t80", "t82", "m4"], writes=[("gt", blk)])
            h2r = [("h2T", i) for i in range(4)]
            for e in range(nexp):
                for fc in range(NFC):
                    wi = nxt("w13", 3)
                    src, tokw = w13_src(e, fc)
                    s.dma("sp", lambda h, wi=wi, src=src: h.dma_start(out=w13[wi], in_=src), reads=[tokw], writes=[("w13", wi)])
                    b1 = nxt("CB", 4)
                    mm_group(ps[b1][:, :], [(w13[wi][:, kc, 0:128], h2T[:, kc, :], [("w13", wi)] + h2r) for kc in range(8)], ("ps", b1))
                    b3 = nxt("CB", 4)
                    mm_group(ps[b3][:, :], [(w13[wi][:, kc, 128:256], h2T[:, kc, :], [("w13", wi)] + h2r) for kc in range(8)], ("ps", b3))
                    si = nxt("silt", 2)
                    s.op("act", lambda h, b1=b1, si=si: h.activation(out=silt[si], in_=ps[b1], func=AF.Silu), reads=[("ps", b1)], writes=[("silt", si)])
                    s.op("dve", lambda h, b3=b3, si=si, fc=fc: h.tensor_tensor(out=actT[:, fc, :], in0=ps[b3], in1=silt[si], op=ALU.mult),
                         reads=[("ps", b3), ("silt", si)], writes=[("actT", fc)])
                for hf in range(2):
                    for f0 in range(0, NFC, 4):
                        f1 = min(NFC, f0 + 4)
                        wi = nxt("w2", 3)
                        src, tk0, tk1 = w2_src(e, hf, f0)
                        s.dma("sp", lambda h, wi=wi, src=src, n=f1 - f0: h.dma_start(out=w2[wi][:, 0:n, :], in_=src),
                              reads=[tk0, tk1], writes=[("w2", wi)])
                        for fc in range(f0, f1):
                            for blk in range(4):
                                s.op("pe", lambda h, fc=fc, blk=blk, wi=wi, f0=f0: h.matmul(
                                    ps[4 + blk][:, :], actT[:, fc, blk * 128:(blk + 1) * 128], w2[wi][:, fc - f0, :],
                                    start=(fc == 0), stop=(fc == NFC - 1)),
                                    reads=[("actT", fc), ("w2", wi)], writes=[("ps", 4 + blk)] if fc in (0, NFC - 1) else [])
                    for blk in range(4):
                        if moe:
                            s.op("dve", lambda h, blk=blk, hf=hf, e=e: h.scalar_tensor_tensor(
                                out=X[:, blk, hf * 512:(hf + 1) * 512], in0=ps[4 + blk], scalar=gt[:, blk, e:e + 1],
                                in1=X[:, blk, hf * 512:(hf + 1) * 512], op0=ALU.mult, op1=ALU.add),
                                reads=[("ps", 4 + blk), ("gt", blk), ("xs", b, blk)], writes=[("xs", b, blk)])
                        else:
                            s.op("dve", lambda h, blk=blk, hf=hf: h.tensor_tensor(
                                out=X[:, blk, hf * 512:(hf + 1) * 512], in0=ps[4 + blk], in1=X[:, blk, hf * 512:(hf + 1) * 512], op=ALU.add),
                                reads=[("ps", 4 + blk), ("xs", b, blk)], writes=[("xs", b, blk)])
            xr = [("xs", b, i) for i in range(4)]
            if final:
                s.op("dve", lambda h: h.memset(ss4, 0.0), writes=[("ss4", i) for i in range(4)])
                for blk in range(4):
                    s.op("act", lambda h, blk=blk: h.activation(out=junk, in_=X[:, blk, :], func=AF.Square, accum_out=ss4[:, blk:blk + 1]),
                         reads=[("xs", b, blk), ("ss4", blk)], writes=[("ss4", blk)])
                rms_rstd(ss4, rs4, D, [("ss4", i) for i in range(4)], "rs4")
                for blk in range(4):
                    s.op("dve", lambda h, blk=blk: h.scalar_tensor_tensor(out=X[:, blk, :], in0=X[:, blk, :], scalar=rs4[:, blk:blk + 1], in1=fn,
                                                                         op0=ALU.mult, op1=ALU.mult),
                         reads=[("xs", b, blk), "rs4", "fn"], writes=[("xs", b, blk)])
            s.dma("act", lambda h: h.dma_start(out=xdst[t * 512:(t + 1) * 512, :].rearrange("(b p) d -> p b d", p=128), in_=X),
                  reads=xr, writes=[("xdst", t)])

        load_tile(0)
        for t in range(NTA):
            if t + 1 < NTA:
                load_tile(t + 1)
            tileC(t)

    for l in range(n_layers):
        xsrc = x_in if l == 0 else x1
        phase_A(l, xsrc)
        dump("mine%d" % l, mine, s.group("mine"))
        if stop_after == ("A", l):
            break
        exchange1()
        if l == 0:
            cast_ffn0()
        if l == 0 and n_layers == 2:
            cast_layer_small(1)
            for e in range(NEXP // 2):
                cast_moe(e)
        if l == 1:
            for e in range(NEXP // 2, NEXP):
                cast_moe(e)
        dump("my%d" % l, my, s.group("my"))
        phase_B(l)
        dump("omine%d" % l, omine, s.group("omine"))
        if stop_after == ("B", l):
            break
        exchange2()
        final = (l == n_layers - 1)
        phase_C(l, xsrc, out_d if final else x1, final)
    s.barrier(full=True)
    s.emit()
    return nc


def run(inputs, S, dbg=(), stop_after=None, n_layers=2):
    import time
    t0 = time.time()
    in_maps = _prep(inputs, S)
    t1 = time.time()
    nc = build(S, dbg=dbg, stop_after=stop_after, n_layers=n_layers)
    t2 = time.time()
    if n_layers < 2 or stop_after is not None:
        names = set(_USED)
        in_maps = [{k: v for k, v in m.items() if k in names} for m in in_maps]
    res = run_bass_kernel_spmd(nc, in_maps, core_ids=list(range(len(in_maps))))
    print("prep %.1fs build %.1fs run %.1fs" % (t1 - t0, t2 - t1, time.time() - t2), flush=True)
    return res


def kernel(**inputs):
    x = np.asarray(inputs["x"])
    B, S, _ = x.shape
    res = run(inputs, S)
    T = S // 2
    out = np.empty((B, S, D), np.float32)
    for c in range(2 * B):
        out[c // 2, (c % 2) * T:(c % 2 + 1) * T] = res.results[c]["out"]
    return out
```

```python
import contextlib
import numpy as np
import ml_dtypes
import concourse.bass as bass
import concourse.mybir as mybir
from concourse.bass_utils import run_bass_kernel_spmd

F32 = mybir.dt.float32
BF16 = mybir.dt.bfloat16
AF = mybir.ActivationFunctionType
ALU = mybir.AluOpType

ENGS = ("pe", "act", "dve", "pool", "sp")
DMAQ = ("sp", "pool", "act")

D = 1024
KC = 8
EPS = 1e-6
NEG = -1e30
D_FF = 2816
NFC0 = 22
D_FFE = 3584
NFCE = 28
NEXP = 8
NCOLS = 2624
GROWS = 1504
FMROWS = 1056
R_A = 0
R_B = 256
R_CQ = 512
R_CK = 704
R_KPE = 832
R_DQ = 864
R_DK = 992
VSLOT = {"A0": 0, "A1": 1, "B0": 2, "B1": 3, "C0": 4, "C1": 5, "D": 6}
HMAP_A = [[0, 3], [1, 2]]
INV_A = {0: (0, 0), 3: (0, 1), 1: (1, 0), 2: (1, 1)}
ALIBI_TH = 80.0


class Op:
    __slots__ = ("eng", "fn", "waits", "needed", "idx", "sigval", "kind", "slot", "seq", "q")

    def __init__(self, eng, fn, kind):
        self.eng = eng
        self.fn = fn
        self.kind = kind
        self.waits = []
        self.needed = False
        self.idx = -1
        self.sigval = 0
        self.slot = -1
        self.seq = 0
        self.q = None


class Sched:
    def __init__(self, nc, n_slots=8):
        self.nc = nc
        self.n_slots = n_slots
        self.lists = {e: [] for e in ENGS}
        self.last_write = {}
        self.readers = {}
        self.waited = {e: {} for e in ENGS}
        self.waited_d = {e: {} for e in ENGS}
        self.slots = {q: [None] * n_slots for q in DMAQ}
        self.rr = {q: 0 for q in DMAQ}
        self.rr_bg = 0
        self.cc_count = 0
        self.waited_cc = {e: 0 for e in ENGS}
        self.groups = {}

    def _dep(self, x, d):
        if d is None or d is x:
            return
        if d.kind == "c":
            if x.eng == "pe" and d.eng == "pe":
                return
            w = self.waited[x.eng]
            if w.get(d.eng, -1) >= d.idx:
                return
            w[d.eng] = d.idx
            d.needed = True
            x.waits.append(d)
        elif d.kind == "d":
            key = (d.q, d.slot)
            w = self.waited_d[x.eng]
            if w.get(key, 0) >= d.seq:
                return
            w[key] = d.seq
            x.waits.append(d)
        elif d.kind == "cc":
            if self.waited_cc[x.eng] >= d.seq:
                return
            self.waited_cc[x.eng] = d.seq
            x.waits.append(d)

    def _track(self, op, reads, writes):
        for t in reads:
            self._dep(op, self.last_write.get(t))
        for t in writes:
            self._dep(op, self.last_write.get(t))
            for r in self.readers.get(t, ()):
                self._dep(op, r)
        for t in reads:
            self.readers.setdefault(t, []).append(op)
        for t in writes:
            self.last_write[t] = op
            self.readers[t] = []
            if isinstance(t, tuple):
                self.groups.setdefault(t[0], set()).add(t)

    def group(self, prefix):
        return list(self.groups.get(prefix, ()))

    def _append(self, op):
        lst = self.lists[op.eng]
        op.idx = len(lst)
        lst.append(op)

    def op(self, eng, fn, reads=(), writes=()):
        o = Op(eng, fn, "c")
        self._append(o)
        self._track(o, reads, writes)
        return o

    def dma(self, q, fn, reads=(), writes=(), bg=False):
        o = Op(q, fn, "d")
        o.q = q
        if bg:
            j = self.n_slots - 2 + self.rr_bg
            self.rr_bg = (self.rr_bg + 1) % 2
        else:
            j = self.rr[q]
            self.rr[q] = (j + 1) % (self.n_slots - 2 if q == "pool" else self.n_slots)
        prev = self.slots[q][j]
        o.slot = j
        o.seq = (prev.seq + 1) if prev is not None else 1
        self._append(o)
        if prev is not None:
            self._dep(o, prev)
        self.slots[q][j] = o
        self._track(o, reads, writes)
        return o

    def collective(self, fn, reads=(), writes=()):
        o = Op("pool", fn, "cc")
        self.cc_count += 1
        o.seq = self.cc_count
        self._append(o)
        self._track(o, reads, writes)
        return o

    def barrier(self, full=False):
        lasts = []
        for e in ENGS:
            for o in reversed(self.lists[e]):
                if o.kind == "c":
                    lasts.append(o)
                    break
        dl = [o for q in DMAQ if (full or q != "pool") for o in self.slots[q] if o is not None]
        for e in ENGS:
            m = Op(e, None, "m")
            self._append(m)
            for d in lasts:
                if d.eng != e:
                    self._dep(m, d)
            for d in dl:
                self._dep(m, d)
            if self.cc_count:
                cc = Op("pool", None, "cc")
                cc.seq = self.cc_count
                self._dep(m, cc)

    def emit(self):
        nc = self.nc
        for e in ENGS:
            c = 0
            for o in self.lists[e]:
                if o.kind == "c" and o.needed:
                    c += 1
                    o.sigval = c
        with contextlib.ExitStack() as st:
            sem = {e: st.enter_context(nc.semaphore("s_" + e)) for e in ENGS}
            dsem = {q: [st.enter_context(nc.semaphore("d_%s%d" % (q, j))) for j in range(self.n_slots)]
                    for q in DMAQ}
            ccsem = st.enter_context(nc.semaphore("s_cc"))
            block = st.enter_context(nc.Block())
            sched = self

            def run(e, h):
                for o in sched.lists[e]:
                    for d in o.waits:
                        if d.kind == "c":
                            h.wait_ge(sem[d.eng], d.sigval)
                        elif d.kind == "d":
                            h.wait_ge(dsem[d.q][d.slot], 16 * d.seq)
                        else:
                            h.wait_ge(ccsem, d.seq)
                    if o.fn is None:
                        continue
                    ins = o.fn(h)
                    if o.kind == "d":
                        ins.then_inc(dsem[o.q][o.slot], 16)
                    elif o.kind == "cc":
                        ins.then_inc(ccsem)
                    elif o.needed:
                        ins.then_inc(sem[e], 1)

            @block.tensor
            def _(h):
                run("pe", h)

            @block.scalar
            def _(h):
                run("act", h)

            @block.vector
            def _(h):
                run("dve", h)

            @block.gpsimd
            def _(h):
                run("pool", h)

            @block.sync
            def _(h):
                run("sp", h)


def _split3(v):
    v = np.asarray(v, np.float64)
    out = []
    rem = v.copy()
    for _ in range(3):
        a = rem.astype(np.float32).astype(ml_dtypes.bfloat16).astype(np.float64)
        out.append(a.astype(np.float32))
        rem = rem - a
    return out


def _alibi_slopes():
    s = (2.0 ** (-8.0 * (np.arange(12) + 1) / 12)).astype(np.float32).astype(np.float64)
    return s[0::3], s[1::3], s[2::3]


def _win_cols():
    A_Q, A_K, A_V, B_Q, B_K, B_V = 0, 256, 512, 768, 1024, 1280
    C_Q, C_KV, C_PE, D_Q, D_K, D_V = 1536, 1920, 2048, 2080, 2336, 2464
    r = lambda a, n: list(range(a, a + n))
    cols = []
    for h in range(4):
        cols += r(A_Q + 64 * h, 64) + r(A_K + 64 * h, 64)
    for h in range(4):
        cols += r(B_Q + 64 * h, 64) + r(B_K + 64 * h, 64)
    cols += r(D_Q, 256)
    cols += r(D_K, 128)
    cols += r(C_PE, 32)
    cols += r(C_PE + 16, 16) + r(C_PE, 16)
    cols += r(C_Q, 384) + r(C_KV, 128)
    cols += r(A_V, 256) + r(B_V, 256)
    cols += r(D_V, 128)
    assert len(cols) == NCOLS
    return np.array(cols)


CO_A = 0
CO_B = 512
CO_DQ = 1024
CO_DK = 1280
CO_KPE = 1408
CO_KPES = 1440
CO_TA = 1472
CO_TB = 1984
CO_TC = 2496


def _pk(w, ncol):
    k = w.shape[0] // 128
    return np.ascontiguousarray(w.reshape(k, 128, ncol).transpose(1, 0, 2))


def _consts(S):
    masks = np.zeros((7, 128, 512), np.float32)
    j = np.arange(128)[:, None]
    c = np.arange(512)[None, :]
    for v in range(4):
        masks[v] = np.where(c >= 128 * v + j, 0.0, NEG)
    m = c % 128
    masks[4] = np.where(j <= m, 0.0, NEG)
    masks[5] = np.where(j > m, 0.0, NEG)
    masks[6] = np.where(j >= m, 0.0, NEG)
    return masks


def _prep(inputs, S):
    B = inputs["x"].shape[0]
    T = S // 2
    f32 = lambda a: np.ascontiguousarray(np.asarray(a, np.float32))
    sl_a, sl_b, sl_d = _alibi_slopes()
    cols = _win_cols()
    shared = {"ident": np.eye(128, dtype=np.float32), "masks": _consts(S)}
    for l in range(2):
        shared["win%d" % l] = _pk(f32(inputs["w_in"][l])[:, cols], NCOLS)
        wo = f32(inputs["w_out"][l])
        ridx = list(range(1024))
        for rank in range(2):
            for j in range(2):
                hh = HMAP_A[rank][j]
                ridx[rank * 128 + j * 64: rank * 128 + (j + 1) * 64] = list(range(hh * 64, hh * 64 + 64))
        shared["wout%d" % l] = _pk(wo[ridx], 1024)
        wq = f32(inputs["mla_w_uq"][l])
        qc = []
        for h in range(4):
            qc += list(range(h * 96, h * 96 + 96))
        for h in range(4):
            qc += list(range(h * 96, h * 96 + 64)) + list(range(h * 96 + 80, h * 96 + 96)) + list(range(h * 96 + 64, h * 96 + 80))
        shared["wuq%d" % l] = _pk(wq[:, qc], 768)
        wkv = f32(inputs["mla_w_ukv"][l])
        kc_ = []
        for h in range(4):
            kc_ += list(range(h * 128, h * 128 + 64))
        for h in range(4):
            kc_ += list(range(h * 128 + 64, h * 128 + 128))
        shared["wukv%d" % l] = np.ascontiguousarray(wkv[:, kc_])
        g = np.zeros((128, 24), np.float32)
        g[:, 0:8] = f32(inputs["attn_norm"][l]).reshape(8, 128).T
        g[:, 8:16] = f32(inputs["ffn_norm"][l]).reshape(8, 128).T
        g[:, 16:19] = f32(inputs["mla_q_norm"][l]).reshape(3, 128).T
        g[:, 19] = f32(inputs["mla_kv_norm"][l])
        g[:, 20] = np.tile(f32(inputs["diff_subln"][l]), 2)
        shared["gains%d" % l] = g
        shared["dlam%d" % l] = f32(inputs["diff_lambda"][l]).reshape(1, 128)
    shared["fnorm"] = f32(inputs["final_norm"]).reshape(1, 1024)
    w1, w3, w2 = f32(inputs["ffn_w1"][0]), f32(inputs["ffn_w3"][0]), f32(inputs["ffn_w2"][0])

    def pack13(a, b, nfc):
        a = a.reshape(8, 128, nfc, 128).transpose(2, 1, 0, 3)
        b = b.reshape(8, 128, nfc, 128).transpose(2, 1, 0, 3)
        return np.ascontiguousarray(np.concatenate([a, b], axis=3))

    def pack2(a, nfc):
        return np.ascontiguousarray(a.reshape(nfc, 128, 2, 512).transpose(2, 0, 1, 3))

    shared["w13_0"] = pack13(w1, w3, NFC0)
    shared["w2_0"] = pack2(w2, NFC0)
    m1, m3, m2 = inputs["moe_w1"][0], inputs["moe_w3"][0], inputs["moe_w2"][0]
    shared["w13_m"] = np.stack([pack13(f32(m1[e]), f32(m3[e]), NFCE) for e in range(NEXP)])
    shared["w2_m"] = np.stack([pack2(f32(m2[e]), NFCE) for e in range(NEXP)])
    shared["router"] = _pk(f32(inputs["moe_router"][0]), 8)

    pos = np.arange(S, dtype=np.float64)
    inv = (10000.0 ** (-np.arange(0, 32, 2, dtype=np.float32) / 32)).astype(np.float32)
    ang = np.arange(S, dtype=np.float32)[:, None] * inv[None, :]
    cos, sin = np.cos(ang).astype(np.float32), np.sin(ang).astype(np.float32)
    rope_full = np.stack([np.concatenate([cos, cos], 1).T, np.concatenate([-sin, sin], 1).T])

    def aug(slope, scale):
        v = slope * pos / scale
        q = [-a for a in _split3(v)] + [np.ones(S, np.float32)] * 3
        k = [np.ones(S, np.float32)] * 3 + _split3(v)
        return np.stack(q).astype(np.float32), np.stack(k).astype(np.float32)

    in_maps = []
    x = f32(inputs["x"])
    for c in range(2 * B):
        b, r = c // 2, c % 2
        m = dict(shared)
        m["x"] = np.ascontiguousarray(x[b, r * T:(r + 1) * T])
        m["rope"] = np.ascontiguousarray(rope_full[:, :, r * T:(r + 1) * T])
        qa, ka = [], []
        for j in range(2):
            q_, k_ = aug(sl_a[HMAP_A[r][j]], 32 ** -0.5); qa.append(q_); ka.append(k_)
        for j in range(2):
            q_, k_ = aug(sl_b[2 * r + j], 64 ** -0.5); qa.append(q_); ka.append(k_)
        for j in range(2):
            q_, k_ = aug(sl_d[2 * r + j], 64 ** -0.5); qa.append(q_); ka.append(k_)
        m["qaug"] = np.stack(qa)
        m["kaug"] = np.stack(ka)
        for l in range(2):
            m["sinks%d" % l] = f32(inputs["swa_sinks"][l])[2 * r:2 * r + 2].reshape(1, 2)
        in_maps.append(m)
    return in_maps


_USED = []


class Arena:
    def __init__(self, ap, nelem):
        self.ap = ap
        self.n = nelem
        self.off = 0
        self.base = 0

    def alloc(self, shape, dt):
        per = 1
        for d_ in shape[1:]:
            per *= d_
        size = per * (2 if dt == F32 else 1)
        off = self.off + (self.off % 2)
        assert off + size <= self.n, ("arena overflow", off, size, self.n)
        v = self.ap[:, off:off + size]
        if dt == F32:
            v = v.bitcast(F32)
        if len(shape) == 3:
            v = v.rearrange("p (a b) -> p a b", a=shape[1])
        elif len(shape) == 4:
            v = v.rearrange("p (a b c) -> p a b c", a=shape[1], b=shape[2])
        self.off = off + size
        return v

    def persist(self):
        self.base = self.off

    def reset(self):
        self.off = self.base


def build(S, dbg=(), stop_after=None, n_layers=2):
    T = S // 2
    NTA = T // 512
    NQT = S // 512
    NKB = S // 128
    NBT = T // 128
    nc = bass.Bass("TRN2", target_bir_lowering=False)
    RG = [[0, 1], [2, 3], [4, 5], [6, 7]]

    _USED.clear()
    full = (n_layers == 2 and stop_after is None)

    def ein(name, shape, dt=F32):
        if not full and name in ("w13_m", "w2_m", "router"):
            return nc.dram_tensor(name + "_unused", list(shape), dt)
        _USED.append(name)
        return nc.dram_tensor(name, list(shape), dt, kind="ExternalInput")

    def scr(name, shape, dt):
        return nc.dram_tensor(name, list(shape), dt)

    x_in = ein("x", [T, D]).ap()
    ident_d = ein("ident", [128, 128]).ap()
    masks_d = ein("masks", [7, 128, 512]).ap()
    rope_d = ein("rope", [2, 32, T]).ap()
    qaug_d = ein("qaug", [6, 6, S]).ap()
    kaug_d = ein("kaug", [6, 6, S]).ap()
    fnorm_h = ein("fnorm", [1, 1024])
    win_d = [ein("win%d" % l, [128, 8, NCOLS]).ap() for l in range(2)]
    wout_d = [ein("wout%d" % l, [128, 8, 1024]).ap() for l in range(2)]
    wuq_d = [ein("wuq%d" % l, [128, 3, 768]).ap() for l in range(2)]
    wukv_d = [ein("wukv%d" % l, [128, 512]).ap() for l in range(2)]
    gains_d = [ein("gains%d" % l, [128, 24]).ap() for l in range(2)]
    dlam_h = [ein("dlam%d" % l, [1, 128]) for l in range(2)]
    sinks_h = [ein("sinks%d" % l, [1, 2]) for l in range(2)]
    w13_0_d = ein("w13_0", [NFC0, 128, 8, 256]).ap()
    w2_0_d = ein("w2_0", [2, NFC0, 128, 512]).ap()
    w13_m_d = ein("w13_m", [NEXP, NFCE, 128, 8, 256]).ap()
    w2_m_d = ein("w2_m", [NEXP, 2, NFCE, 128, 512]).ap()
    router_d = ein("router", [128, 8, 8]).ap()
    out_d = nc.dram_tensor("out", [T, D], F32, kind="ExternalOutput").ap()

    win_b = [scr("win_b%d" % l, [128, 8, NCOLS], BF16).ap() for l in range(2)]
    wout_b = [scr("wout_b%d" % l, [128, 8, 1024], BF16).ap() for l in range(2)]
    wuq_b = [scr("wuq_b%d" % l, [128, 3, 768], BF16).ap() for l in range(2)]
    wukv_b = [scr("wukv_b%d" % l, [128, 512], BF16).ap() for l in range(2)]
    w13_0_b = scr("w13_0_b", [NFC0, 128, 8, 256], BF16).ap()
    w2_0_b = scr("w2_0_b", [2, NFC0, 128, 512], BF16).ap()
    w13_m_b = scr("w13_m_b", [NEXP, NFCE, 128, 8, 256], BF16).ap()
    w2_m_b = scr("w2_m_b", [NEXP, 2, NFCE, 128, 512], BF16).ap()
    qaug_b = scr("qaug_b", [6, 6, S], BF16).ap()
    kaug_b = scr("kaug_b", [6, 6, S], BF16).ap()
    mine_h = scr("qkv_mine", [2 * GROWS, T], BF16)
    allq_h = scr("qkv_all", [4 * GROWS, T], BF16)
    my_h = scr("qkv_my", [2 * GROWS, T], BF16)
    mine, allq, my = mine_h.ap(), allq_h.ap(), my_h.ap()
    omine = scr("o_mine", [512, S], BF16).ap()
    oall = scr("o_all", [1024, S], BF16).ap()
    omy = scr("o_my", [1024, T], BF16).ap()
    x1 = scr("x1", [T, D], F32).ap()

    dbg_out = {}
    for name, shape, dt in dbg:
        dbg_out[name] = nc.dram_tensor("dbg_" + name, list(shape), dt, kind="ExternalOutput").ap()

    ARENA = 94208
    arena_t = nc.alloc_sbuf_tensor("arena", [128, ARENA], BF16)
    ar = Arena(arena_t.ap(), ARENA)
    ps = [nc.alloc_psum_tensor("ps%d" % i, [128, 512], F32).ap() for i in range(8)]
    psb = [p.bitcast(BF16) for p in ps]

    s = Sched(nc)

    identf = ar.alloc([128, 128], F32)
    identb = ar.alloc([128, 128], BF16)
    masks = ar.alloc([128, 7, 512], F32)
    onesf = ar.alloc([128, 128], F32)
    gains = [ar.alloc([128, 24], F32) for _ in range(2)]
    ar.persist()

    s.dma("sp", lambda h: h.dma_start(out=identf, in_=ident_d), writes=["identf"])
    s.dma("pool", lambda h: h.dma_start(out=identb, in_=ident_d), writes=["identb"])
    s.dma("sp", lambda h: h.dma_start(out=masks, in_=masks_d.rearrange("v p c -> p v c")), writes=["masks"])
    s.op("pool", lambda h: h.memset(onesf, 1.0), writes=["onesf"])
    for l in range(2):
        s.dma("sp", lambda h, l=l: h.dma_start(out=gains[l], in_=gains_d[l]), writes=[("gains", l)])

    def cast(dst, src, tok, bg=False):
        s.dma("pool", lambda h: h.dma_start(out=dst, in_=src), writes=[tok], bg=bg)

    def cast_layer_small(l):
        for kc in range(8):
            cast(win_b[l][:, kc, :], win_d[l][:, kc, :], ("wb_win", l, kc))
        cast(wuq_b[l], wuq_d[l], ("wb_wuq", l))
        cast(wukv_b[l], wukv_d[l], ("wb_wukv", l))
        for kc in range(0, 8, 4):
            cast(wout_b[l][:, kc:kc + 4, :], wout_d[l][:, kc:kc + 4, :], ("wb_wout", l, kc // 4))

    def cast_ffn0():
        for f0 in range(0, NFC0, 4):
            f1 = min(NFC0, f0 + 4)
            cast(w13_0_b[f0:f1], w13_0_d[f0:f1], ("wb_w13_0", f0 // 4), bg=True)
        for hf in range(2):
            for f0 in range(0, NFC0, 8):
                f1 = min(NFC0, f0 + 8)
                cast(w2_0_b[hf, f0:f1], w2_0_d[hf, f0:f1], ("wb_w2_0", hf, f0 // 8), bg=True)

    def cast_moe(e):
        for f0 in range(0, NFCE):
            cast(w13_m_b[e, f0:f0 + 1], w13_m_d[e, f0:f0 + 1], ("wb_w13_m", e, f0), bg=True)
        for hf in range(2):
            for f0 in range(0, NFCE, 4):
                cast(w2_m_b[e, hf, f0:f0 + 4], w2_m_d[e, hf, f0:f0 + 4], ("wb_w2_m", e, hf, f0 // 4), bg=True)

    cast(qaug_b, qaug_d, "qaug_b")
    cast(kaug_b, kaug_d, "kaug_b")
    cast_layer_small(0)

    rot = {}

    def nxt(key, n):
        v = rot.get(key, 0)
        rot[key] = (v + 1) % n
        return v

    def mm_group(out, parts, tok):
        n = len(parts)
        for i, (l_, r_, rd) in enumerate(parts):
            s.op("pe", lambda h, l_=l_, r_=r_, i=i: h.matmul(out, l_, r_, start=(i == 0), stop=(i == n - 1)),
                 reads=list(rd), writes=[tok] if i in (0, n - 1) else [])

    def dump(name, src, reads):
        if name in dbg_out:
            s.dma("sp", lambda h: h.dma_start(out=dbg_out[name], in_=src), reads=reads)

    def rms_rstd(ss_ap, out_ap, n, toks_in, tok_out):
        s.op("act", lambda h: h.activation(out=out_ap, in_=ss_ap, func=AF.Sqrt, scale=1.0 / n, bias=EPS),
             reads=toks_in, writes=[tok_out])
        s.op("dve", lambda h: h.reciprocal(out=out_ap, in_=out_ap), reads=[tok_out], writes=[tok_out])

    def phase_A(l, xsrc):
        s.barrier()
        ar.reset()
        win = ar.alloc([128, 8, NCOLS], BF16)
        wuq = ar.alloc([128, 3, 768], BF16)
        wukv = ar.alloc([128, 512], BF16)
        xt = [ar.alloc([128, 4, 1024], F32) for _ in range(2)]
        xn = ar.alloc([128, 4, 1024], BF16)
        junk = ar.alloc([128, 1024], BF16)
        hT = [ar.alloc([128, 8, 512], BF16) for _ in range(2)]
        ss4 = ar.alloc([128, 4], F32)
        rs4 = ar.alloc([128, 4], F32)
        ssm = ar.alloc([128, 4, 2], F32)
        rsm = ar.alloc([128, 4, 2], F32)
        cn = [ar.alloc([128, 512], BF16) for _ in range(2)]
        cT = ar.alloc([128, 4, 512], BF16)
        fmst = [ar.alloc([128, 512], BF16) for _ in range(4)]
        vst = ar.alloc([128, 4, 896], BF16)
        ropeK = ar.alloc([128, 2, 512], F32)
        ropeQ = ar.alloc([128, 2, 512], F32)
        rtmp = [ar.alloc([128, 512], F32) for _ in range(2)]
        G = gains[l]
        for kc in range(8):
            s.dma("sp", lambda h, kc=kc: h.dma_start(out=win[:, kc, :], in_=win_b[l][:, kc, :]),
                  reads=[("wb_win", l, kc)], writes=[("win", kc)])
        s.dma("sp", lambda h: h.dma_start(out=wuq, in_=wuq_b[l]), reads=[("wb_wuq", l)], writes=["wuq"])
        s.dma("sp", lambda h: h.dma_start(out=wukv, in_=wukv_b[l]), reads=[("wb_wukv", l)], writes=["wukv"])
        winr = [("win", kc) for kc in range(8)]

        def load_x(t):
            b = t % 2
            s.dma("sp", lambda h: h.dma_start(out=xt[b], in_=xsrc[t * 512:(t + 1) * 512, :].rearrange("(b p) d -> p b d", p=128)),
                  reads=[("xsrc", t)], writes=[("xt", b)])

        def store_fm(src_ap, nrows, g, row, t, rd):
            dst = mine[g * GROWS + row: g * GROWS + row + nrows, t * 512:(t + 1) * 512]
            s.dma("act", lambda h: h.dma_start(out=dst, in_=src_ap), reads=rd, writes=[("mine", g, row, t)])

        def evac(bank_ap, dst_ap, rd, wr, k=[0]):
            k[0] += 1
            if k[0] % 2:
                s.op("act", lambda h: h.copy(out=dst_ap, in_=bank_ap), reads=rd, writes=wr)
            else:
                s.op("dve", lambda h: h.tensor_copy(out=dst_ap, in_=bank_ap), reads=rd, writes=wr)

        def tileA(t):
            b = t % 2
            X = xt[b]
            s.dma("sp", lambda h, t=t: h.dma_start(out=ropeK[0:32], in_=rope_d[:, :, t * 512:(t + 1) * 512].rearrange("a r c -> r a c")),
                  writes=["ropeK"])
            s.dma("sp", lambda h, t=t: h.dma_start(out=ropeQ[64:96], in_=rope_d[:, :, t * 512:(t + 1) * 512].rearrange("a r c -> r a c")),
                  writes=["ropeQ"])
            s.op("dve", lambda h: h.memset(ss4, 0.0), writes=[("ss4", i) for i in range(4)])
            for blk in range(4):
                s.op("act", lambda h, blk=blk: h.activation(out=junk, in_=X[:, blk, :], func=AF.Square, accum_out=ss4[:, blk:blk + 1]),
                     reads=[("xt", b), ("ss4", blk)], writes=[("ss4", blk)])
            rms_rstd(ss4, rs4, D, [("ss4", i) for i in range(4)], "rs4")
            for blk in range(4):
                s.op("dve", lambda h, blk=blk: h.tensor_scalar(out=xn[:, blk, :], in0=X[:, blk, :], scalar1=rs4[:, blk:blk + 1],
                                                              scalar2=None, op0=ALU.mult),
                     reads=[("xt", b), "rs4"], writes=[("xn", blk)])
            for blk in range(4):
                tb = nxt("TB", 2)
                for kc in range(8):
                    s.op("pe", lambda h, blk=blk, kc=kc, tb=tb: h.transpose(out=psb[tb][:, kc * 128:(kc + 1) * 128],
                                                                            in_=xn[:, blk, kc * 128:(kc + 1) * 128], identity=identb),
                         reads=[("xn", blk), "identb"], writes=[("ps", tb)] if kc in (0, 7) else [])
                s.op("dve", lambda h, blk=blk, tb=tb: h.tensor_tensor(
                    out=hT[b][:, :, blk * 128:(blk + 1) * 128], in0=psb[tb].rearrange("p (k t) -> p k t", k=8),
                    in1=G[:, 0:8].unsqueeze(2).broadcast_to([128, 8, 128]), op=ALU.mult),
                    reads=[("ps", tb), ("gains", l)], writes=[("hT", b, blk)])
            hTr = [("hT", b, i) for i in range(4)]

            def fm_group(co, M):
                bank = 2 + nxt("FM", 3)
                mm_group(ps[bank][0:M, :], [(win[:, kc, co:co + M], hT[b][:, kc, :], winr[kc:kc + 1] + hTr) for kc in range(8)],
                         ("ps", bank))
                return bank

            for hh in range(4):
                bank = fm_group(CO_A + hh * 128, 128)
                st = nxt("fmst", 4)
                evac(ps[bank], fmst[st], [("ps", bank)], [("fmst", st)])
                store_fm(fmst[st], 128, INV_A[hh][0], R_A + INV_A[hh][1] * 128, t, [("fmst", st)])
            for hh in range(4):
                bank = fm_group(CO_B + hh * 128, 128)
                st = nxt("fmst", 4)
                evac(ps[bank], fmst[st], [("ps", bank)], [("fmst", st)])
                store_fm(fmst[st], 128, hh // 2, R_B + (hh % 2) * 128, t, [("fmst", st)])
            for g in range(2):
                bank = fm_group(CO_DQ + g * 128, 128)
                st = nxt("fmst", 4)
                evac(ps[bank], fmst[st], [("ps", bank)], [("fmst", st)])
                store_fm(fmst[st], 128, g, R_DQ, t, [("fmst", st)])
            bank = fm_group(CO_DK, 128)
            st = nxt("fmst", 4)
            evac(ps[bank], fmst[st], [("ps", bank)], [("fmst", st)])
            for g in range(2):
                store_fm(fmst[st][g * 64:(g + 1) * 64], 64, g, R_DK, t, [("fmst", st)])
            b1 = fm_group(CO_KPE, 32)
            b2 = fm_group(CO_KPES, 32)
            s.op("dve", lambda h, b1=b1: h.tensor_tensor(out=rtmp[0][0:32], in0=ps[b1][0:32], in1=ropeK[0:32, 0, :], op=ALU.mult),
                 reads=[("ps", b1), "ropeK"], writes=["rtmp0"])
            s.op("dve", lambda h, b2=b2: h.tensor_tensor(out=rtmp[1][0:32], in0=ps[b2][0:32], in1=ropeK[0:32, 1, :], op=ALU.mult),
                 reads=[("ps", b2), "ropeK"], writes=["rtmp1"])
            st = nxt("fmst", 4)
            s.op("dve", lambda h, st=st: h.tensor_tensor(out=fmst[st][0:32], in0=rtmp[0][0:32], in1=rtmp[1][0:32], op=ALU.add),
                 reads=["rtmp0", "rtmp1"], writes=[("fmst", st)])
            for g in range(2):
                store_fm(fmst[st][0:32], 32, g, R_KPE, t, [("fmst", st)])
            for blk in range(4):
                def tm_group(co, N):
                    bank = 5 + nxt("TM", 3)
                    mm_group(ps[bank][:, 0:N], [(hT[b][:, kc, blk * 128:(blk + 1) * 128], win[:, kc, co:co + N],
                                                 winr[kc:kc + 1] + [("hT", b, blk)]) for kc in range(8)], ("ps", bank))
                    return bank
                bk = tm_group(CO_TB, 512)
                evac(ps[bk], vst[:, blk, 0:512], [("ps", bk)], [("vst", blk, 0)])
                bk = tm_group(CO_TC, 128)
                evac(ps[bk][:, 0:128], vst[:, blk, 768:896], [("ps", bk)], [("vst", blk, 2)])
                bk = tm_group(CO_TA, 512)
                s.op("dve", lambda h, blk=blk: h.memset(ssm[:, blk, :], 0.0), writes=[("ssm", blk)])
                s.op("act", lambda h, blk=blk, bk=bk: h.activation(out=junk[:, 0:384], in_=ps[bk][:, 0:384], func=AF.Square,
                                                                  accum_out=ssm[:, blk, 0:1]),
                     reads=[("ps", bk), ("ssm", blk)], writes=[("ssm", blk)])
                s.op("act", lambda h, blk=blk, bk=bk: h.activation(out=junk[:, 384:512], in_=ps[bk][:, 384:512], func=AF.Square,
                                                                  accum_out=ssm[:, blk, 1:2]),
                     reads=[("ps", bk), ("ssm", blk)], writes=[("ssm", blk)])
                s.op("act", lambda h, blk=blk: h.activation(out=rsm[:, blk, 0:1], in_=ssm[:, blk, 0:1], func=AF.Sqrt, scale=1.0 / 384, bias=EPS),
                     reads=[("ssm", blk)], writes=[("rsm", blk)])
                s.op("act", lambda h, blk=blk: h.activation(out=rsm[:, blk, 1:2], in_=ssm[:, blk, 1:2], func=AF.Sqrt, scale=1.0 / 128, bias=EPS),
                     reads=[("ssm", blk)], writes=[("rsm", blk)])
                s.op("dve", lambda h, blk=blk: h.reciprocal(out=rsm[:, blk, :], in_=rsm[:, blk, :]), reads=[("rsm", blk)], writes=[("rsm", blk)])
                ci = nxt("cn", 2)
                s.op("dve", lambda h, blk=blk, bk=bk, ci=ci: h.tensor_scalar(out=cn[ci][:, 0:384], in0=ps[bk][:, 0:384], scalar1=rsm[:, blk, 0:1],
                                                                            scalar2=None, op0=ALU.mult),
                     reads=[("ps", bk), ("rsm", blk)], writes=[("cn", ci)])
                s.op("dve", lambda h, blk=blk, bk=bk, ci=ci: h.tensor_scalar(out=cn[ci][:, 384:512], in0=ps[bk][:, 384:512], scalar1=rsm[:, blk, 1:2],
                                                                            scalar2=None, op0=ALU.mult),
                     reads=[("ps", bk), ("rsm", blk)], writes=[("cn", ci)])
                tb = nxt("TB", 2)
                for i in range(4):
                    s.op("pe", lambda h, i=i, tb=tb, ci=ci: h.transpose(out=psb[tb][:, i * 128:(i + 1) * 128], in_=cn[ci][:, i * 128:(i + 1) * 128],
                                                                       identity=identb),
                         reads=[("cn", ci), "identb"], writes=[("ps", tb)] if i in (0, 3) else [])
                s.op("dve", lambda h, blk=blk, tb=tb: h.tensor_tensor(
                    out=cT[:, :, blk * 128:(blk + 1) * 128], in0=psb[tb][:, 0:512].rearrange("p (k t) -> p k t", k=4),
                    in1=G[:, 16:20].unsqueeze(2).broadcast_to([128, 4, 128]), op=ALU.mult),
                    reads=[("ps", tb), ("gains", l)], writes=[("cT", blk)])
                bank = 5 + nxt("TM", 3)
                mm_group(ps[bank][:, 0:256], [(cT[:, 3, blk * 128:(blk + 1) * 128], wukv[:, 256:512], ["wukv", ("cT", blk)])], ("ps", bank))
                evac(ps[bank][:, 0:256], vst[:, blk, 512:768], [("ps", bank)], [("vst", blk, 1)])
            cTr = [("cT", i) for i in range(4)]
            vr = [("vst", i, k) for i in range(4) for k in range(3)]
            for g in range(2):
                for name, slot in VSLOT.items():
                    if name[0] == "D":
                        col = 768 + g * 64
                    elif name[0] == "A":
                        col = HMAP_A[g][int(name[1])] * 64
                    else:
                        col = {"A": 0, "B": 256, "C": 512}[name[0]] + (2 * g + int(name[1])) * 64
                    dst = bass.AP(tensor=mine_h, offset=(g * GROWS + FMROWS + slot * 64) * T + t * 512 * 64,
                                  ap=[[64, 128], [128 * 64, 4], [1, 64]])
                    s.dma("act", lambda h, dst=dst, col=col: h.dma_start(out=dst, in_=vst[:, :, col:col + 64]),
                          reads=vr, writes=[("mine", g, "v", slot, t)])
            for hh in range(4):
                bo = 2 + nxt("FM", 3)
                mm_group(ps[bo][0:96, :], [(wuq[:, kc, hh * 96:(hh + 1) * 96], cT[:, kc, :], ["wuq"] + cTr) for kc in range(3)], ("ps", bo))
                bs = 2 + nxt("FM", 3)
                mm_group(ps[bs][0:96, :], [(wuq[:, kc, 384 + hh * 96:384 + (hh + 1) * 96], cT[:, kc, :], ["wuq"] + cTr) for kc in range(3)], ("ps", bs))
                st = nxt("fmst", 4)
                s.op("act", lambda h, bo=bo, st=st: h.copy(out=fmst[st][0:64], in_=ps[bo][0:64]), reads=[("ps", bo)], writes=[("fmst", st)])
                s.op("dve", lambda h, bo=bo: h.tensor_tensor(out=rtmp[0][64:96], in0=ps[bo][64:96], in1=ropeQ[64:96, 0, :], op=ALU.mult),
                     reads=[("ps", bo), "ropeQ"], writes=["rtmp0"])
                s.op("dve", lambda h, bs=bs: h.tensor_tensor(out=rtmp[1][64:96], in0=ps[bs][64:96], in1=ropeQ[64:96, 1, :], op=ALU.mult),
                     reads=[("ps", bs), "ropeQ"], writes=["rtmp1"])
                s.op("dve", lambda h, st=st: h.tensor_tensor(out=fmst[st][64:96], in0=rtmp[0][64:96], in1=rtmp[1][64:96], op=ALU.add),
                     reads=["rtmp0", "rtmp1", ("fmst", st)], writes=[("fmst", st)])
                store_fm(fmst[st][0:96], 96, hh // 2, R_CQ + (hh % 2) * 96, t, [("fmst", st)])
            for g in range(2):
                bank = 2 + nxt("FM", 3)
                mm_group(ps[bank][:, :], [(wukv[:, g * 128:(g + 1) * 128], cT[:, 3, :], ["wukv"] + cTr)], ("ps", bank))
                st = nxt("fmst", 4)
                evac(ps[bank], fmst[st], [("ps", bank)], [("fmst", st)])
                store_fm(fmst[st], 128, g, R_CK, t, [("fmst", st)])

        load_x(0)
        for t in range(NTA):
            if t + 1 < NTA:
                load_x(t + 1)
            tileA(t)

    rank_cache = {}

    def rank_of(h):
        if "r" not in rank_cache:
            rank_cache["r"] = h.partition_id() % 2
        return rank_cache["r"]

    CR = GROWS // 8
    NCG = 8

    def exchange1():
        toks = []
        for k in range(2 * NCG):
            g_, kk = k // NCG, k % NCG
            rows = slice(g_ * GROWS + kk * CR, g_ * GROWS + (kk + 1) * CR)
            mt = [t_ for t_ in s.group("mine")]
            s.collective(lambda h, k=k, rows=rows: h.collective_compute(
                "AllGather", ALU.bypass, replica_groups=RG, ins=[mine[rows, :]], outs=[allq[k * 2 * CR:(k + 1) * 2 * CR, :]]),
                reads=mt, writes=[("allq", k)])
            toks.append(("allq", k))
        for hf in range(2):
            def f(h, hf=hf):
                r = rank_of(h)
                src = allq[bass.ds(r * (NCG * 2 * CR), NCG * 2 * CR), :].rearrange("(k two c) t -> k two c t", two=2, c=CR)[:, hf]
                dst = my[hf * GROWS:(hf + 1) * GROWS, :].rearrange("(k c) t -> k c t", c=CR)
                return h.dma_start(out=dst, in_=src)
            s.dma("pool", f, reads=toks, writes=[("my", hf)])

    def exchange2():
        toks = []
        for k in range(4):
            s.collective(lambda h, k=k: h.collective_compute(
                "AllGather", ALU.bypass, replica_groups=RG, ins=[omine[k * 128:(k + 1) * 128, :]], outs=[oall[k * 256:(k + 1) * 256, :]]),
                reads=s.group("omine"), writes=[("oall", k)])
            toks.append(("oall", k))
        for half in range(2):
            def f(h, half=half):
                r = rank_of(h)
                return h.dma_start(out=omy[half * 512:(half + 1) * 512, :], in_=oall[half * 512:(half + 1) * 512, bass.ds(r * T, T)])
            s.dma("pool", f, reads=toks, writes=[("omy", half)])

    def phase_B(l):
        s.barrier()
        ar.reset()
        lam_init = 0.8 - 0.6 * float(np.exp(-0.3 * l))
        Kt = [ar.alloc([128, S], BF16) for _ in range(2)]
        Vsb = ar.alloc([128, NKB, 65], BF16)
        Qt = [[ar.alloc([128, 512], BF16) for _ in range(2)] for _ in range(2)]
        P = [ar.alloc([128, 512], BF16) for _ in range(4)]
        Ssb = [ar.alloc([128, 512], F32) for _ in range(2)]
        rec = ar.alloc([128, 512], F32)
        bcs = ar.alloc([128, 512], F32)
        onr = [ar.alloc([128, 512], F32) for _ in range(2)]
        df = ar.alloc([128, 512], F32)
        sq = ar.alloc([128, 512], F32)
        rsb = ar.alloc([128, 512], F32)
        ost = [ar.alloc([128, 512], BF16) for _ in range(2)]
        acc = ar.alloc([128, S], F32)
        dl = ar.alloc([128, 128], F32)
        sm = ar.alloc([128, 16], F32)
        G = gains[l]
        myr = s.group("my")
        HQ = NQT // 2

        s.dma("sp", lambda h: h.dma_start(out=dl, in_=bass.AP(tensor=dlam_h[l], offset=0, ap=[[0, 128], [1, 128]])), writes=["dl"])
        s.dma("sp", lambda h: h.dma_start(out=sm[:, 8:10], in_=bass.AP(tensor=sinks_h[l], offset=0, ap=[[0, 128], [1, 2]])), writes=["sink"])
        s.op("dve", lambda h: h.tensor_tensor(out=dl[:, 0:32], in0=dl[:, 0:32], in1=dl[:, 32:64], op=ALU.mult), reads=["dl"], writes=["dl"])
        s.op("dve", lambda h: h.tensor_tensor(out=dl[:, 64:96], in0=dl[:, 64:96], in1=dl[:, 96:128], op=ALU.mult), reads=["dl"], writes=["dl"])
        s.op("dve", lambda h: h.reduce_sum(out=sm[:, 0:1], in_=dl[:, 0:32], axis=mybir.AxisListType.X), reads=["dl"], writes=["sm"])
        s.op("dve", lambda h: h.reduce_sum(out=sm[:, 1:2], in_=dl[:, 64:96], axis=mybir.AxisListType.X), reads=["dl", "sm"], writes=["sm"])
        s.op("act", lambda h: h.activation(out=sm[:, 2:4], in_=sm[:, 0:2], func=AF.Exp), reads=["sm"], writes=["sm"])
        s.op("dve", lambda h: h.tensor_tensor(out=sm[:, 4:5], in0=sm[:, 3:4], in1=sm[:, 2:3], op=ALU.subtract), reads=["sm"], writes=["sm"])
        s.op("dve", lambda h: h.tensor_scalar(out=sm[:, 4:5], in0=sm[:, 4:5], scalar1=-lam_init, scalar2=None, op0=ALU.add), reads=["sm"], writes=["sm"])
        s.op("dve", lambda h: h.tensor_scalar(out=sm[:, 5:6], in0=G[:, 20:21], scalar1=1.0 - lam_init, scalar2=None, op0=ALU.mult),
             reads=["sm", ("gains", l)], writes=["sm"])
        s.op("act", lambda h: h.activation(out=sm[:, 10:12], in_=sm[:, 8:10], func=AF.Exp), reads=["sink", "sm"], writes=["sm"])
        neglam = sm[:, 4:5]
        gainA = sm[:, 5:6]
        s.op("dve", lambda h: h.memset(Vsb[:, :, 64:65], 1.0), writes=["Vones"])

        def load_rows(dst_tile, prow, nrows, row, tok_fn, cols=None):
            for hf in range(2):
                s.dma("sp", lambda h, hf=hf: h.dma_start(out=dst_tile[prow:prow + nrows, hf * T:(hf + 1) * T],
                                                        in_=my[hf * GROWS + row: hf * GROWS + row + nrows, :]),
                      reads=myr, writes=[tok_fn(hf)])

        def load_V(slot, d):
            for hf in range(2):
                c16 = [("Vsb", k) for k in range(hf * NBT // 16, (hf + 1) * NBT // 16)]
                if d == 1:
                    src = bass.AP(tensor=my_h, offset=(hf * GROWS + FMROWS + slot * 64) * T, ap=[[64, 128], [128 * 64, NBT], [1, 64]])
                    s.dma("sp", lambda h, hf=hf, src=src: h.dma_start(out=Vsb[:, hf * NBT:(hf + 1) * NBT, 0:64], in_=src),
                          reads=myr, writes=c16)
                else:
                    ncb = T // (128 * d)
                    for c in range(ncb):
                        src = bass.AP(tensor=my_h, offset=(hf * GROWS + FMROWS + slot * 64) * T + c * 128 * d * 64,
                                      ap=[[d * 64, 128], [64, d], [1, 64]])
                        b0 = hf * NBT + c * d
                        s.dma("sp", lambda h, src=src, b0=b0: h.dma_start(out=Vsb[:, b0:b0 + d, 0:64], in_=src),
                              reads=myr, writes=[("Vsb", b0 // 16)])

        def finalize_norm(src65, qt_cols_tok, extra_den=None):
            if extra_den is not None:
                s.op("dve", lambda h: h.tensor_scalar(out=rec[64:65, :], in0=src65[64:65, :], scalar1=extra_den, scalar2=None, op0=ALU.add),
                     reads=qt_cols_tok + ["sm"], writes=["rec"])
                s.op("dve", lambda h: h.reciprocal(out=rec[64:65, :], in_=rec[64:65, :]), reads=["rec"], writes=["rec"])
            else:
                s.op("dve", lambda h: h.reciprocal(out=rec[64:65, :], in_=src65[64:65, :]), reads=qt_cols_tok, writes=["rec"])
            s.op("pe", lambda h: h.matmul(ps[7][0:64, :], onesf[64:65, 0:64], rec[64:65, :], start=True, stop=True),
                 reads=["rec", "onesf"], writes=[("ps", 7)])
            s.op("act", lambda h: h.copy(out=bcs[0:64], in_=ps[7][0:64]), reads=[("ps", 7)], writes=["bcs"])

        def store_o(st, mixer, j, qt):
            dst = omine[mixer * 128 + j * 64: mixer * 128 + j * 64 + 64, qt * 512:(qt + 1) * 512]
            s.dma("act", lambda h: h.dma_start(out=dst, in_=ost[st][0:64]), reads=[("ost", st)], writes=[("omine", mixer, j, qt)])

        def dense_head(kind, j):
            if kind == "A":
                maps, dk, dd = 2, 96, 32
                scale = 32 ** -0.5
                rq = [R_A + j * 128, R_A + j * 128 + 32]
                rk = [R_A + j * 128 + 64, R_A + j * 128 + 96]
                vslot, mixer = VSLOT["A%d" % j], 0
            else:
                maps, dk, dd = 1, 96, 96
                scale = 96 ** -0.5
                rq = [R_CQ + j * 96]
                rk = [R_CK + j * 64]
                vslot, mixer = VSLOT["C%d" % j], 2
            for m in range(maps):
                if kind == "A":
                    for p0 in (32, 64):
                        s.op("dve", lambda h, m=m, p0=p0: h.memset(Kt[m][p0:p0 + 32, :], 0.0), writes=[("Kt", m, "aug")])
                        for b_ in range(2):
                            s.op("dve", lambda h, m=m, b_=b_, p0=p0: h.memset(Qt[b_][m][p0:p0 + 32, :], 0.0), writes=[("Qa", b_, m)])
                    load_rows(Kt[m], 0, 32, rk[m], lambda hf, m=m: ("Kt", m, hf))
                    s.dma("sp", lambda h, m=m: h.dma_start(out=Kt[m][32:38, :], in_=kaug_b[j]), reads=["kaug_b"], writes=[("Kt", m, "aug")])
                else:
                    load_rows(Kt[m], 0, 64, rk[m], lambda hf, m=m: ("Kt", m, hf))
                    load_rows(Kt[m], 64, 32, R_KPE, lambda hf, m=m: ("Kt", m, "aug"))
            load_V(vslot, 1)
            def qtile(qt, hook):
                b = qt % 2
                hfq, lq = qt // HQ, qt % HQ
                nr = 32 if kind == "A" else 96
                for m in range(maps):
                    s.dma("sp", lambda h, m=m: h.dma_start(out=Qt[b][m][0:nr, :],
                                                           in_=my[hfq * GROWS + rq[m]: hfq * GROWS + rq[m] + nr, lq * 512:(lq + 1) * 512]),
                          reads=myr, writes=[("Qt", b, m)])
                    if kind == "A":
                        s.dma("sp", lambda h, m=m: h.dma_start(out=Qt[b][m][32:38, :], in_=qaug_b[j][:, qt * 512:(qt + 1) * 512]),
                              reads=["qaug_b"], writes=[("Qa", b, m)])
                kb_lo = 0
                if kind == "A":
                    sl_min = min(float(_alibi_slopes()[0][HMAP_A[0][j]]), float(_alibi_slopes()[0][HMAP_A[1][j]]))
                    kb_lo = max(0, int(np.ceil((qt * 512 - 127 - ALIBI_TH / sl_min) / 128.0)))
                kbs = list(range(kb_lo, 4 * qt + 4))
                units = [(kb, m) for kb in kbs for m in range(maps)]
                ob = [3 + 2 * (qt % 2) + m for m in range(maps)]
                pend = []

                def issue_S(kb, m):
                    bank = nxt("SB", 3)
                    hfk = kb // NBT
                    s.op("pe", lambda h: h.matmul(ps[bank][:, :], Kt[m][0:dk, kb * 128:(kb + 1) * 128], Qt[b][m][0:dk, :], start=True, stop=True),
                         reads=[("Kt", m, hfk), ("Kt", m, "aug"), ("Qt", b, m), ("Qa", b, m)], writes=[("ps", bank)])
                    pi = nxt("P", 4)
                    v = kb - 4 * qt
                    if v >= 0:
                        si = nxt("Ssb", 2)
                        s.op("dve", lambda h: h.tensor_tensor(out=Ssb[si], in0=ps[bank], in1=masks[:, v, :], op=ALU.add),
                             reads=[("ps", bank), "masks"], writes=[("Ssb", si)])
                        s.op("act", lambda h: h.activation(out=P[pi], in_=Ssb[si], func=AF.Exp, scale=scale), reads=[("Ssb", si)], writes=[("P", pi)])
                    else:
                        s.op("act", lambda h: h.activation(out=P[pi], in_=ps[bank], func=AF.Exp, scale=scale), reads=[("ps", bank)], writes=[("P", pi)])
                    return pi

                def issue_PV(kb, m, pi):
                    first, last = kb == kbs[0], kb == kbs[-1]
                    s.op("pe", lambda h: h.matmul(ps[ob[m]][0:65, :], Vsb[:, kb, 0:65], P[pi], start=first, stop=last),
                         reads=[("Vsb", kb // 16), "Vones", ("P", pi)], writes=[("ps", ob[m])] if (first or last) else [])

                for ui, (kb, m) in enumerate(units):
                    pi = issue_S(kb, m)
                    pend.append((kb, m, pi))
                    if len(pend) > 2:
                        issue_PV(*pend.pop(0))
                    if ui == 3 and hook is not None:
                        hook()
                        hook = None
                while pend:
                    issue_PV(*pend.pop(0))
                if hook is not None:
                    hook()
                return lambda: fin(qt, ob)

            def fin(qt, ob):
                st = nxt("ost", 2)
                if kind == "C":
                    finalize_norm(ps[ob[0]], [("ps", ob[0])])
                    s.op("dve", lambda h: h.tensor_tensor(out=ost[st][0:64], in0=ps[ob[0]][0:64], in1=bcs[0:64], op=ALU.mult),
                         reads=[("ps", ob[0]), "bcs"], writes=[("ost", st)])
                else:
                    for m in range(2):
                        finalize_norm(ps[ob[m]], [("ps", ob[m])])
                        s.op("dve", lambda h, m=m: h.tensor_tensor(out=onr[m][0:64], in0=ps[ob[m]][0:64], in1=bcs[0:64], op=ALU.mult),
                             reads=[("ps", ob[m]), "bcs"], writes=[("onr", m)])
                    s.op("dve", lambda h: h.scalar_tensor_tensor(out=df[0:64], in0=onr[1][0:64], scalar=neglam[0:64], in1=onr[0][0:64],
                                                                 op0=ALU.mult, op1=ALU.add),
                         reads=[("onr", 0), ("onr", 1), "sm"], writes=["df"])
                    s.op("act", lambda h: h.activation(out=sq[0:64], in_=df[0:64], func=AF.Square), reads=["df"], writes=["sq"])
                    s.op("pe", lambda h: h.matmul(ps[7][0:64, :], onesf[0:64, 0:64], sq[0:64, :], start=True, stop=True),
                         reads=["sq", "onesf"], writes=[("ps", 7)])
                    s.op("act", lambda h: h.activation(out=rsb[0:64], in_=ps[7][0:64], func=AF.Sqrt, scale=1.0 / 64, bias=EPS),
                         reads=[("ps", 7)], writes=["rsb"])
                    s.op("dve", lambda h: h.reciprocal(out=rsb[0:64], in_=rsb[0:64]), reads=["rsb"], writes=["rsb"])
                    s.op("dve", lambda h: h.scalar_tensor_tensor(out=ost[st][0:64], in0=df[0:64], scalar=gainA[0:64], in1=rsb[0:64],
                                                                 op0=ALU.mult, op1=ALU.mult),
                         reads=["df", "rsb", "sm"], writes=[("ost", st)])
                store_o(st, mixer, j, qt)

            prev = None
            for qt in range(NQT):
                prev = qtile(qt, prev)
            prev()

        def banded_head(kind, j):
            scale = 64 ** -0.5
            if kind == "B":
                rowq, rowk = R_B + j * 128, R_B + j * 128 + 64
                augslot, vslot, mixer = 2 + j, VSLOT["B%d" % j], 1
                patterns = [(1, 6), (4, 6), (16, 6)]
            else:
                rowq, rowk = R_DQ + j * 64, R_DK
                augslot, vslot, mixer = 4 + j, VSLOT["D"], 3
                patterns = [(1, 5)]
            Kf, Qf = Kt[0], Kt[1]
            s.op("dve", lambda h: h.memset(Kf[64:96, :], 0.0), writes=[("Kt", 0, "aug")])
            s.op("dve", lambda h: h.memset(Qf[64:96, :], 0.0), writes=[("Kt", 1, "aug")])
            load_rows(Kf, 0, 64, rowk, lambda hf: ("Kt", 0, hf))
            s.dma("sp", lambda h: h.dma_start(out=Kf[64:70, :], in_=kaug_b[augslot]), reads=["kaug_b"], writes=[("Kt", 0, "aug")])
            load_rows(Qf, 0, 64, rowq, lambda hf: ("Kt", 1, hf))
            s.dma("sp", lambda h: h.dma_start(out=Qf[64:70, :], in_=qaug_b[augslot]), reads=["qaug_b"], writes=[("Kt", 1, "aug")])
            kq_r = [("Kt", m, x) for m in range(2) for x in (0, 1, "aug")]
            def group4(pidx, d, mprev, g4):
                if True:
                    idxs = [g4 * 4 + i for i in range(4)]
                    cr = [divmod(ix, d) for ix in idxs]
                    base = [128 * d * c + r for (c, r) in cr]
                    has_prev = [c >= 1 for (c, r) in cr]
                    bo = nxt("SB", 3)
                    for i in range(4):
                        sl = slice(base[i], base[i] + 127 * d + 1, d)
                        s.op("pe", lambda h, i=i, sl=sl: h.matmul(ps[bo][:, i * 128:(i + 1) * 128], Kf[0:96, sl], Qf[0:96, sl], start=True, stop=True),
                             reads=kq_r, writes=[("ps", bo)] if i in (0, 3) else [])
                    so = nxt("Ssb", 2)
                    s.op("dve", lambda h: h.tensor_tensor(out=Ssb[so], in0=ps[bo], in1=masks[:, 4, :], op=ALU.add),
                         reads=[("ps", bo), "masks"], writes=[("Ssb", so)])
                    po = nxt("P", 4)
                    s.op("act", lambda h: h.activation(out=P[po], in_=Ssb[so], func=AF.Exp, scale=scale), reads=[("Ssb", so)], writes=[("P", po)])
                    pp = None
                    if any(has_prev):
                        bp = nxt("SB", 3)
                        ii = [i for i in range(4) if has_prev[i]]
                        for i in ii:
                            slq = slice(base[i], base[i] + 127 * d + 1, d)
                            slk = slice(base[i] - 128 * d, base[i] - d + 1, d)
                            s.op("pe", lambda h, i=i, slq=slq, slk=slk: h.matmul(ps[bp][:, i * 128:(i + 1) * 128], Kf[0:96, slk], Qf[0:96, slq],
                                                                                start=True, stop=True),
                                 reads=kq_r, writes=[("ps", bp)] if i in (ii[0], ii[-1]) else [])
                        sp_ = nxt("Ssb", 2)
                        s.op("dve", lambda h: h.tensor_tensor(out=Ssb[sp_], in0=ps[bp], in1=masks[:, mprev, :], op=ALU.add),
                             reads=[("ps", bp), "masks"], writes=[("Ssb", sp_)])
                        pp = nxt("P", 4)
                        s.op("act", lambda h: h.activation(out=P[pp], in_=Ssb[sp_], func=AF.Exp, scale=scale), reads=[("Ssb", sp_)], writes=[("P", pp)])
                    obk = 3 + nxt("OB", 4)
                    nmm = []
                    for i in range(4):
                        if has_prev[i]:
                            nmm.append((i, idxs[i] - d, pp, True, False))
                        nmm.append((i, idxs[i], po, not has_prev[i], True))
                    for k_, (i, vb, pt, st_, sp2) in enumerate(nmm):
                        s.op("pe", lambda h, i=i, vb=vb, pt=pt, st_=st_, sp2=sp2: h.matmul(
                            ps[obk][0:65, i * 128:(i + 1) * 128], Vsb[:, vb, 0:65], P[pt][:, i * 128:(i + 1) * 128], start=st_, stop=sp2),
                            reads=[("Vsb", vb // 16), "Vones", ("P", pt)], writes=[("ps", obk)] if k_ in (0, len(nmm) - 1) else [])
                    for i in range(4):
                        sl = slice(base[i], base[i] + 127 * d + 1, d)
                        tl = sorted(set([base[i] // 512, (base[i] + 128 * d - 1) // 512]))
                        toks = [("acc", q_) for q_ in range(tl[0], tl[-1] + 1)]
                        if pidx == 0:
                            s.op("act", lambda h, i=i, sl=sl: h.copy(out=acc[0:65, sl], in_=ps[obk][0:65, i * 128:(i + 1) * 128]),
                                 reads=[("ps", obk)], writes=toks)
                        else:
                            s.op("dve", lambda h, i=i, sl=sl: h.tensor_tensor(out=acc[0:65, sl], in0=ps[obk][0:65, i * 128:(i + 1) * 128],
                                                                              in1=acc[0:65, sl], op=ALU.add),
                                 reads=[("ps", obk)] + toks, writes=toks)
            for pidx, (d, mprev) in enumerate(patterns):
                load_V(vslot, d)
                for g4 in range(NKB // 4):
                    group4(pidx, d, mprev, g4)
            for qt in range(NQT):
                cols = slice(qt * 512, (qt + 1) * 512)
                finalize_norm(acc[0:65, cols], [("acc", qt)], extra_den=(sm[64:65, 10 + j:11 + j] if kind == "D" else None))
                st = nxt("ost", 2)
                s.op("dve", lambda h, cols=cols, st=st: h.tensor_tensor(out=ost[st][0:64], in0=acc[0:64, cols], in1=bcs[0:64], op=ALU.mult),
                     reads=[("acc", qt), "bcs"], writes=[("ost", st)])
                store_o(st, mixer, j, qt)

        for j in range(2):
            banded_head("D", j)
        for j in range(2):
            banded_head("B", j)
        for j in range(2):
            dense_head("C", j)
        for j in range(2):
            dense_head("A", j)

    def phase_C(l, xsrc, xdst, final):
        s.barrier()
        ar.reset()
        moe = (l % 2 == 1)
        NFC = NFCE if moe else NFC0
        nexp = NEXP if moe else 1
        wout = ar.alloc([128, 8, 1024], BF16)
        xs = [ar.alloc([128, 4, 1024], F32) for _ in range(2)]
        oT1 = ar.alloc([128, 8, 512], BF16)
        oT = [oT1, oT1]
        xn = ar.alloc([128, 4, 1024], F32 if moe else BF16)
        junk = ar.alloc([128, 1024], BF16)
        h2T = [ar.alloc([128, 8, 512], BF16) for _ in range(2)]
        h2Tf = ar.alloc([128, 8, 512], F32) if moe else None
        actT = ar.alloc([128, NFC, 512], BF16)
        silt = [ar.alloc([128, 512], BF16) for _ in range(2)]
        w13 = [ar.alloc([128, 8, 256], BF16) for _ in range(3)]
        w2 = [ar.alloc([128, 4, 512], BF16) for _ in range(3)]
        ss4 = ar.alloc([128, 4], F32)
        rs4 = ar.alloc([128, 4], F32)
        fn = ar.alloc([128, 1024], F32) if final else None
        if moe:
            rt = ar.alloc([128, 8, 8], F32)
            lg = ar.alloc([128, 4, 8], F32)
            gt = [ar.alloc([128, 4, 8], F32) for _ in range(2)]
            t8 = [ar.alloc([128, 8], F32) for _ in range(3)]
            m4 = ar.alloc([128, 8], F32)
        G = gains[l]
        omr = s.group("omy")
        for kc in range(0, 8, 4):
            s.dma("sp", lambda h, kc=kc: h.dma_start(out=wout[:, kc:kc + 4, :], in_=wout_b[l][:, kc:kc + 4, :]),
                  reads=[("wb_wout", l, kc // 4)], writes=[("wout", kc // 4)])
        if moe:
            s.dma("sp", lambda h: h.dma_start(out=rt, in_=router_d), writes=["rt"])
        if final:
            s.dma("sp", lambda h: h.dma_start(out=fn, in_=bass.AP(tensor=fnorm_h, offset=0, ap=[[0, 128], [1, 1024]])), writes=["fn"])

        def load_tile(t):
            b = t % 2
            s.dma("sp", lambda h: h.dma_start(out=xs[b], in_=xsrc[t * 512:(t + 1) * 512, :].rearrange("(b p) d -> p b d", p=128)),
                  writes=[("xs", b, i) for i in range(4)])
            for kc in range(0, 8, 4):
                s.dma("sp", lambda h, kc=kc: h.dma_start(out=oT[b][:, kc:kc + 4, :],
                                                        in_=omy[kc * 128:(kc + 4) * 128, t * 512:(t + 1) * 512].rearrange("(k p) c -> p k c", p=128)),
                      reads=omr, writes=[("oT", 0, kc // 4)])

        def w13_src(e, fc):
            return (w13_m_b[e, fc], ("wb_w13_m", e, fc)) if moe else (w13_0_b[fc], ("wb_w13_0", fc // 4))

        def w2_src(e, hf, f0):
            if moe:
                return w2_m_b[e, hf, f0:f0 + 4].rearrange("f p c -> p f c"), ("wb_w2_m", e, hf, f0 // 4), ("wb_w2_m", e, hf, f0 // 4)
            f1 = min(NFC, f0 + 4)
            return w2_0_b[hf, f0:f1].rearrange("f p c -> p f c"), ("wb_w2_0", hf, f0 // 8), ("wb_w2_0", hf, (f1 - 1) // 8)

        def tileC(t, part, hooks=None):
            b = t % 2
            X = xs[b]
            H2 = h2T[b]
            GT = gt[b] if moe else None
            if part == 1:
                for blk in range(4):
                    for hf in range(2):
                        bank = nxt("CB", 4)
                        mm_group(ps[bank][:, :], [(oT[b][:, kc, blk * 128:(blk + 1) * 128], wout[:, kc, hf * 512:(hf + 1) * 512],
                                                   [("oT", 0, kc // 4), ("wout", kc // 4)]) for kc in range(8)], ("ps", bank))
                        s.op("dve", lambda h, blk=blk, hf=hf, bank=bank: h.tensor_tensor(
                            out=X[:, blk, hf * 512:(hf + 1) * 512], in0=ps[bank], in1=X[:, blk, hf * 512:(hf + 1) * 512], op=ALU.add),
                            reads=[("ps", bank), ("xs", b, blk)], writes=[("xs", b, blk)])
                s.op("dve", lambda h: h.memset(ss4, 0.0), writes=[("ss4", i) for i in range(4)])
                for blk in range(4):
                    s.op("act", lambda h, blk=blk: h.activation(out=junk, in_=X[:, blk, :], func=AF.Square, accum_out=ss4[:, blk:blk + 1]),
                         reads=[("xs", b, blk), ("ss4", blk)], writes=[("ss4", blk)])
                rms_rstd(ss4, rs4, D, [("ss4", i) for i in range(4)], "rs4")
                for blk in range(4):
                    s.op("dve", lambda h, blk=blk: h.tensor_scalar(out=xn[:, blk, :], in0=X[:, blk, :], scalar1=rs4[:, blk:blk + 1],
                                                                  scalar2=None, op0=ALU.mult),
                         reads=[("xs", b, blk), "rs4"], writes=[("xn", blk)])
            if part == 2:
                for blk in range(4):
                    if not moe:
                        tb = nxt("CB", 4)
                        for kc in range(8):
                            s.op("pe", lambda h, blk=blk, kc=kc, tb=tb: h.transpose(out=psb[tb][:, kc * 128:(kc + 1) * 128],
                                                                                    in_=xn[:, blk, kc * 128:(kc + 1) * 128], identity=identb),
                                 reads=[("xn", blk), "identb"], writes=[("ps", tb)] if kc in (0, 7) else [])
                        s.op("dve", lambda h, blk=blk, tb=tb: h.tensor_tensor(
                            out=H2[:, :, blk * 128:(blk + 1) * 128], in0=psb[tb].rearrange("p (k t) -> p k t", k=8),
                            in1=G[:, 8:16].unsqueeze(2).broadcast_to([128, 8, 128]), op=ALU.mult),
                            reads=[("ps", tb), ("gains", l)], writes=[("h2T", b, blk)])
                    else:
                        for q in range(2):
                            tb = nxt("CB", 4)
                            for k4 in range(4):
                                kc = q * 4 + k4
                                s.op("pe", lambda h, blk=blk, kc=kc, k4=k4, tb=tb: h.transpose(out=ps[tb][:, k4 * 128:(k4 + 1) * 128],
                                                                                              in_=xn[:, blk, kc * 128:(kc + 1) * 128], identity=identf),
                                     reads=[("xn", blk), "identf"], writes=[("ps", tb)] if k4 in (0, 3) else [])
                            s.op("dve", lambda h, blk=blk, q=q, tb=tb: h.tensor_tensor(
                                out=h2Tf[:, q * 4:q * 4 + 4, blk * 128:(blk + 1) * 128], in0=ps[tb].rearrange("p (k t) -> p k t", k=4),
                                in1=G[:, 8 + q * 4:12 + q * 4].unsqueeze(2).broadcast_to([128, 4, 128]), op=ALU.mult),
                                reads=[("ps", tb), ("gains", l)], writes=[("h2Tf", blk, q)])
                            s.op("pool", lambda h, blk=blk, q=q: h.tensor_copy(out=H2[:, q * 4:q * 4 + 4, blk * 128:(blk + 1) * 128],
                                                                              in_=h2Tf[:, q * 4:q * 4 + 4, blk * 128:(blk + 1) * 128]),
                                 reads=[("h2Tf", blk, q)], writes=[("h2T", b, blk)])
                        bank = nxt("CB", 4)
                        mm_group(ps[bank][:, 0:8], [(h2Tf[:, kc, blk * 128:(blk + 1) * 128], rt[:, kc, :], [("h2Tf", blk, kc // 4), "rt"])
                                                    for kc in range(8)], ("ps", bank))
                        L = lg[:, blk, :]
                        s.op("act", lambda h, L=L, bank=bank: h.copy(out=L, in_=ps[bank][:, 0:8]), reads=[("ps", bank)], writes=["lg"])
                        s.op("dve", lambda h, L=L: h.reduce_max(out=m4[:, 0:1], in_=L, axis=mybir.AxisListType.X), reads=["lg"], writes=["m4"])
                        s.op("dve", lambda h, L=L: h.tensor_scalar(out=t8[0], in0=L, scalar1=m4[:, 0:1], scalar2=None, op0=ALU.is_equal),
                             reads=["lg", "m4"], writes=["t80"])
                        s.op("dve", lambda h, L=L: h.scalar_tensor_tensor(out=t8[1], in0=t8[0], scalar=NEG, in1=L, op0=ALU.mult, op1=ALU.add),
                             reads=["t80", "lg"], writes=["t81"])
                        s.op("dve", lambda h: h.reduce_max(out=m4[:, 1:2], in_=t8[1], axis=mybir.AxisListType.X), reads=["t81", "m4"], writes=["m4"])
                        s.op("dve", lambda h: h.tensor_scalar(out=t8[2], in0=t8[1], scalar1=m4[:, 1:2], scalar2=None, op0=ALU.is_equal),
                             reads=["t81", "m4"], writes=["t82"])
                        s.op("dve", lambda h: h.tensor_tensor(out=m4[:, 2:3], in0=m4[:, 0:1], in1=m4[:, 1:2], op=ALU.subtract), reads=["m4"], writes=["m4"])
                        s.op("act", lambda h: h.activation(out=m4[:, 3:4], in_=m4[:, 2:3], func=AF.Sigmoid), reads=["m4"], writes=["m4"])
                        s.op("act", lambda h: h.activation(out=m4[:, 4:5], in_=m4[:, 2:3], func=AF.Sigmoid, scale=-1.0), reads=["m4"], writes=["m4"])
                        s.op("dve", lambda h: h.tensor_scalar(out=t8[0], in0=t8[0], scalar1=m4[:, 3:4], scalar2=None, op0=ALU.mult),
                             reads=["t80", "m4"], writes=["t80"])
                        s.op("dve", lambda h, blk=blk: h.scalar_tensor_tensor(out=GT[:, blk, :], in0=t8[2], scalar=m4[:, 4:5], in1=t8[0],
                                                                             op0=ALU.mult, op1=ALU.add),
                             reads=["t80", "t82", "m4"], writes=[("gt", b, blk)])
            if part == 3:
                h2r = [("h2T", b, i) for i in range(4)]
                for e in range(nexp):
                    for fc in range(NFC):
                        wi = nxt("w13", 3)
                        src, tokw = w13_src(e, fc)
                        s.dma("sp", lambda h, wi=wi, src=src: h.dma_start(out=w13[wi], in_=src), reads=[tokw], writes=[("w13", wi)])
                        b1 = nxt("CB", 4)
                        mm_group(ps[b1][:, :], [(w13[wi][:, kc, 0:128], H2[:, kc, :], [("w13", wi)] + h2r) for kc in range(8)], ("ps", b1))
                        b3 = nxt("CB", 4)
                        mm_group(ps[b3][:, :], [(w13[wi][:, kc, 128:256], H2[:, kc, :], [("w13", wi)] + h2r) for kc in range(8)], ("ps", b3))
                        si = nxt("silt", 2)
                        s.op("act", lambda h, b1=b1, si=si: h.activation(out=silt[si], in_=ps[b1], func=AF.Silu), reads=[("ps", b1)], writes=[("silt", si)])
                        s.op("dve", lambda h, b3=b3, si=si, fc=fc: h.tensor_tensor(out=actT[:, fc, :], in0=ps[b3], in1=silt[si], op=ALU.mult),
                             reads=[("ps", b3), ("silt", si)], writes=[("actT", fc)])
                    for hf in range(2):
                        for f0 in range(0, NFC, 4):
                            f1 = min(NFC, f0 + 4)
                            wi = nxt("w2", 3)
                            src, tk0, tk1 = w2_src(e, hf, f0)
                            s.dma("sp", lambda h, wi=wi, src=src, n=f1 - f0: h.dma_start(out=w2[wi][:, 0:n, :], in_=src),
                                  reads=[tk0, tk1], writes=[("w2", wi)])
                            for fc in range(f0, f1):
                                for blk in range(4):
                                    s.op("pe", lambda h, fc=fc, blk=blk, wi=wi, f0=f0: h.matmul(
                                        ps[4 + blk][:, :], actT[:, fc, blk * 128:(blk + 1) * 128], w2[wi][:, fc - f0, :],
                                        start=(fc == 0), stop=(fc == NFC - 1)),
                                        reads=[("actT", fc), ("w2", wi)], writes=[("ps", 4 + blk)] if fc in (0, NFC - 1) else [])
                        for blk in range(4):
                            if moe:
                                s.op("dve", lambda h, blk=blk, hf=hf, e=e: h.scalar_tensor_tensor(
                                    out=X[:, blk, hf * 512:(hf + 1) * 512], in0=ps[4 + blk], scalar=GT[:, blk, e:e + 1],
                                    in1=X[:, blk, hf * 512:(hf + 1) * 512], op0=ALU.mult, op1=ALU.add),
                                    reads=[("ps", 4 + blk), ("gt", b, blk), ("xs", b, blk)], writes=[("xs", b, blk)])
                            else:
                                s.op("dve", lambda h, blk=blk, hf=hf: h.tensor_tensor(
                                    out=X[:, blk, hf * 512:(hf + 1) * 512], in0=ps[4 + blk], in1=X[:, blk, hf * 512:(hf + 1) * 512], op=ALU.add),
                                    reads=[("ps", 4 + blk), ("xs", b, blk)], writes=[("xs", b, blk)])
                    if hooks and e in hooks:
                        hooks[e]()
            if part == 4:
                xr = [("xs", b, i) for i in range(4)]
                if final:
                    s.op("dve", lambda h: h.memset(ss4, 0.0), writes=[("ss4", i) for i in range(4)])
                    for blk in range(4):
                        s.op("act", lambda h, blk=blk: h.activation(out=junk, in_=X[:, blk, :], func=AF.Square, accum_out=ss4[:, blk:blk + 1]),
                             reads=[("xs", b, blk), ("ss4", blk)], writes=[("ss4", blk)])
                    rms_rstd(ss4, rs4, D, [("ss4", i) for i in range(4)], "rs4")
                    for blk in range(4):
                        s.op("dve", lambda h, blk=blk: h.scalar_tensor_tensor(out=X[:, blk, :], in0=X[:, blk, :], scalar=rs4[:, blk:blk + 1], in1=fn,
                                                                             op0=ALU.mult, op1=ALU.mult),
                             reads=[("xs", b, blk), "rs4", "fn"], writes=[("xs", b, blk)])
                s.dma("act", lambda h: h.dma_start(out=xdst[t * 512:(t + 1) * 512, :].rearrange("(b p) d -> p b d", p=128), in_=X),
                      reads=xr, writes=[("xdst", t)])

        load_tile(0)
        tileC(0, 1)
        tileC(0, 2)
        for t in range(NTA):
            nx = t + 1 < NTA
            if nx:
                load_tile(t + 1)
            hooks = None
            if nx and moe:
                hooks = {0: (lambda t=t: tileC(t + 1, 1)), 1: (lambda t=t: tileC(t + 1, 2))}
            tileC(t, 3, hooks)
            if nx and not moe:
                tileC(t + 1, 1)
                tileC(t + 1, 2)
            tileC(t, 4)

    for l in range(n_layers):
        xsrc = x_in if l == 0 else x1
        phase_A(l, xsrc)
        dump("mine%d" % l, mine, s.group("mine"))
        if stop_after == ("A", l):
            break
        exchange1()
        if l == 0:
            cast_ffn0()
        if l == 0 and n_layers == 2:
            cast_layer_small(1)
            for e in range(NEXP // 2):
                cast_moe(e)
        if l == 1:
            for e in range(NEXP // 2, NEXP):
                cast_moe(e)
        dump("my%d" % l, my, s.group("my"))
        phase_B(l)
        dump("omine%d" % l, omine, s.group("omine"))
        if stop_after == ("B", l):
            break
        exchange2()
        final = (l == n_layers - 1)
        phase_C(l, xsrc, out_d if final else x1, final)
    s.barrier(full=True)
    s.emit()
    return nc


def run(inputs, S, dbg=(), stop_after=None, n_layers=2):
    import time
    t0 = time.time()
    in_maps = _prep(inputs, S)
    t1 = time.time()
    nc = build(S, dbg=dbg, stop_after=stop_after, n_layers=n_layers)
    t2 = time.time()
    if n_layers < 2 or stop_after is not None:
        names = set(_USED)
        in_maps = [{k: v for k, v in m.items() if k in names} for m in in_maps]
    res = run_bass_kernel_spmd(nc, in_maps, core_ids=list(range(len(in_maps))))
    print("prep %.1fs build %.1fs run %.1fs" % (t1 - t0, t2 - t1, time.time() - t2), flush=True)
    return res


def kernel(**inputs):
    x = np.asarray(inputs["x"])
    B, S, _ = x.shape
    res = run(inputs, S)
    T = S // 2
    out = np.empty((B, S, D), np.float32)
    for c in range(2 * B):
        out[c // 2, (c % 2) * T:(c % 2 + 1) * T] = res.results[c]["out"]
    return out
```
